# Optimizing a Trainium2 kernel written in Bass

```python
import math
import jax, jax.numpy as jnp
from jax import lax
import numpy as np

D_MODEL = 1024
BATCH = 8
SEQ = 2048
DEPTH = 2

D_CONV = 512
CONV_GROUPS = 8
CONV_WIDTH = 3
N_HEADS = 8
QK_NOPE = 64
QK_ROPE = 32
V_HEAD = 64
Q_LORA = 384
KV_LORA = 256
ROPE_THETA = 10000.0
Q_BLOCK = 128
D_FF = 2816
N_EXPERTS = 8
TOP_K = 2
D_FF_EXPERT = 1408
N_DENSE = (DEPTH + 1) // 2
N_MOE = DEPTH // 2
EPS = 1e-6
IN_WIDTHS = (D_CONV, D_CONV, D_CONV, Q_LORA, KV_LORA, QK_ROPE, D_MODEL, D_MODEL)
D_IN = sum(IN_WIDTHS)

kernel_name = "hybrid_gated_conv_mla_moe_trunk"


def rmsnorm(x, g):
    xf = x.astype(jnp.float32)
    y = xf * lax.rsqrt(jnp.mean(xf * xf, axis=-1, keepdims=True) + EPS)
    return (y * g.astype(jnp.float32)).astype(x.dtype)


def rope_tables(positions, dtype):
    inv_freq = ROPE_THETA ** (-jnp.arange(0, QK_ROPE, 2, dtype=jnp.float32) / QK_ROPE)
    ang = positions.astype(jnp.float32)[..., None] * inv_freq
    return jnp.cos(ang).astype(dtype), jnp.sin(ang).astype(dtype)


def apply_rope(x, cos, sin):
    half = x.shape[-1] // 2
    x1, x2 = x[..., :half], x[..., half:]
    return jnp.concatenate([x1 * cos - x2 * sin, x2 * cos + x1 * sin], axis=-1)


def causal_short_conv(u, w):
    s = u.shape[1]
    up = jnp.pad(u, ((0, 0), (CONV_WIDTH - 1, 0), (0, 0)))
    return sum(up[:, k:k + s] * w[k] for k in range(CONV_WIDTH))


def causal_block_attention(q, k, v):
    b, s, h, dqk = q.shape
    nb = s // Q_BLOCK
    scale = 1.0 / math.sqrt(dqk)
    qb = q.reshape(b, nb, Q_BLOCK, h, dqk).transpose(1, 0, 2, 3, 4)
    key_pos = jnp.arange(s)

    def one_block(args):
        q_blk, i = args
        sc = jnp.einsum('bqhd,bkhd->bhqk', q_blk, k).astype(jnp.float32) * scale
        q_pos = i * Q_BLOCK + jnp.arange(Q_BLOCK)
        mask = key_pos[None, :] <= q_pos[:, None]
        sc = jnp.where(mask[None, None], sc, -1e30)
        p = jax.nn.softmax(sc, axis=-1).astype(v.dtype)
        return jnp.einsum('bhqk,bkhd->bqhd', p, v)

    out = lax.map(one_block, (qb, jnp.arange(nb)))
    return out.transpose(1, 0, 2, 3, 4).reshape(b, s, h, v.shape[-1])


def token_mixer(xn, cos, sin, w_in, conv_w, w_conv_out, q_norm, w_uq, kv_norm, w_ukv, w_mla_out, w_o):
    b, s, _ = xn.shape
    offsets = list(np.cumsum(IN_WIDTHS)[:-1])
    b_g, c_g, u, c_q, c_kv, k_r, g_conv, g_mla = jnp.split(xn @ w_in, offsets, axis=-1)

    y_conv = (b_g * causal_short_conv(c_g * u, conv_w)) @ w_conv_out

    q = (rmsnorm(c_q, q_norm) @ w_uq).reshape(b, s, N_HEADS, QK_NOPE + QK_ROPE)
    q_nope, q_pe = q[..., :QK_NOPE], q[..., QK_NOPE:]
    q_pe = apply_rope(q_pe, cos[:, :, None, :], sin[:, :, None, :])
    kv = (rmsnorm(c_kv, kv_norm) @ w_ukv).reshape(b, s, N_HEADS, QK_NOPE + V_HEAD)
    k_nope, v = kv[..., :QK_NOPE], kv[..., QK_NOPE:]
    k_pe = apply_rope(k_r, cos, sin)
    k_pe = jnp.broadcast_to(k_pe[:, :, None, :], (b, s, N_HEADS, QK_ROPE))
    q_full = jnp.concatenate([q_nope, q_pe], axis=-1)
    k_full = jnp.concatenate([k_nope, k_pe], axis=-1)
    attn = causal_block_attention(q_full, k_full, v).reshape(b, s, N_HEADS * V_HEAD)
    y_mla = attn @ w_mla_out

    merged = jax.nn.sigmoid(g_conv) * y_conv + jax.nn.sigmoid(g_mla) * y_mla
    return merged @ w_o


def swiglu(x, wg, wu, wd):
    return (jax.nn.silu(x @ wg) * (x @ wu)) @ wd


def moe_swiglu(x, router, wg, wu, wd):
    b, s, d = x.shape
    xt = x.reshape(b * s, d)
    logits = (xt @ router).astype(jnp.float32)
    top_vals, top_idx = lax.top_k(logits, TOP_K)
    top_w = jax.nn.softmax(top_vals, axis=-1)
    combine = jnp.sum(jax.nn.one_hot(top_idx, N_EXPERTS, dtype=jnp.float32) * top_w[..., None], axis=1)
    combine = combine.astype(x.dtype)
    y = jnp.zeros_like(xt)
    for e in range(N_EXPERTS):
        y = y + combine[:, e:e + 1] * swiglu(xt, wg[e], wu[e], wd[e])
    return y.reshape(b, s, d)


def setup_inputs(seed: int = 0) -> dict:
    key = jax.random.key(seed)
    ks = jax.random.split(key, 24)
    f32 = jnp.float32

    def nrm(k, shape, fan_in):
        return jax.random.normal(k, shape, f32) * (fan_in ** -0.5)

    def gain(k, shape):
        return 1.0 + 0.02 * jax.random.normal(k, shape, f32)

    x = jax.random.normal(ks[0], (BATCH, SEQ, D_MODEL), f32)
    offset = jax.random.randint(ks[1], (BATCH, 1), 0, 1024, dtype=jnp.int32)
    positions = offset + jnp.arange(SEQ, dtype=jnp.int32)[None, :]
    return {
        "x": x,
        "positions": positions,
        "attn_norm": gain(ks[2], (DEPTH, D_MODEL)),
        "w_in": nrm(ks[3], (DEPTH, D_MODEL, D_IN), D_MODEL),
        "conv_w": nrm(ks[4], (DEPTH, CONV_WIDTH, D_CONV), CONV_WIDTH),
        "w_conv_out": nrm(ks[5], (DEPTH, D_CONV, D_MODEL), D_CONV),
        "q_norm": gain(ks[6], (DEPTH, Q_LORA)),
        "w_uq": nrm(ks[7], (DEPTH, Q_LORA, N_HEADS * (QK_NOPE + QK_ROPE)), Q_LORA),
        "kv_norm": gain(ks[8], (DEPTH, KV_LORA)),
        "w_ukv": nrm(ks[9], (DEPTH, KV_LORA, N_HEADS * (QK_NOPE + V_HEAD)), KV_LORA),
        "w_mla_out": nrm(ks[10], (DEPTH, N_HEADS * V_HEAD, D_MODEL), N_HEADS * V_HEAD),
        "w_o": nrm(ks[11], (DEPTH, D_MODEL, D_MODEL), D_MODEL),
        "ffn_norm": gain(ks[12], (DEPTH, D_MODEL)),
        "w_gate": nrm(ks[13], (N_DENSE, D_MODEL, D_FF), D_MODEL),
        "w_up": nrm(ks[14], (N_DENSE, D_MODEL, D_FF), D_MODEL),
        "w_down": nrm(ks[15], (N_DENSE, D_FF, D_MODEL), D_FF),
        "router": nrm(ks[16], (N_MOE, D_MODEL, N_EXPERTS), D_MODEL),
        "w_gate_e": nrm(ks[17], (N_MOE, N_EXPERTS, D_MODEL, D_FF_EXPERT), D_MODEL),
        "w_up_e": nrm(ks[18], (N_MOE, N_EXPERTS, D_MODEL, D_FF_EXPERT), D_MODEL),
        "w_down_e": nrm(ks[19], (N_MOE, N_EXPERTS, D_FF_EXPERT, D_MODEL), D_FF_EXPERT),
        "final_norm": gain(ks[20], (D_MODEL,)),
    }


def reference(x, positions, attn_norm, w_in, conv_w, w_conv_out, q_norm, w_uq, kv_norm, w_ukv,
              w_mla_out, w_o, ffn_norm, w_gate, w_up, w_down, router, w_gate_e, w_up_e, w_down_e,
              final_norm):
    cos, sin = rope_tables(positions, x.dtype)
    for l in range(DEPTH):
        xn = rmsnorm(x, attn_norm[l])
        x = x + token_mixer(xn, cos, sin, w_in[l], conv_w[l], w_conv_out[l], q_norm[l], w_uq[l],
                            kv_norm[l], w_ukv[l], w_mla_out[l], w_o[l])
        hn = rmsnorm(x, ffn_norm[l])
        if l % 2 == 0:
            i = l // 2
            x = x + swiglu(hn, w_gate[i], w_up[i], w_down[i])
        else:
            i = l // 2
            x = x + moe_swiglu(hn, router[i], w_gate_e[i], w_up_e[i], w_down_e[i])
    return rmsnorm(x, final_norm)
```

```python
import math
import numpy as np
import concourse.bass as bass
import concourse.mybir as mybir
from concourse.bass_utils import run_bass_kernel_spmd

F32 = mybir.dt.float32
BF16 = mybir.dt.bfloat16
I32 = mybir.dt.int32
ALU = mybir.AluOpType
AF = mybir.ActivationFunctionType
AX = mybir.AxisListType

D = 1024
S = 2048
KC = 8
TT = 512
NT = 4
NH = 8
DIN = 4256
DFF = 2816
DFE = 1408
NE = 8
EPS = 1e-6
NV = 80
SCALE = 1.0 / math.sqrt(96.0)
ARENA_BYTES = 140800
GSZ = (6, 5)
SAME_ENGINE_SYNC = True


class Tok:
    __slots__ = ("eng", "idx", "sem", "val")

    def __init__(self, eng=None, idx=None, sem=None, val=None):
        self.eng, self.idx, self.sem, self.val = eng, idx, sem, val


class Buf:
    __slots__ = ("w", "r", "name")

    def __init__(self, name=""):
        self.w = None
        self.r = []
        self.name = name


class Eng:
    def __init__(self, name, sem, dma_sems):
        self.name = name
        self.sem = sem
        self.ops = []
        self.waited = {}
        self.last_real = -1
        self.dma_sems = dma_sems
        self.dma_cnt = [0] * len(dma_sems)
        self.dma_rr = 0


class Sched:
    def __init__(self, sems):
        it = iter(sems)
        self.e = {}
        for name, nd in (("pe", 0), ("act", 0), ("dve", 0), ("pool", 24), ("sp", 12)):
            s = next(it)
            self.e[name] = Eng(name, s, [next(it) for _ in range(nd)])

    def _waits(self, e, toks):
        best = {}
        for t in toks:
            if t is None:
                continue
            if t.eng is None:
                k = ("d", id(t.sem))
                if e.waited.get(k, 0) >= t.val:
                    continue
                if k not in best or best[k].val < t.val:
                    best[k] = t
            else:
                if t.eng is e and (e.name == "pe" or not SAME_ENGINE_SYNC):
                    continue
                k = ("e", t.eng.name)
                if e.waited.get(k, -1) >= t.idx:
                    continue
                if k not in best or best[k].idx < t.idx:
                    best[k] = t
        out = []
        for k, t in best.items():
            if t.eng is None:
                e.waited[k] = t.val
            else:
                e.waited[k] = t.idx
                t.eng.ops[t.idx][2] = True
            out.append(t)
        return out

    @staticmethod
    def _deps(reads, writes):
        toks = []
        for b in reads:
            toks.append(b.w)
        for b in writes:
            toks.append(b.w)
            toks.extend(b.r)
        return toks

    @staticmethod
    def _mark(tok, reads, writes):
        for b in reads:
            b.r.append(tok)
        for b in writes:
            b.w = tok
            b.r = []

    def op(self, eng, fn, reads=(), writes=()):
        e = self.e[eng]
        waits = self._waits(e, self._deps(reads, writes))
        idx = len(e.ops)
        e.ops.append([waits, fn, False])
        e.last_real = idx
        tok = Tok(eng=e, idx=idx)
        self._mark(tok, reads, writes)
        return tok

    def dma(self, eng, out, in_, reads=(), writes=()):
        e = self.e[eng]
        slot = e.dma_rr % len(e.dma_sems)
        e.dma_rr += 1
        sem = e.dma_sems[slot]
        toks = self._deps(reads, writes)
        if e.dma_cnt[slot] > 0:
            toks.append(Tok(sem=sem, val=e.dma_cnt[slot]))
        waits = self._waits(e, toks)
        e.dma_cnt[slot] += 16
        tok = Tok(sem=sem, val=e.dma_cnt[slot])
        e.ops.append([waits, (lambda h, o=out, i=in_: h.dma_start(out=o, in_=i)), (sem, 16)])
        self._mark(tok, reads, writes)
        return tok

    def _all_toks(self, skip=None):
        toks = []
        for f in self.e.values():
            if f is not skip and f.last_real >= 0:
                toks.append(Tok(eng=f, idx=f.last_real))
            for s, c in zip(f.dma_sems, f.dma_cnt):
                if c > 0:
                    toks.append(Tok(sem=s, val=c))
        return toks

    def barrier(self):
        for e in self.e.values():
            own = (e.name == "pe" or not SAME_ENGINE_SYNC)
            waits = self._waits(e, self._all_toks(skip=e if own else None))
            if waits:
                e.ops.append([waits, None, False])

    def final_wait(self, eng):
        e = self.e[eng]
        waits = self._waits(e, self._all_toks(skip=e))
        if waits:
            e.ops.append([waits, None, False])

    def finalize(self):
        self.cnt = {}
        for name, e in self.e.items():
            c = 0
            arr = []
            for (_, fn, sig) in e.ops:
                if sig is True:
                    c += 1
                arr.append(c)
            self.cnt[name] = arr

    def emit(self, name, h):
        for waits, fn, sig in self.e[name].ops:
            for t in waits:
                if t.eng is None:
                    h.wait_ge(t.sem, t.val)
                else:
                    h.wait_ge(t.eng.sem, self.cnt[t.eng.name][t.idx])
            if fn is not None:
                ins = fn(h)
                if sig is True:
                    ins.then_inc(self.e[name].sem, 1)
                elif sig:
                    ins.then_inc(sig[0], sig[1])


def build_nc(layers, last=True, do_mixer=True, do_ffn=True):
    nc = bass.Bass("TRN2", target_bir_lowering=False)

    def din(name, shape, dt=F32):
        return nc.dram_tensor(name, list(shape), dt, kind="ExternalInput").ap()

    xT = din("xT", [D, S])
    posrep = din("posrep", [32, S], I32)
    vecs_d = din("vecs", [128, NV])
    cf_d = din("cf", [128, 128 + 1024])
    cb_d = din("cb", [128, 256])
    w_in = din("w_in", [2, D, DIN])
    w_conv_out = din("w_conv_out", [2, 512, D])
    w_uq = din("w_uq", [2, 384, 768])
    wq_rot = din("wq_rot", [2, 384, 256])
    wkr = din("wkr", [2, D, 32])
    wkr_rot = din("wkr_rot", [2, D, 32])
    w_ukv = din("w_ukv", [2, 256, 1024])
    w_mla_out = din("w_mla_out", [2, 512, D])
    w_o = din("w_o", [2, D, D])
    w_gate = din("w_gate", [1, D, DFF])
    w_up = din("w_up", [1, D, DFF])
    w_down = din("w_down", [1, DFF, D])
    router = din("router", [1, D, NE])
    w_gate_e = din("w_gate_e", [1, NE, D, DFE])
    w_up_e = din("w_up_e", [1, NE, D, DFE])
    w_down_e = din("w_down_e", [1, NE, DFE, D])
    outT = nc.dram_tensor("outT", [D, S], F32, kind="ExternalOutput").ap()

    import contextlib
    es = contextlib.ExitStack()
    with es:
        XRES = es.enter_context(nc.sbuf_tensor("XRES", [128, KC, S], F32))
        VECS = es.enter_context(nc.sbuf_tensor("VECS", [128, NV], F32))
        CF = es.enter_context(nc.sbuf_tensor("CF", [128, 128 + 1024], F32))
        CB = es.enter_context(nc.sbuf_tensor("CB", [128, 256], BF16))
        ONESB = es.enter_context(nc.sbuf_tensor("ONESB", [128, 128], BF16))
        ONESF = es.enter_context(nc.sbuf_tensor("ONESF", [128, 128], F32))
        ARENA = es.enter_context(nc.sbuf_tensor("ARENA", [128, ARENA_BYTES // 2], BF16))
        PS = [es.enter_context(nc.psum_tensor(f"PS{i}", [128, 512], F32)) for i in range(8)]
        sems = [es.enter_context(nc.semaphore(f"sem{i}")) for i in range(5 + 24 + 12)]
        block = es.enter_context(nc.Block())

        sch = Sched(sems)
        IDENTF = CF[:, 0:128]
        SELF = CF[:, 128:128 + 1024].rearrange("p (e m) -> p e m", e=8)
        IDENTB = CB[:, 0:128]
        TRIB = CB[:, 128:256]

        def view(off, shape, dt):
            n = int(np.prod(shape))
            esz = 4 if dt in (F32, I32) else 2
            assert off % 4 == 0 and off + n * esz <= ARENA_BYTES, (off, shape, ARENA_BYTES)
            a = ARENA[:, off // 2: off // 2 + n * esz // 2]
            if dt != BF16:
                a = a.bitcast(dt)
            if len(shape) == 2:
                a = a.rearrange("p (a b) -> p a b", a=shape[0])
            elif len(shape) == 3:
                a = a.rearrange("p (a b c) -> p a b c", a=shape[0], b=shape[1])
            return a

        class Alloc:
            def __init__(self, start=0):
                self.off = start

            def get(self, shape, dt):
                n = int(np.prod(shape))
                esz = 4 if dt in (F32, I32) else 2
                v = view(self.off, shape, dt)
                self.off += (n * esz + 63) // 64 * 64
                return v

        def mm(out, lhsT, rhs, start, stop, reads, writes):
            sch.op("pe", lambda h: h.matmul(out, lhsT=lhsT, rhs=rhs, start=start, stop=stop,
                                            skip_group_check=True),
                   reads=reads, writes=writes)

        def act(out, in_, func, reads, writes, scale=1.0, bias=0.0):
            sch.op("act", lambda h: h.activation(out=out, in_=in_, func=func, bias=bias, scale=scale),
                   reads=reads, writes=writes)

        def tt(eng, out, in0, in1, op, reads, writes):
            sch.op(eng, lambda h: h.tensor_tensor(out=out, in0=in0, in1=in1, op=op),
                   reads=reads, writes=writes)

        def stt(eng, out, in0, scalar, in1, op0, op1, reads, writes):
            sch.op(eng, lambda h: h.scalar_tensor_tensor(out=out, in0=in0, scalar=scalar, in1=in1,
                                                          op0=op0, op1=op1),
                   reads=reads, writes=writes)

        def ts(eng, out, in0, s1, op0, reads, writes, s2=None, op1=None):
            if op1 is None:
                sch.op(eng, lambda h: h.tensor_scalar(out=out, in0=in0, scalar1=s1, scalar2=None, op0=op0),
                       reads=reads, writes=writes)
            else:
                sch.op(eng, lambda h: h.tensor_scalar(out=out, in0=in0, scalar1=s1, scalar2=s2,
                                                      op0=op0, op1=op1),
                       reads=reads, writes=writes)

        def cp(eng, out, in_, reads, writes):
            if eng == "act":
                sch.op("act", lambda h: h.copy(out=out, in_=in_), reads=reads, writes=writes)
            else:
                sch.op(eng, lambda h: h.tensor_copy(out=out, in_=in_), reads=reads, writes=writes)

        def memset(eng, ap, val, writes):
            sch.op(eng, lambda h: h.memset(ap, val), reads=(), writes=writes)

        bX = [[Buf(f"x{c}_{t}") for t in range(NT)] for c in range(KC)]
        bPS = [Buf(f"ps{i}") for i in range(8)]
        bCONST = Buf("const")

        def tsl(t):
            return slice(t * TT, (t + 1) * TT)

        sch.dma("sp", VECS[:], vecs_d[:], writes=[bCONST])
        sch.dma("sp", CF[:], cf_d[:], writes=[bCONST])
        sch.dma("pool", CB[:], cb_d[:], writes=[bCONST])
        memset("dve", ONESB[:], 1.0, [bCONST])
        memset("dve", ONESF[:], 1.0, [bCONST])
        xT_v = xT.rearrange("(c p) s -> p c s", p=128)
        x_loaded = [False]

        def load_x_tiles(t0, t1):
            for t in range(t0, t1):
                sch.dma("sp", XRES[:, :, tsl(t)], xT_v[:, :, tsl(t)], writes=[bX[c][t] for c in range(KC)],
                        reads=([bX[0][t - 1]] if t > 0 else []))

        load_x_tiles(0, 1)

        def vcol(i):
            return VECS[:, i:i + 1]

        def rms_scale(srcs, src_bufs, nfeat, SQ, bSQ, RS, bRS, bank):
            n = len(srcs)
            for i, (s, sb) in enumerate(zip(srcs, src_bufs)):
                q = i % 2
                act(SQ[q], s, AF.Square, reads=[sb], writes=[bSQ[q]])
                mm(PS[bank][:], ONESB[:], SQ[q], i == 0, i == n - 1,
                   reads=[bSQ[q], bCONST], writes=[bPS[bank]])
            act(RS, PS[bank][:], AF.Ln, reads=[bPS[bank]], writes=[bRS], scale=1.0 / nfeat, bias=EPS)
            act(RS, RS, AF.Exp, reads=[bRS], writes=[bRS], scale=-0.5)

        def emit_xn(t, gcol0, XNT, bXNT, SQ, bSQ, RS, bRS, bank):
            rms_scale([XRES[:, c, tsl(t)] for c in range(KC)], [bX[c][t] for c in range(KC)], D,
                      SQ, bSQ, RS, bRS, bank)
            for c in range(KC):
                stt("dve", XNT[:, c, :], XRES[:, c, tsl(t)], vcol(gcol0 + c), RS, ALU.mult, ALU.mult,
                    reads=[bX[c][t], bRS, bCONST], writes=[bXNT[c]])

        def wload(dst, src, buf):
            sch.dma("pool", dst, src, writes=[buf])

        def kview(w2d):
            return w2d.rearrange("(c p) n -> p c n", p=128)

        W1_OFF = ARENA_BYTES - 17920
        w1state = {}

        def w1_load(l):
            al = Alloc(W1_OFF)
            WINB = al.get([KC, 672], BF16)
            WKR = al.get([KC, 96], BF16)
            WKRROT = al.get([KC, 96], BF16)
            WKVK = al.get([2, 512], BF16)
            WKVV = al.get([2, 512], BF16)
            assert al.off <= ARENA_BYTES
            bWINB, bWKR, bWKRROT, bWKVK, bWKVV = Buf(), Buf(), Buf(), Buf(), Buf()
            bWKZ = Buf()
            win_v = kview(w_in[l])
            memset("dve", WKR[:, :, 0:64], 0.0, [bWKZ])
            memset("dve", WKRROT[:, :, 0:64], 0.0, [bWKZ])
            wload(WKR[:, :, 64:96], kview(wkr[l]), bWKR)
            wload(WKRROT[:, :, 64:96], kview(wkr_rot[l]), bWKRROT)
            wload(WINB, win_v[:, :, 1536:2208], bWINB)
            ukv = w_ukv[l].rearrange("(c p) (h e) -> p c h e", p=128, e=128)
            for c in range(2):
                wload(WKVK[:, c, :].rearrange("p (h e) -> p h e", e=64), ukv[:, c, :, 0:64], bWKVK)
                wload(WKVV[:, c, :].rearrange("p (h e) -> p h e", e=64), ukv[:, c, :, 64:128], bWKVV)
            w1state[l] = (WINB, WKR, WKRROT, WKVK, WKVV, bWINB, bWKR, bWKRROT, bWKVK, bWKVV, bWKZ)

        def mixer(l):
            sch.barrier()
            al = Alloc()
            KT = al.get([NH, S], BF16)
            V = al.get([16, NH, 65], BF16)
            c_off = al.off
            CQN = al.get([3, S], BF16)
            WQ = al.get([3, 768], BF16)
            WQROT = al.get([3, NH, 96], BF16)
            COS = al.get([S], F32)
            SIN = al.get([S], F32)
            p12 = al.off
            XNTS = [al.get([KC, TT], BF16) for _ in range(2)]
            SQ = [al.get([TT], BF16) for _ in range(2)]
            RS = al.get([TT], F32)
            RS2 = al.get([TT], F32)
            SQ2 = SQ
            CF32 = al.get([5, TT], F32)
            CKVN = al.get([2, TT], BF16)
            T1 = CF32[:, 3, :]
            T2 = CF32[:, 0, :]
            RT0 = CF32[:, 1, :]
            RT1 = CF32[:, 2, :]
            assert al.off <= W1_OFF, al.off

            bKT = [[Buf() for _ in range(NT)] for _ in range(NH)]
            bV = [Buf() for _ in range(16)]
            bCQN = [[Buf() for _ in range(NT)] for _ in range(3)]
            bWQ, bWQROT = Buf(), Buf()
            bROPE = [Buf() for _ in range(NT)]
            bXNTS = [[Buf() for _ in range(KC)] for _ in range(2)]
            bSQ = [Buf(), Buf()]
            bSQ2 = bSQ
            bRS, bRS2 = Buf(), Buf()
            bCF32 = [Buf() for _ in range(5)]
            bT1, bT2, bRT0, bRT1 = bCF32[3], bCF32[0], bCF32[1], bCF32[2]
            bCKVN = [Buf(), Buf()]

            sch.dma("pool", RT0[slice(64, 96), :], posrep[:, tsl(0)], writes=[bRT0])
            if l not in w1state:
                w1_load(l)
            (WINB, WKR, WKRROT, WKVK, WKVV, bWINB, bWKR, bWKRROT, bWKVK, bWKVV, bWKZ) = w1state[l]
            win_v = kview(w_in[l])
            bWQZ = Buf()
            memset("dve", WQROT[:, :, :, 0:64], 0.0, [bWQZ])
            wload(WQ, kview(w_uq[l]), bWQ)
            for c in range(3):
                wload(WQROT[:, c, :, 64:96], kview(wq_rot[l])[:, c, :].rearrange("p (h e) -> p h e", e=32), bWQROT)

            R = slice(64, 96)

            def rope_pass(t):
                if t > 0:
                    sch.dma("pool", RT0[R, :], posrep[:, tsl(t)], writes=[bRT0])
                ts("dve", RT0[R, :], RT0[R, :], VECS[R, 74:75], ALU.mult, reads=[bRT0, bCONST], writes=[bRT0])
                for tab, shift in ((SIN, 0.0), (COS, math.pi / 2)):
                    tv = tab[R, tsl(t)]
                    ts("dve", tv, RT0[R, :], shift, ALU.add, reads=[bRT0], writes=[bROPE[t]],
                       s2=1.0 / (2 * math.pi), op1=ALU.mult)
                    ts("dve", RT1[R, :], tv, 12582912.0, ALU.add, reads=[bROPE[t]], writes=[bRT1])
                    ts("dve", tv, RT1[R, :], -12582912.0, ALU.add, reads=[bRT1], writes=[bROPE[t]])
                    stt("dve", tv, tv, -2 * math.pi, RT0[R, :], ALU.mult, ALU.add,
                        reads=[bROPE[t], bRT0], writes=[bROPE[t]])
                    ts("dve", tv, tv, shift, ALU.add, reads=[bROPE[t]], writes=[bROPE[t]],
                       s2=3.1415925, op1=ALU.min)
                    ts("dve", tv, tv, -3.1415925, ALU.max, reads=[bROPE[t]], writes=[bROPE[t]])
                    act(tv, tv, AF.Sin, reads=[bROPE[t]], writes=[bROPE[t]])

            memset("dve", V.rearrange("p a h e -> p (a h) e")[:, :, 64:65], 1.0, bV)

            qg = 40 + l * 3
            kg = 46 + l * 2
            rope_pass(0)
            if not x_loaded[0]:
                load_x_tiles(1, NT)
                x_loaded[0] = True
            emit_xn(0, l * 8, XNTS[0], bXNTS[0], SQ2, bSQ2, RS2, bRS2, 7)
            for t in range(NT):
                XNT, bXNT = XNTS[t % 2], bXNTS[t % 2]
                for oc in range(5):
                    bk = oc % 4
                    for k in range(KC):
                        mm(PS[bk][:], WINB[:, k, oc * 128:(oc + 1) * 128], XNT[:, k, :], k == 0, k == KC - 1,
                           reads=[bWINB, bXNT[k]], writes=[bPS[bk]])
                    cp("dve", CF32[:, oc, :], PS[bk][:], reads=[bPS[bk]], writes=[bCF32[oc]])
                if t == 0:
                    ts("dve", WKRROT[:, :, 64:80], WKRROT[:, :, 64:80], -1.0, ALU.mult, reads=[], writes=[bWKRROT])
                for k in range(KC):
                    mm(PS[4][0:96, :], WKR[:, k, :], XNT[:, k, :], k == 0, k == KC - 1,
                       reads=[bWKR, bWKZ, bXNT[k]], writes=[bPS[4]])
                for k in range(KC):
                    mm(PS[5][0:96, :], WKRROT[:, k, :], XNT[:, k, :], k == 0, k == KC - 1,
                       reads=[bWKRROT, bWKZ, bXNT[k]], writes=[bPS[5]])
                if t + 1 < NT:
                    emit_xn(t + 1, l * 8, XNTS[(t + 1) % 2], bXNTS[(t + 1) % 2], SQ2, bSQ2, RS2, bRS2, 7)
                rms_scale([CF32[:, 3 + c, :] for c in range(2)], bCF32[3:5], 256, SQ, bSQ, RS, bRS, 6)
                for c in range(2):
                    stt("dve", CKVN[:, c, :], CF32[:, 3 + c, :], vcol(kg + c), RS, ALU.mult, ALU.mult,
                        reads=[bCF32[3 + c], bRS, bCONST], writes=[bCKVN[c]])
                rms_scale([CF32[:, c, :] for c in range(3)], bCF32[0:3], 384, SQ, bSQ, RS, bRS, 6)
                for c in range(3):
                    stt("dve", CQN[:, c, tsl(t)], CF32[:, c, :], vcol(qg + c), RS, ALU.mult, ALU.mult,
                        reads=[bCF32[c], bRS, bCONST], writes=[bCQN[c][t]])
                tt("dve", T1[R, :], PS[4][R, :], COS[R, tsl(t)], ALU.mult, reads=[bPS[4], bROPE[t]], writes=[bT1])
                tt("dve", T2[R, :], PS[5][R, :], SIN[R, tsl(t)], ALU.mult, reads=[bPS[5], bROPE[t]], writes=[bT2])
                tt("dve", KT[R, 0, tsl(t)], T1[R, :], T2[R, :], ALU.add, reads=[bT1, bT2], writes=[bKT[0][t]])
                for h in range(1, NH):
                    cp("act", KT[R, h, tsl(t)], KT[R, 0, tsl(t)], reads=[bKT[0][t]], writes=[bKT[h][t]])
                for hp in range(4):
                    bk = hp % 4
                    for k in range(2):
                        mm(PS[bk][:], WKVK[:, k, hp * 128:(hp + 1) * 128], CKVN[:, k, :], k == 0, k == 1,
                           reads=[bWKVK, bCKVN[k]], writes=[bPS[bk]])
                    cp("act", KT[0:64, 2 * hp, tsl(t)], PS[bk][0:64, :], reads=[bPS[bk]], writes=[bKT[2 * hp][t]])
                    cp("dve", KT[0:64, 2 * hp + 1, tsl(t)], PS[bk][64:128, :], reads=[bPS[bk]],
                       writes=[bKT[2 * hp + 1][t]])
                for bi in range(4):
                    blk = 4 * t + bi
                    bk = 4 + (bi % 2)
                    for k in range(2):
                        mm(PS[bk][:], CKVN[:, k, bi * 128:(bi + 1) * 128], WKVV[:, k, :], k == 0, k == 1,
                           reads=[bWKVV, bCKVN[k]], writes=[bPS[bk]])
                    cp("act", V[:, blk, :, 0:64], PS[bk][:].rearrange("p (h e) -> p h e", e=64),
                       reads=[bPS[bk]], writes=[bV[blk]])
                if t + 1 < NT:
                    rope_pass(t + 1)
            ts("dve", WQROT[:, :, :, 64:80], WQROT[:, :, :, 64:80], -1.0, ALU.mult, reads=[], writes=[bWQROT])

            sch.barrier()
            al = Alloc(p12)
            QT = [al.get([NH, TT], BF16) for _ in range(2)]
            PT = [al.get([TT], BF16) for _ in range(4)]
            ONUM = [al.get([TT], F32) for _ in range(2)]
            RDEN = [al.get([TT], F32) for _ in range(2)]
            RD = [al.get([TT], BF16) for _ in range(2)]
            T1s = [al.get([TT], F32) for _ in range(2)]
            _t2 = al.get([TT], F32)
            T2s = [_t2, _t2]
            assert al.off <= ARENA_BYTES - 16384, al.off
            ATT = view(ARENA_BYTES - 16384, [4, S], BF16)
            WC = view(c_off, [KC, 1536], BF16)
            bWC = Buf()
            bQT = [[Buf() for _ in range(NH)] for _ in range(2)]
            bPT = [Buf() for _ in range(4)]
            bONUM, bRDEN = [Buf(), Buf()], [Buf(), Buf()]
            bRD = [Buf(), Buf()]
            for q in range(2):
                memset("dve", RD[q], 0.0, [bRD[q]])
            _b2 = Buf()
            bT1s, bT2s = [Buf(), Buf()], [_b2, _b2]
            bATT = [[Buf() for _ in range(NT)] for _ in range(NH)]

            QBUF = {3: 0, 2: 0, 0: 1, 1: 1}

            def qproj(qt, h):
                Q, bQ = QT[QBUF[qt]], bQT[QBUF[qt]]
                bq, br = 6, 7
                z = h % 2
                for k in range(3):
                    mm(PS[bq][0:96, :], WQ[:, k, h * 96:(h + 1) * 96], CQN[:, k, tsl(qt)], k == 0, k == 2,
                       reads=[bWQ, bCQN[k][qt]], writes=[bPS[bq]])
                for k in range(3):
                    mm(PS[br][0:96, :], WQROT[:, k, h, :], CQN[:, k, tsl(qt)], k == 0, k == 2,
                       reads=[bWQROT, bWQZ, bCQN[k][qt]], writes=[bPS[br]])
                cp("dve", Q[0:64, h, :], PS[bq][0:64, :], reads=[bPS[bq]], writes=[bQ[h]])
                tt("dve", T1s[z][R, :], PS[bq][R, :], COS[R, tsl(qt)], ALU.mult, reads=[bPS[bq], bROPE[qt]], writes=[bT1s[z]])
                tt("dve", T2s[z][R, :], PS[br][R, :], SIN[R, tsl(qt)], ALU.mult, reads=[bPS[br], bROPE[qt]], writes=[bT2s[z]])
                tt("dve", Q[R, h, :], T1s[z][R, :], T2s[z][R, :], ALU.add, reads=[bT1s[z], bT2s[z]], writes=[bQ[h]])

            def normalize_a(z, ob):
                cp("dve", ONUM[z][0:64, :], PS[ob][0:64, :], reads=[bPS[ob]], writes=[bONUM[z]])
                act(RDEN[z][64:65, :], PS[ob][64:65, :], AF.Ln, reads=[bPS[ob]], writes=[bRDEN[z]])
                act(RDEN[z][64:65, :], RDEN[z][64:65, :], AF.Exp, reads=[bRDEN[z]], writes=[bRDEN[z]], scale=-1.0)
                cp("dve", RD[z][64:65, :], RDEN[z][64:65, :], reads=[bRDEN[z]], writes=[bRD[z]])
                tt("dve", RD[z][0:1, :], RDEN[z][64:65, :], RD[z][64:65, :], ALU.subtract,
                   reads=[bRDEN[z], bRD[z]], writes=[bRD[z]])

            def normalize_b(z, qt, h):
                mm(PS[7][0:64, :], ONESB[0:65, 0:64], RD[z][0:65, :], True, True,
                   reads=[bRD[z], bCONST], writes=[bPS[7]])
                r0 = (h % 2) * 64
                tt("dve", ATT[r0:r0 + 64, h // 2, tsl(qt)], ONUM[z][0:64, :], PS[7][0:64, :], ALU.mult,
                   reads=[bONUM[z], bPS[7]], writes=[bATT[h][qt]])

            for h in range(NH):
                qproj(3, h)
            for h in range(NH):
                qproj(0, h)
            LA = 3
            NDEF = 8
            cnt = [0, 0]
            NEXTQ = {3: 2, 0: 1}
            for (qa, qb) in ((3, 0), (2, 1)):
                jobs = []
                for h in range(NH):
                    jobs.append((qa, h))
                    jobs.append((qb, h))
                items = [(ji, kc) for ji, (qt, h) in enumerate(jobs) for kc in range(4 * qt + 4)]
                slots = {}
                pending = []

                def SC(i):
                    ji, kc = items[i]
                    qt, h = jobs[ji]
                    Q, bQ = QT[QBUF[qt]], bQT[QBUF[qt]]
                    j = kc - 4 * qt
                    c0 = 128 * j if j > 0 else 0
                    sb = cnt[0] % 4
                    cnt[0] += 1
                    slots[i] = (sb, c0)
                    mm(PS[sb][:, c0:TT], KT[0:96, h, kc * 128:(kc + 1) * 128], Q[0:96, h, c0:TT],
                       True, j < 0, reads=[bKT[h][kc // 4], bQ[h]], writes=[bPS[sb]])
                    if j >= 0:
                        mm(PS[sb][:, c0:c0 + 128], IDENTB, TRIB, False, True, reads=[bCONST], writes=[bPS[sb]])

                def E(i):
                    ji, kc = items[i]
                    qt, h = jobs[ji]
                    nk = 4 * qt + 4
                    sb, c0 = slots.pop(i)
                    z = ji % 2
                    ob = 4 + z
                    p = cnt[1] % 4
                    cnt[1] += 1
                    act(PT[p][:, c0:TT], PS[sb][:, c0:TT], AF.Exp, reads=[bPS[sb]], writes=[bPT[p]], scale=SCALE)
                    mm(PS[ob][0:65, c0:TT], V[:, kc, h, :], PT[p][:, c0:TT], kc == 0, kc == nk - 1,
                       reads=[bV[kc], bPT[p]], writes=[bPS[ob]])
                    if kc == nk - 1:
                        normalize_a(z, ob)
                        if qt in NEXTQ:
                            qproj(NEXTQ[qt], h)
                            if qt == 0 and h == NH - 1:
                                dead = [b for row in bCQN for b in row] + [bWQ, bWQROT] + bROPE
                                sch.dma("pool", WC, win_v[:, :, 0:1536], writes=[bWC] + dead)
                        pending.append((i + NDEF, z, qt, h))

                n = len(items)
                for i in range(n + LA):
                    if i < n:
                        SC(i)
                    if i >= LA:
                        E(i - LA)
                    while pending and pending[0][0] <= i - LA:
                        _, z, qt, h = pending.pop(0)
                        normalize_b(z, qt, h)
                while pending:
                    _, z, qt, h = pending.pop(0)
                    normalize_b(z, qt, h)

            sch.barrier()
            al = Alloc()
            XNT3 = [al.get([KC, TT], BF16) for _ in range(2)]
            SQ = [al.get([TT], BF16) for _ in range(2)]
            RS = al.get([TT], F32)
            CSB = al.get([TT], F32)
            CU = al.get([4, TT + 16], F32)
            T1 = al.get([TT], F32)
            T2 = CSB
            p3_end = max(al.off, c_off - 16384)
            assert p3_end + 16384 <= c_off, p3_end
            CONVIN = view(ARENA_BYTES - 32768, [4, S], BF16)
            bXNT3 = [[Buf() for _ in range(KC)] for _ in range(2)]
            bSQ = [Buf(), Buf()]
            bRS, bCSB, bT1 = Buf(), Buf(), Buf()
            bT2 = bCSB
            bCU = [Buf() for _ in range(4)]
            bCONVIN = [[Buf() for _ in range(NT)] for _ in range(4)]
            memset("dve", CU[:, :, 0:2], 0.0, bCU)
            WGA = view(p3_end, [KC, 1024], BF16)
            al4 = Alloc(c_off + 24576)
            WO = al4.get([KC, D], BF16)
            WCO = al4.get([4, D], BF16)
            WMO = al4.get([4, D], BF16)
            assert al4.off <= ARENA_BYTES - 32768, al4.off
            bWGA, bWGB, bWCO, bWMO, bWO = Buf(), Buf(), Buf(), Buf(), Buf()
            wload(WGA, win_v[:, :, 2208:3232], bWGA)
            wload(WCO, kview(w_conv_out[l]), bWCO)
            wload(WMO, kview(w_mla_out[l]), bWMO)
            wload(WO, kview(w_o[l]), bWO)
            cw = 50 + l * 12
            for t in range(NT):
                XNT, bXNT = XNT3[t % 2], bXNT3[t % 2]
                if t == 0:
                    emit_xn(0, l * 8, XNT3[0], bXNT3[0], SQ, bSQ, RS, bRS, 7)
                for c in range(4):
                    if c == 2 and t + 1 < NT:
                        emit_xn(t + 1, l * 8, XNT3[(t + 1) % 2], bXNT3[(t + 1) % 2], SQ, bSQ, RS, bRS, 7)
                    pb = 3 * (c % 2)
                    bC, bU, bB = pb, pb + 1, pb + 2
                    for (bk, col0) in ((bC, 512), (bU, 1024), (bB, 0)):
                        for k in range(KC):
                            mm(PS[bk][:], WC[:, k, col0 + c * 128: col0 + (c + 1) * 128], XNT[:, k, :],
                               k == 0, k == KC - 1, reads=[bWC, bXNT[k]], writes=[bPS[bk]])
                    cp("act", CSB, PS[bC][:], reads=[bPS[bC]], writes=[bCSB])
                    tt("dve", CU[:, c, 2:TT + 2], CSB, PS[bU][:], ALU.mult, reads=[bCSB, bPS[bU], bCU[c]], writes=[bCU[c]])
                    ts("dve", T1, CU[:, c, 0:TT], vcol(cw + 0 * 4 + c), ALU.mult, reads=[bCU[c], bCONST], writes=[bT1])
                    stt("dve", T2, CU[:, c, 1:TT + 1], vcol(cw + 1 * 4 + c), T1, ALU.mult, ALU.add,
                        reads=[bCU[c], bT1, bCONST], writes=[bT2])
                    stt("dve", T1, CU[:, c, 2:TT + 2], vcol(cw + 2 * 4 + c), T2, ALU.mult, ALU.add,
                        reads=[bCU[c], bT2, bCONST], writes=[bT1])
                    tt("dve", CONVIN[:, c, tsl(t)], T1, PS[bB][:], ALU.mult, reads=[bT1, bPS[bB]],
                       writes=[bCONVIN[c][t]])
                    cp("act", CU[:, c, 0:2], CU[:, c, TT:TT + 2], reads=[bCU[c]], writes=[bCU[c]])

            sch.barrier()
            WGB = view(c_off, [KC, 1024], BF16)
            wload(WGB, win_v[:, :, 3232:4256], bWGB)
            al = Alloc()
            XNTS = [al.get([KC, TT], BF16) for _ in range(2)]
            MERGED = al.get([KC, TT], BF16)
            SG = [al.get([TT], F32) for _ in range(2)]
            SQ = [al.get([TT], BF16) for _ in range(2)]
            RS = al.get([TT], F32)
            assert al.off <= p3_end, al.off
            bXNTS = [[Buf() for _ in range(KC)] for _ in range(2)]
            bMERGED = [Buf() for _ in range(KC)]
            bSG = [Buf(), Buf()]
            bSQ = [Buf(), Buf()]
            bRS = Buf()
            emit_xn(0, l * 8, XNTS[0], bXNTS[0], SQ, bSQ, RS, bRS, 7)
            for t in range(NT):
                XNT, bXNT = XNTS[t % 2], bXNTS[t % 2]
                for j in range(KC):
                    if j == 4 and t + 1 < NT:
                        emit_xn(t + 1, l * 8, XNTS[(t + 1) % 2], bXNTS[(t + 1) % 2], SQ, bSQ, RS, bRS, 7)
                    pb = 0 if j % 2 == 0 else 3
                    b_gc, b_gm, b_yc = pb, pb + 1, pb + 2
                    b_ym = 6
                    js = slice(j * 128, (j + 1) * 128)
                    for k in range(KC):
                        mm(PS[b_gc][:], WGA[:, k, j * 128:(j + 1) * 128], XNT[:, k, :], k == 0, k == KC - 1,
                           reads=[bWGA, bXNT[k]], writes=[bPS[b_gc]])
                    for k in range(KC):
                        mm(PS[b_gm][:], WGB[:, k, j * 128:(j + 1) * 128], XNT[:, k, :], k == 0, k == KC - 1,
                           reads=[bWGB, bXNT[k]], writes=[bPS[b_gm]])
                    for k in range(4):
                        mm(PS[b_yc][:], WCO[:, k, js], CONVIN[:, k, tsl(t)], k == 0, k == 3,
                           reads=[bWCO, bCONVIN[k][t]], writes=[bPS[b_yc]])
                    for k in range(4):
                        mm(PS[b_ym][:], WMO[:, k, js], ATT[:, k, tsl(t)], k == 0, k == 3,
                           reads=[bWMO, bATT[2 * k][t], bATT[2 * k + 1][t]], writes=[bPS[b_ym]])
                    act(SG[0], PS[b_gc][:], AF.Sigmoid, reads=[bPS[b_gc]], writes=[bSG[0]])
                    act(SG[1], PS[b_gm][:], AF.Sigmoid, reads=[bPS[b_gm]], writes=[bSG[1]])
                    tt("dve", SG[0], SG[0], PS[b_yc][:], ALU.mult, reads=[bSG[0], bPS[b_yc]], writes=[bSG[0]])
                    tt("dve", SG[1], SG[1], PS[b_ym][:], ALU.mult, reads=[bSG[1], bPS[b_ym]], writes=[bSG[1]])
                    tt("dve", MERGED[:, j, :], SG[0], SG[1], ALU.add, reads=[bSG[0], bSG[1]], writes=[bMERGED[j]])
                for j in range(KC):
                    bk = 0 if j % 2 == 0 else 3
                    for k in range(KC):
                        mm(PS[bk][:], WO[:, k, j * 128:(j + 1) * 128], MERGED[:, k, :], k == 0, k == KC - 1,
                           reads=[bWO, bMERGED[k]], writes=[bPS[bk]])
                    tt("dve", XRES[:, j, tsl(t)], XRES[:, j, tsl(t)], PS[bk][:], ALU.add,
                       reads=[bX[j][t], bPS[bk]], writes=[bX[j][t]])

        def ffn(l):
            moe = (l % 2 == 1)
            i = l // 2
            sch.barrier()
            if (l + 1) in layers and not moe and do_mixer:
                w1_load(l + 1)
            al = Alloc()
            HN = al.get([KC, S], BF16)
            HB = [al.get([max(GSZ), S], BF16) for _ in range(2)]
            WGU = [(al.get([KC, 128], BF16), al.get([KC, 128], BF16)) for _ in range(3)]
            WDS = [al.get([D], BF16) for _ in range(8)]
            SG = [al.get([TT], F32) for _ in range(2)]
            _tf = al.get([TT], F32)
            TF = [_tf, _tf]
            SQ = [al.get([TT], BF16) for _ in range(2)]
            RS = al.get([TT], F32)
            if moe:
                CBC = al.get([S], F32)
                COMBT = al.get([S], F32)
                ROUT = al.get([KC, NE], F32)
                GR = al.get([KC, NE], F32)
                LG = al.get([8, 4 * NE], F32)
                SM = al.get([8, 8], F32)
            assert al.off <= ARENA_BYTES, al.off
            bHN = [[Buf() for _ in range(NT)] for _ in range(KC)]
            bHB = [[[Buf() for _ in range(NT)] for _ in range(max(GSZ))] for _ in range(2)]
            bWGU = [Buf() for _ in range(3)]
            bWDS = [Buf() for _ in range(8)]
            _btf = Buf()
            bSG, bTF, bSQ = [Buf(), Buf()], [_btf, _btf], [Buf(), Buf()]
            bRS, bCBC, bCOMBT, bGR, bLG = Buf(), [Buf() for _ in range(NT)], [Buf() for _ in range(NT)], Buf(), Buf()

            gcol = 16 + l * 8
            if moe:
                sch.dma("sp", ROUT, router[i].rearrange("(c p) e -> p c e", p=128), writes=[bGR])
                tt("dve", GR, ROUT, VECS[:, gcol:gcol + 8].unsqueeze(2).to_broadcast([128, KC, NE]), ALU.mult,
                   reads=[bGR, bCONST], writes=[bGR])
            for t in range(NT):
                rms_scale([XRES[:, c, tsl(t)] for c in range(KC)], [bX[c][t] for c in range(KC)], D,
                          SQ, bSQ, RS, bRS, 7)
                for c in range(KC):
                    stt("dve", HN[:, c, tsl(t)], XRES[:, c, tsl(t)], vcol(gcol + c), RS, ALU.mult, ALU.mult,
                        reads=[bX[c][t], bRS, bCONST], writes=[bHN[c][t]])
                if moe:
                    pb = 4 + (t % 2)
                    for bi in range(4):
                        tok = slice(t * TT + bi * 128, t * TT + (bi + 1) * 128)
                        for k in range(KC):
                            mm(PS[pb][:, bi * NE:(bi + 1) * NE], XRES[:, k, tok], GR[:, k, :], k == 0, k == KC - 1,
                               reads=[bX[k][t], bGR], writes=[bPS[pb]])
                        mm(PS[pb][:, 64 + bi:65 + bi], RS[0:1, bi * 128:(bi + 1) * 128], ONESF[0:1, 0:1], True, True,
                           reads=[bRS, bCONST], writes=[bPS[pb]])
                    B4 = [4, NE]

                    def v3(i):
                        return LG[:, i, :].rearrange("p (b e) -> p b e", e=NE)

                    def bc(ap2):
                        return ap2.unsqueeze(2).to_broadcast([128, 4, NE])
                    lg, lg2, e1, e2, cmb = v3(0), v3(1), v3(2), v3(3), v3(4)
                    rstd, m1, m2, dm, w1, w2 = (SM[:, 0, 0:4], SM[:, 1, 0:4], SM[:, 2, 0:4], SM[:, 3, 0:4],
                                                SM[:, 4, 0:4], SM[:, 5, 0:4])
                    cp("dve", rstd, PS[pb][:, 64:68], reads=[bPS[pb]], writes=[bLG])
                    tt("dve", lg, PS[pb][:, 0:4 * NE].rearrange("p (b e) -> p b e", e=NE), bc(rstd), ALU.mult,
                       reads=[bPS[pb], bLG], writes=[bLG])
                    sch.op("dve", lambda h, o=m1, i_=lg: h.reduce_max(out=o, in_=i_, axis=AX.X), reads=[bLG], writes=[bLG])
                    tt("dve", e1, lg, bc(m1), ALU.is_ge, reads=[bLG], writes=[bLG])
                    stt("dve", lg2, e1, -1e30, lg, ALU.mult, ALU.add, reads=[bLG], writes=[bLG])
                    sch.op("dve", lambda h, o=m2, i_=lg2: h.reduce_max(out=o, in_=i_, axis=AX.X), reads=[bLG], writes=[bLG])
                    tt("dve", e2, lg2, bc(m2), ALU.is_ge, reads=[bLG], writes=[bLG])
                    tt("dve", dm, m2, m1, ALU.subtract, reads=[bLG], writes=[bLG])
                    act(w2, dm, AF.Sigmoid, reads=[bLG], writes=[bLG])
                    act(w1, dm, AF.Sigmoid, reads=[bLG], writes=[bLG], scale=-1.0)
                    tt("dve", cmb, e1, bc(w1), ALU.mult, reads=[bLG], writes=[bLG])
                    tt("dve", e2, e2, bc(w2), ALU.mult, reads=[bLG], writes=[bLG])
                    tt("dve", cmb, cmb, e2, ALU.add, reads=[bLG], writes=[bLG])
                    for bi in range(4):
                        mm(PS[6][0:NE, bi * 128:(bi + 1) * 128], LG[:, 4, bi * NE:(bi + 1) * NE], IDENTF, True, True,
                           reads=[bLG, bCONST], writes=[bPS[6]])
                    cp("act", COMBT[0:NE, tsl(t)], PS[6][0:NE, :], reads=[bPS[6]], writes=[bCOMBT[t]])

            if moe:
                experts = [(w_gate_e[i, e], w_up_e[i, e], w_down_e[i, e], e) for e in range(NE)]
            else:
                experts = [(w_gate[i][:, h * DFE:(h + 1) * DFE], w_up[i][:, h * DFE:(h + 1) * DFE],
                            w_down[i][h * DFE:(h + 1) * DFE, :], None) for h in range(2)]
            gi = 0
            wi = 0
            di = 0
            ev = 0
            for (wg_d, wu_d, wd_d, e) in experts:
                wg_v = kview(wg_d)
                wu_v = kview(wu_d)
                if moe:
                    for t in range(NT):
                        mm(PS[6][:], SELF[0:NE, e, :], COMBT[0:NE, tsl(t)], True, True,
                           reads=[bCOMBT[t], bCONST], writes=[bPS[6]])
                        cp("act", CBC[:, tsl(t)], PS[6][:], reads=[bPS[6]], writes=[bCBC[t]])
                c0 = 0
                for G in GSZ:
                    hb = gi % 2
                    gi += 1
                    dslots = []
                    for cc in range(G):
                        f = c0 + cc
                        ws = wi % 3
                        wi += 1
                        ds_ = di % 8
                        di += 1
                        dslots.append(ds_)
                        wload(WGU[ws][0], wg_v[:, :, f * 128:(f + 1) * 128], bWGU[ws])
                        wload(WGU[ws][1], wu_v[:, :, f * 128:(f + 1) * 128], bWGU[ws])
                        wload(WDS[ds_], wd_d[f * 128:(f + 1) * 128, :], bWDS[ds_])
                        for t in range(NT):
                            pg = (ev % 2) * 2
                            pu = pg + 1
                            sg = ev % 2
                            ev += 1
                            for k in range(KC):
                                mm(PS[pg][:], WGU[ws][0][:, k, :], HN[:, k, tsl(t)], k == 0, k == KC - 1,
                                   reads=[bWGU[ws], bHN[k][t]], writes=[bPS[pg]])
                            for k in range(KC):
                                mm(PS[pu][:], WGU[ws][1][:, k, :], HN[:, k, tsl(t)], k == 0, k == KC - 1,
                                   reads=[bWGU[ws], bHN[k][t]], writes=[bPS[pu]])
                            act(SG[sg], PS[pg][:], AF.Silu, reads=[bPS[pg]], writes=[bSG[sg]])
                            if moe:
                                tt("dve", TF[sg], SG[sg], PS[pu][:], ALU.mult, reads=[bSG[sg], bPS[pu]], writes=[bTF[sg]])
                                tt("dve", HB[hb][:, cc, tsl(t)], TF[sg], CBC[:, tsl(t)], ALU.mult,
                                   reads=[bTF[sg], bCBC[t]], writes=[bHB[hb][cc][t]])
                            else:
                                tt("dve", HB[hb][:, cc, tsl(t)], SG[sg], PS[pu][:], ALU.mult,
                                   reads=[bSG[sg], bPS[pu]], writes=[bHB[hb][cc][t]])
                    dj = 0
                    for j in range(KC):
                        for t in range(NT):
                            pd = 4 + (dj % 2)
                            dj += 1
                            for cc in range(G):
                                mm(PS[pd][:], WDS[dslots[cc]][:, j * 128:(j + 1) * 128], HB[hb][:, cc, tsl(t)],
                                   cc == 0, cc == G - 1, reads=[bWDS[dslots[cc]], bHB[hb][cc][t]], writes=[bPS[pd]])
                            tt("dve", XRES[:, j, tsl(t)], XRES[:, j, tsl(t)], PS[pd][:], ALU.add,
                               reads=[bX[j][t], bPS[pd]], writes=[bX[j][t]])
                    c0 += G

        if not do_mixer:
            load_x_tiles(1, NT)
            x_loaded[0] = True
        for l in layers:
            if do_mixer:
                mixer(l)
            if do_ffn:
                ffn(l)

        sch.barrier()
        al = Alloc()
        OUTS = [al.get([KC, TT], F32) for _ in range(2)]
        SQ = [al.get([TT], BF16) for _ in range(2)]
        RS = al.get([TT], F32)
        bOUT = [[Buf() for _ in range(KC)] for _ in range(2)]
        bSQ = [Buf(), Buf()]
        bRS = Buf()
        for t in range(NT):
            o = t % 2
            if last:
                rms_scale([XRES[:, c, tsl(t)] for c in range(KC)], [bX[c][t] for c in range(KC)], D,
                          SQ, bSQ, RS, bRS, 7)
                for c in range(KC):
                    stt("dve", OUTS[o][:, c, :], XRES[:, c, tsl(t)], vcol(32 + c), RS, ALU.mult, ALU.mult,
                        reads=[bX[c][t], bRS, bCONST], writes=[bOUT[o][c]])
                    sch.dma("sp", outT[c * 128:(c + 1) * 128, tsl(t)], OUTS[o][:, c, :], reads=[bOUT[o][c]])
            else:
                for c in range(KC):
                    sch.dma("sp", outT[c * 128:(c + 1) * 128, tsl(t)], XRES[:, c, tsl(t)], reads=[bX[c][t]])
        sch.final_wait("sp")
        sch.finalize()

        @block.tensor
        def _(h):
            sch.emit("pe", h)

        @block.scalar
        def _(h):
            sch.emit("act", h)

        @block.vector
        def _(h):
            sch.emit("dve", h)

        @block.gpsimd
        def _(h):
            sch.emit("pool", h)

        @block.sync
        def _(h):
            sch.emit("sp", h)

    return nc


def _host_consts():
    cf = np.zeros((128, 128 + 1024), np.float32)
    cf[:, 0:128] = np.eye(128, dtype=np.float32)
    for e in range(8):
        cf[e, 128 + e * 128: 128 + (e + 1) * 128] = 1.0
    cb = np.zeros((128, 256), np.float32)
    cb[:, 0:128] = np.eye(128, dtype=np.float32)
    k = np.arange(128)[:, None]
    q = np.arange(128)[None, :]
    cb[:, 128:256] = np.where(q >= k, 0.0, -30000.0).astype(np.float32)
    return cf, cb


def _shared_inputs(inp):
    f = lambda a: np.ascontiguousarray(np.asarray(a, dtype=np.float32))
    vecs = np.zeros((128, NV), np.float32)

    def put(col, v):
        v = np.asarray(v, np.float32).reshape(-1, 128)
        vecs[:, col:col + v.shape[0]] = v.T

    for l in range(2):
        put(0 + l * 8, inp["attn_norm"][l])
        put(16 + l * 8, inp["ffn_norm"][l])
        put(40 + l * 3, inp["q_norm"][l])
        put(46 + l * 2, inp["kv_norm"][l])
        for k in range(3):
            put(50 + l * 12 + k * 4, inp["conv_w"][l, k])
    put(32, inp["final_norm"])
    inv_freq = (10000.0 ** (-np.arange(0, 32, 2, dtype=np.float32) / np.float32(32))).astype(np.float32)
    vecs[64:96, 74] = np.concatenate([inv_freq, inv_freq])
    cf, cb = _host_consts()
    w_uq = f(inp["w_uq"])
    w_in = f(inp["w_in"])
    wq4 = w_uq.reshape(2, 384, 8, 96)
    wq_rot = np.concatenate([wq4[..., 80:96], wq4[..., 64:80]], axis=-1).reshape(2, 384, 256)
    wkr = w_in[:, :, 2176:2208]
    wkr_rot = np.concatenate([w_in[:, :, 2192:2208], w_in[:, :, 2176:2192]], axis=-1)
    sh = {
        "vecs": vecs, "cf": cf, "cb": cb, "w_in": w_in, "w_conv_out": f(inp["w_conv_out"]),
        "w_uq": w_uq, "wq_rot": f(wq_rot), "wkr": f(wkr), "wkr_rot": f(wkr_rot),
        "w_ukv": f(inp["w_ukv"]), "w_mla_out": f(inp["w_mla_out"]), "w_o": f(inp["w_o"]),
        "w_gate": f(inp["w_gate"]), "w_up": f(inp["w_up"]), "w_down": f(inp["w_down"]),
        "router": f(inp["router"]), "w_gate_e": f(inp["w_gate_e"]), "w_up_e": f(inp["w_up_e"]),
        "w_down_e": f(inp["w_down_e"]),
    }
    return sh


_NC_CACHE = {}


def run_layers(inp, xT_list, layers, last):
    key = (tuple(layers), last)
    if key not in _NC_CACHE:
        _NC_CACHE[key] = build_nc(layers, last=last)
    nc = _NC_CACHE[key]
    sh = _shared_inputs(inp)
    pos = np.asarray(inp["positions"]).astype(np.int32)
    in_maps = []
    for b in range(8):
        m = dict(sh)
        m["xT"] = np.ascontiguousarray(xT_list[b])
        m["posrep"] = np.ascontiguousarray(np.broadcast_to(pos[b][None, :], (32, S)))
        in_maps.append(m)
    res = run_bass_kernel_spmd(nc, in_maps, core_ids=list(range(8)))
    return [r["outT"] for r in res.results]


def kernel(**inputs):
    x = np.asarray(inputs["x"], dtype=np.float32)
    xT = [np.ascontiguousarray(x[b].T) for b in range(8)]
    outs = run_layers(inputs, xT, [0, 1], True)
    return np.stack([np.ascontiguousarray(o.T) for o in outs], axis=0).astype(np.float32)
```

```python
import math
import numpy as np
import concourse.bass as bass
import concourse.mybir as mybir
from concourse.bass_utils import run_bass_kernel_spmd

F32 = mybir.dt.float32
BF16 = mybir.dt.bfloat16
I32 = mybir.dt.int32
ALU = mybir.AluOpType
AF = mybir.ActivationFunctionType
AX = mybir.AxisListType

D = 1024
S = 2048
KC = 8
TT = 512
NT = 4
NH = 8
DIN = 4256
DFF = 2816
DFE = 1408
NE = 8
EPS = 1e-6
NV = 80
SCALE = 1.0 / math.sqrt(96.0)
ARENA_BYTES = 140800
GSZ = (6, 5)
SAME_ENGINE_SYNC = True


class Tok:
    __slots__ = ("eng", "idx", "sem", "val")

    def __init__(self, eng=None, idx=None, sem=None, val=None):
        self.eng, self.idx, self.sem, self.val = eng, idx, sem, val


class Buf:
    __slots__ = ("w", "r", "name")

    def __init__(self, name=""):
        self.w = None
        self.r = []
        self.name = name


class Eng:
    def __init__(self, name, sem, dma_sems):
        self.name = name
        self.sem = sem
        self.ops = []
        self.waited = {}
        self.last_real = -1
        self.dma_sems = dma_sems
        self.dma_cnt = [0] * len(dma_sems)
        self.dma_rr = 0


class Sched:
    def __init__(self, sems):
        it = iter(sems)
        self.e = {}
        for name, nd in (("pe", 0), ("act", 0), ("dve", 0), ("pool", 24), ("sp", 12)):
            s = next(it)
            self.e[name] = Eng(name, s, [next(it) for _ in range(nd)])

    def _waits(self, e, toks):
        best = {}
        for t in toks:
            if t is None:
                continue
            if t.eng is None:
                k = ("d", id(t.sem))
                if e.waited.get(k, 0) >= t.val:
                    continue
                if k not in best or best[k].val < t.val:
                    best[k] = t
            else:
                if t.eng is e and (e.name == "pe" or not SAME_ENGINE_SYNC):
                    continue
                k = ("e", t.eng.name)
                if e.waited.get(k, -1) >= t.idx:
                    continue
                if k not in best or best[k].idx < t.idx:
                    best[k] = t
        out = []
        for k, t in best.items():
            if t.eng is None:
                e.waited[k] = t.val
            else:
                e.waited[k] = t.idx
                t.eng.ops[t.idx][2] = True
            out.append(t)
        return out

    @staticmethod
    def _deps(reads, writes):
        toks = []
        for b in reads:
            toks.append(b.w)
        for b in writes:
            toks.append(b.w)
            toks.extend(b.r)
        return toks

    @staticmethod
    def _mark(tok, reads, writes):
        for b in reads:
            b.r.append(tok)
        for b in writes:
            b.w = tok
            b.r = []

    def op(self, eng, fn, reads=(), writes=()):
        e = self.e[eng]
        waits = self._waits(e, self._deps(reads, writes))
        idx = len(e.ops)
        e.ops.append([waits, fn, False])
        e.last_real = idx
        tok = Tok(eng=e, idx=idx)
        self._mark(tok, reads, writes)
        return tok

    def dma(self, eng, out, in_, reads=(), writes=()):
        e = self.e[eng]
        slot = e.dma_rr % len(e.dma_sems)
        e.dma_rr += 1
        sem = e.dma_sems[slot]
        toks = self._deps(reads, writes)
        if e.dma_cnt[slot] > 0:
            toks.append(Tok(sem=sem, val=e.dma_cnt[slot]))
        waits = self._waits(e, toks)
        e.dma_cnt[slot] += 16
        tok = Tok(sem=sem, val=e.dma_cnt[slot])
        e.ops.append([waits, (lambda h, o=out, i=in_: h.dma_start(out=o, in_=i)), (sem, 16)])
        self._mark(tok, reads, writes)
        return tok

    def _all_toks(self, skip=None):
        toks = []
        for f in self.e.values():
            if f is not skip and f.last_real >= 0:
                toks.append(Tok(eng=f, idx=f.last_real))
            for s, c in zip(f.dma_sems, f.dma_cnt):
                if c > 0:
                    toks.append(Tok(sem=s, val=c))
        return toks

    def barrier(self):
        for e in self.e.values():
            own = (e.name == "pe" or not SAME_ENGINE_SYNC)
            waits = self._waits(e, self._all_toks(skip=e if own else None))
            if waits:
                e.ops.append([waits, None, False])

    def final_wait(self, eng):
        e = self.e[eng]
        waits = self._waits(e, self._all_toks(skip=e))
        if waits:
            e.ops.append([waits, None, False])

    def finalize(self):
        self.cnt = {}
        for name, e in self.e.items():
            c = 0
            arr = []
            for (_, fn, sig) in e.ops:
                if sig is True:
                    c += 1
                arr.append(c)
            self.cnt[name] = arr

    def emit(self, name, h):
        for waits, fn, sig in self.e[name].ops:
            for t in waits:
                if t.eng is None:
                    h.wait_ge(t.sem, t.val)
                else:
                    h.wait_ge(t.eng.sem, self.cnt[t.eng.name][t.idx])
            if fn is not None:
                ins = fn(h)
                if sig is True:
                    ins.then_inc(self.e[name].sem, 1)
                elif sig:
                    ins.then_inc(sig[0], sig[1])


def build_nc(layers, last=True, do_mixer=True, do_ffn=True):
    nc = bass.Bass("TRN2", target_bir_lowering=False)

    def din(name, shape, dt=F32):
        return nc.dram_tensor(name, list(shape), dt, kind="ExternalInput").ap()

    xT = din("xT", [D, S])
    posrep = din("posrep", [32, S], I32)
    vecs_d = din("vecs", [128, NV])
    cf_d = din("cf", [128, 128 + 1024])
    cb_d = din("cb", [128, 256])
    w_in = din("w_in", [2, D, DIN])
    w_conv_out = din("w_conv_out", [2, 512, D])
    w_uq = din("w_uq", [2, 384, 768])
    wq_rot = din("wq_rot", [2, 384, 256])
    wkr = din("wkr", [2, D, 32])
    wkr_rot = din("wkr_rot", [2, D, 32])
    w_ukv = din("w_ukv", [2, 256, 1024])
    w_mla_out = din("w_mla_out", [2, 512, D])
    w_o = din("w_o", [2, D, D])
    w_gate = din("w_gate", [1, D, DFF])
    w_up = din("w_up", [1, D, DFF])
    w_down = din("w_down", [1, DFF, D])
    router = din("router", [1, D, NE])
    w_gate_e = din("w_gate_e", [1, NE, D, DFE])
    w_up_e = din("w_up_e", [1, NE, D, DFE])
    w_down_e = din("w_down_e", [1, NE, DFE, D])
    outT = nc.dram_tensor("outT", [D, S], F32, kind="ExternalOutput").ap()

    import contextlib
    es = contextlib.ExitStack()
    with es:
        XRES = es.enter_context(nc.sbuf_tensor("XRES", [128, KC, S], F32))
        VECS = es.enter_context(nc.sbuf_tensor("VECS", [128, NV], F32))
        CF = es.enter_context(nc.sbuf_tensor("CF", [128, 128 + 1024], F32))
        CB = es.enter_context(nc.sbuf_tensor("CB", [128, 256], BF16))
        ONESB = es.enter_context(nc.sbuf_tensor("ONESB", [128, 128], BF16))
        ONESF = es.enter_context(nc.sbuf_tensor("ONESF", [128, 128], F32))
        ARENA = es.enter_context(nc.sbuf_tensor("ARENA", [128, ARENA_BYTES // 2], BF16))
        PS = [es.enter_context(nc.psum_tensor(f"PS{i}", [128, 512], F32)) for i in range(8)]
        sems = [es.enter_context(nc.semaphore(f"sem{i}")) for i in range(5 + 24 + 12)]
        block = es.enter_context(nc.Block())

        sch = Sched(sems)
        IDENTF = CF[:, 0:128]
        SELF = CF[:, 128:128 + 1024].rearrange("p (e m) -> p e m", e=8)
        IDENTB = CB[:, 0:128]
        TRIB = CB[:, 128:256]

        def view(off, shape, dt):
            n = int(np.prod(shape))
            esz = 4 if dt in (F32, I32) else 2
            assert off % 4 == 0 and off + n * esz <= ARENA_BYTES, (off, shape, ARENA_BYTES)
            a = ARENA[:, off // 2: off // 2 + n * esz // 2]
            if dt != BF16:
                a = a.bitcast(dt)
            if len(shape) == 2:
                a = a.rearrange("p (a b) -> p a b", a=shape[0])
            elif len(shape) == 3:
                a = a.rearrange("p (a b c) -> p a b c", a=shape[0], b=shape[1])
            return a

        class Alloc:
            def __init__(self, start=0):
                self.off = start

            def get(self, shape, dt):
                n = int(np.prod(shape))
                esz = 4 if dt in (F32, I32) else 2
                v = view(self.off, shape, dt)
                self.off += (n * esz + 63) // 64 * 64
                return v

        def mm(out, lhsT, rhs, start, stop, reads, writes):
            sch.op("pe", lambda h: h.matmul(out, lhsT=lhsT, rhs=rhs, start=start, stop=stop,
                                            skip_group_check=True),
                   reads=reads, writes=writes)

        def act(out, in_, func, reads, writes, scale=1.0, bias=0.0):
            sch.op("act", lambda h: h.activation(out=out, in_=in_, func=func, bias=bias, scale=scale),
                   reads=reads, writes=writes)

        def tt(eng, out, in0, in1, op, reads, writes):
            sch.op(eng, lambda h: h.tensor_tensor(out=out, in0=in0, in1=in1, op=op),
                   reads=reads, writes=writes)

        def stt(eng, out, in0, scalar, in1, op0, op1, reads, writes):
            sch.op(eng, lambda h: h.scalar_tensor_tensor(out=out, in0=in0, scalar=scalar, in1=in1,
                                                          op0=op0, op1=op1),
                   reads=reads, writes=writes)

        def ts(eng, out, in0, s1, op0, reads, writes, s2=None, op1=None):
            if op1 is None:
                sch.op(eng, lambda h: h.tensor_scalar(out=out, in0=in0, scalar1=s1, scalar2=None, op0=op0),
                       reads=reads, writes=writes)
            else:
                sch.op(eng, lambda h: h.tensor_scalar(out=out, in0=in0, scalar1=s1, scalar2=s2,
                                                      op0=op0, op1=op1),
                       reads=reads, writes=writes)

        def cp(eng, out, in_, reads, writes):
            if eng == "act":
                sch.op("act", lambda h: h.copy(out=out, in_=in_), reads=reads, writes=writes)
            else:
                sch.op(eng, lambda h: h.tensor_copy(out=out, in_=in_), reads=reads, writes=writes)

        def memset(eng, ap, val, writes):
            sch.op(eng, lambda h: h.memset(ap, val), reads=(), writes=writes)

        bX = [[Buf(f"x{c}_{t}") for t in range(NT)] for c in range(KC)]
        bPS = [Buf(f"ps{i}") for i in range(8)]
        bCONST = Buf("const")

        def tsl(t):
            return slice(t * TT, (t + 1) * TT)

        sch.dma("sp", VECS[:], vecs_d[:], writes=[bCONST])
        sch.dma("sp", CF[:], cf_d[:], writes=[bCONST])
        sch.dma("pool", CB[:], cb_d[:], writes=[bCONST])
        memset("dve", ONESB[:], 1.0, [bCONST])
        memset("dve", ONESF[:], 1.0, [bCONST])
        xT_v = xT.rearrange("(c p) s -> p c s", p=128)
        x_loaded = [False]

        def load_x_tiles(t0, t1):
            for t in range(t0, t1):
                sch.dma("sp", XRES[:, :, tsl(t)], xT_v[:, :, tsl(t)], writes=[bX[c][t] for c in range(KC)],
                        reads=([bX[0][t - 1]] if t > 0 else []))

        load_x_tiles(0, 1)

        def vcol(i):
            return VECS[:, i:i + 1]

        def rms_scale(srcs, src_bufs, nfeat, SQ, bSQ, RS, bRS, bank):
            n = len(srcs)
            for i, (s, sb) in enumerate(zip(srcs, src_bufs)):
                q = i % 2
                act(SQ[q], s, AF.Square, reads=[sb], writes=[bSQ[q]])
                mm(PS[bank][:], ONESB[:], SQ[q], i == 0, i == n - 1,
                   reads=[bSQ[q], bCONST], writes=[bPS[bank]])
            act(RS, PS[bank][:], AF.Ln, reads=[bPS[bank]], writes=[bRS], scale=1.0 / nfeat, bias=EPS)
            act(RS, RS, AF.Exp, reads=[bRS], writes=[bRS], scale=-0.5)

        def emit_xn(t, gcol0, XNT, bXNT, SQ, bSQ, RS, bRS, bank):
            rms_scale([XRES[:, c, tsl(t)] for c in range(KC)], [bX[c][t] for c in range(KC)], D,
                      SQ, bSQ, RS, bRS, bank)
            for c in range(KC):
                stt("dve", XNT[:, c, :], XRES[:, c, tsl(t)], vcol(gcol0 + c), RS, ALU.mult, ALU.mult,
                    reads=[bX[c][t], bRS, bCONST], writes=[bXNT[c]])

        def wload(dst, src, buf):
            sch.dma("pool", dst, src, writes=[buf])

        def kview(w2d):
            return w2d.rearrange("(c p) n -> p c n", p=128)

        W1_OFF = ARENA_BYTES - 17920
        w1state = {}

        def w1_load(l):
            al = Alloc(W1_OFF)
            WINB = al.get([KC, 672], BF16)
            WKR = al.get([KC, 96], BF16)
            WKRROT = al.get([KC, 96], BF16)
            WKVK = al.get([2, 512], BF16)
            WKVV = al.get([2, 512], BF16)
            assert al.off <= ARENA_BYTES
            bWINB, bWKR, bWKRROT, bWKVK, bWKVV = Buf(), Buf(), Buf(), Buf(), Buf()
            bWKZ = Buf()
            win_v = kview(w_in[l])
            memset("dve", WKR[:, :, 0:64], 0.0, [bWKZ])
            memset("dve", WKRROT[:, :, 0:64], 0.0, [bWKZ])
            wload(WKR[:, :, 64:96], kview(wkr[l]), bWKR)
            wload(WKRROT[:, :, 64:96], kview(wkr_rot[l]), bWKRROT)
            wload(WINB, win_v[:, :, 1536:2208], bWINB)
            ukv = w_ukv[l].rearrange("(c p) (h e) -> p c h e", p=128, e=128)
            for c in range(2):
                wload(WKVK[:, c, :].rearrange("p (h e) -> p h e", e=64), ukv[:, c, :, 0:64], bWKVK)
                wload(WKVV[:, c, :].rearrange("p (h e) -> p h e", e=64), ukv[:, c, :, 64:128], bWKVV)
            w1state[l] = (WINB, WKR, WKRROT, WKVK, WKVV, bWINB, bWKR, bWKRROT, bWKVK, bWKVV, bWKZ)

        def mixer(l):
            sch.barrier()
            al = Alloc()
            KT = al.get([NH, S], BF16)
            V = al.get([16, NH, 65], BF16)
            c_off = al.off
            CQN = al.get([3, S], BF16)
            WQ = al.get([3, 768], BF16)
            WQROT = al.get([3, NH, 96], BF16)
            COS = al.get([S], F32)
            SIN = al.get([S], F32)
            p12 = al.off
            XNTS = [al.get([KC, TT], BF16) for _ in range(2)]
            SQ = [al.get([TT], BF16) for _ in range(2)]
            RS = al.get([TT], F32)
            RS2 = al.get([TT], F32)
            SQ2 = SQ
            CF32 = al.get([5, TT], F32)
            CKVN = al.get([2, TT], BF16)
            T1 = CF32[:, 3, :]
            T2 = CF32[:, 0, :]
            RT0 = CF32[:, 1, :]
            RT1 = CF32[:, 2, :]
            assert al.off <= W1_OFF, al.off

            bKT = [[Buf() for _ in range(NT)] for _ in range(NH)]
            bV = [Buf() for _ in range(16)]
            bCQN = [[Buf() for _ in range(NT)] for _ in range(3)]
            bWQ, bWQROT = Buf(), Buf()
            bROPE = [Buf() for _ in range(NT)]
            bXNTS = [[Buf() for _ in range(KC)] for _ in range(2)]
            bSQ = [Buf(), Buf()]
            bSQ2 = bSQ
            bRS, bRS2 = Buf(), Buf()
            bCF32 = [Buf() for _ in range(5)]
            bT1, bT2, bRT0, bRT1 = bCF32[3], bCF32[0], bCF32[1], bCF32[2]
            bCKVN = [Buf(), Buf()]

            sch.dma("pool", RT0[slice(64, 96), :], posrep[:, tsl(0)], writes=[bRT0])
            if l not in w1state:
                w1_load(l)
            (WINB, WKR, WKRROT, WKVK, WKVV, bWINB, bWKR, bWKRROT, bWKVK, bWKVV, bWKZ) = w1state[l]
            win_v = kview(w_in[l])
            bWQZ = Buf()
            memset("dve", WQROT[:, :, :, 0:64], 0.0, [bWQZ])
            wload(WQ, kview(w_uq[l]), bWQ)
            for c in range(3):
                wload(WQROT[:, c, :, 64:96], kview(wq_rot[l])[:, c, :].rearrange("p (h e) -> p h e", e=32), bWQROT)

            R = slice(64, 96)

            def rope_pass(t):
                if t > 0:
                    sch.dma("pool", RT0[R, :], posrep[:, tsl(t)], writes=[bRT0])
                ts("dve", RT0[R, :], RT0[R, :], VECS[R, 74:75], ALU.mult, reads=[bRT0, bCONST], writes=[bRT0])
                for tab, shift in ((SIN, 0.0), (COS, math.pi / 2)):
                    tv = tab[R, tsl(t)]
                    ts("dve", tv, RT0[R, :], shift, ALU.add, reads=[bRT0], writes=[bROPE[t]],
                       s2=1.0 / (2 * math.pi), op1=ALU.mult)
                    ts("dve", RT1[R, :], tv, 12582912.0, ALU.add, reads=[bROPE[t]], writes=[bRT1])
                    ts("dve", tv, RT1[R, :], -12582912.0, ALU.add, reads=[bRT1], writes=[bROPE[t]])
                    stt("dve", tv, tv, -2 * math.pi, RT0[R, :], ALU.mult, ALU.add,
                        reads=[bROPE[t], bRT0], writes=[bROPE[t]])
                    ts("dve", tv, tv, shift, ALU.add, reads=[bROPE[t]], writes=[bROPE[t]],
                       s2=3.1415925, op1=ALU.min)
                    ts("dve", tv, tv, -3.1415925, ALU.max, reads=[bROPE[t]], writes=[bROPE[t]])
                    act(tv, tv, AF.Sin, reads=[bROPE[t]], writes=[bROPE[t]])

            memset("dve", V.rearrange("p a h e -> p (a h) e")[:, :, 64:65], 1.0, bV)

            qg = 40 + l * 3
            kg = 46 + l * 2
            rope_pass(0)
            if not x_loaded[0]:
                load_x_tiles(1, NT)
                x_loaded[0] = True
            emit_xn(0, l * 8, XNTS[0], bXNTS[0], SQ2, bSQ2, RS2, bRS2, 7)
            for t in range(NT):
                XNT, bXNT = XNTS[t % 2], bXNTS[t % 2]
                for oc in range(5):
                    bk = oc % 4
                    for k in range(KC):
                        mm(PS[bk][:], WINB[:, k, oc * 128:(oc + 1) * 128], XNT[:, k, :], k == 0, k == KC - 1,
                           reads=[bWINB, bXNT[k]], writes=[bPS[bk]])
                    cp("dve", CF32[:, oc, :], PS[bk][:], reads=[bPS[bk]], writes=[bCF32[oc]])
                if t == 0:
                    ts("dve", WKRROT[:, :, 64:80], WKRROT[:, :, 64:80], -1.0, ALU.mult, reads=[], writes=[bWKRROT])
                for k in range(KC):
                    mm(PS[4][0:96, :], WKR[:, k, :], XNT[:, k, :], k == 0, k == KC - 1,
                       reads=[bWKR, bWKZ, bXNT[k]], writes=[bPS[4]])
                for k in range(KC):
                    mm(PS[5][0:96, :], WKRROT[:, k, :], XNT[:, k, :], k == 0, k == KC - 1,
                       reads=[bWKRROT, bWKZ, bXNT[k]], writes=[bPS[5]])
                if t + 1 < NT:
                    emit_xn(t + 1, l * 8, XNTS[(t + 1) % 2], bXNTS[(t + 1) % 2], SQ2, bSQ2, RS2, bRS2, 7)
                rms_scale([CF32[:, 3 + c, :] for c in range(2)], bCF32[3:5], 256, SQ, bSQ, RS, bRS, 6)
                for c in range(2):
                    stt("dve", CKVN[:, c, :], CF32[:, 3 + c, :], vcol(kg + c), RS, ALU.mult, ALU.mult,
                        reads=[bCF32[3 + c], bRS, bCONST], writes=[bCKVN[c]])
                rms_scale([CF32[:, c, :] for c in range(3)], bCF32[0:3], 384, SQ, bSQ, RS, bRS, 6)
                for c in range(3):
                    stt("dve", CQN[:, c, tsl(t)], CF32[:, c, :], vcol(qg + c), RS, ALU.mult, ALU.mult,
                        reads=[bCF32[c], bRS, bCONST], writes=[bCQN[c][t]])
                tt("dve", T1[R, :], PS[4][R, :], COS[R, tsl(t)], ALU.mult, reads=[bPS[4], bROPE[t]], writes=[bT1])
                tt("dve", T2[R, :], PS[5][R, :], SIN[R, tsl(t)], ALU.mult, reads=[bPS[5], bROPE[t]], writes=[bT2])
                tt("dve", KT[R, 0, tsl(t)], T1[R, :], T2[R, :], ALU.add, reads=[bT1, bT2], writes=[bKT[0][t]])
                for h in range(1, NH):
                    cp("act", KT[R, h, tsl(t)], KT[R, 0, tsl(t)], reads=[bKT[0][t]], writes=[bKT[h][t]])
                for hp in range(4):
                    bk = hp % 4
                    for k in range(2):
                        mm(PS[bk][:], WKVK[:, k, hp * 128:(hp + 1) * 128], CKVN[:, k, :], k == 0, k == 1,
                           reads=[bWKVK, bCKVN[k]], writes=[bPS[bk]])
                    cp("act", KT[0:64, 2 * hp, tsl(t)], PS[bk][0:64, :], reads=[bPS[bk]], writes=[bKT[2 * hp][t]])
                    cp("dve", KT[0:64, 2 * hp + 1, tsl(t)], PS[bk][64:128, :], reads=[bPS[bk]],
                       writes=[bKT[2 * hp + 1][t]])
                for bi in range(4):
                    blk = 4 * t + bi
                    bk = 4 + (bi % 2)
                    for k in range(2):
                        mm(PS[bk][:], CKVN[:, k, bi * 128:(bi + 1) * 128], WKVV[:, k, :], k == 0, k == 1,
                           reads=[bWKVV, bCKVN[k]], writes=[bPS[bk]])
                    cp("act", V[:, blk, :, 0:64], PS[bk][:].rearrange("p (h e) -> p h e", e=64),
                       reads=[bPS[bk]], writes=[bV[blk]])
                if t + 1 < NT:
                    rope_pass(t + 1)
            ts("dve", WQROT[:, :, :, 64:80], WQROT[:, :, :, 64:80], -1.0, ALU.mult, reads=[], writes=[bWQROT])

            sch.barrier()
            al = Alloc(p12)
            QT = [al.get([NH, TT], BF16) for _ in range(2)]
            PT = [al.get([TT], BF16) for _ in range(4)]
            ONUM = [al.get([TT], F32) for _ in range(2)]
            RDEN = [al.get([TT], F32) for _ in range(2)]
            RD = [al.get([TT], BF16) for _ in range(2)]
            T1s = [al.get([TT], F32) for _ in range(2)]
            _t2 = al.get([TT], F32)
            T2s = [_t2, _t2]
            assert al.off <= ARENA_BYTES - 16384, al.off
            ATT = view(ARENA_BYTES - 16384, [4, S], BF16)
            WC = view(c_off, [KC, 1536], BF16)
            bWC = Buf()
            bQT = [[Buf() for _ in range(NH)] for _ in range(2)]
            bPT = [Buf() for _ in range(4)]
            bONUM, bRDEN = [Buf(), Buf()], [Buf(), Buf()]
            bRD = [Buf(), Buf()]
            for q in range(2):
                memset("dve", RD[q], 0.0, [bRD[q]])
            _b2 = Buf()
            bT1s, bT2s = [Buf(), Buf()], [_b2, _b2]
            bATT = [[Buf() for _ in range(NT)] for _ in range(NH)]

            QBUF = {3: 0, 2: 0, 0: 1, 1: 1}

            def qproj(qt, h):
                Q, bQ = QT[QBUF[qt]], bQT[QBUF[qt]]
                bq, br = 6, 7
                z = h % 2
                for k in range(3):
                    mm(PS[bq][0:96, :], WQ[:, k, h * 96:(h + 1) * 96], CQN[:, k, tsl(qt)], k == 0, k == 2,
                       reads=[bWQ, bCQN[k][qt]], writes=[bPS[bq]])
                for k in range(3):
                    mm(PS[br][0:96, :], WQROT[:, k, h, :], CQN[:, k, tsl(qt)], k == 0, k == 2,
                       reads=[bWQROT, bWQZ, bCQN[k][qt]], writes=[bPS[br]])
                cp("dve", Q[0:64, h, :], PS[bq][0:64, :], reads=[bPS[bq]], writes=[bQ[h]])
                tt("dve", T1s[z][R, :], PS[bq][R, :], COS[R, tsl(qt)], ALU.mult, reads=[bPS[bq], bROPE[qt]], writes=[bT1s[z]])
                tt("dve", T2s[z][R, :], PS[br][R, :], SIN[R, tsl(qt)], ALU.mult, reads=[bPS[br], bROPE[qt]], writes=[bT2s[z]])
                tt("dve", Q[R, h, :], T1s[z][R, :], T2s[z][R, :], ALU.add, reads=[bT1s[z], bT2s[z]], writes=[bQ[h]])

            def normalize_a(z, ob):
                cp("dve", ONUM[z][0:64, :], PS[ob][0:64, :], reads=[bPS[ob]], writes=[bONUM[z]])
                act(RDEN[z][64:65, :], PS[ob][64:65, :], AF.Ln, reads=[bPS[ob]], writes=[bRDEN[z]])
                act(RDEN[z][64:65, :], RDEN[z][64:65, :], AF.Exp, reads=[bRDEN[z]], writes=[bRDEN[z]], scale=-1.0)
                cp("dve", RD[z][64:65, :], RDEN[z][64:65, :], reads=[bRDEN[z]], writes=[bRD[z]])
                tt("dve", RD[z][0:1, :], RDEN[z][64:65, :], RD[z][64:65, :], ALU.subtract,
                   reads=[bRDEN[z], bRD[z]], writes=[bRD[z]])

            def normalize_b(z, qt, h):
                mm(PS[7][0:64, :], ONESB[0:65, 0:64], RD[z][0:65, :], True, True,
                   reads=[bRD[z], bCONST], writes=[bPS[7]])
                r0 = (h % 2) * 64
                tt("dve", ATT[r0:r0 + 64, h // 2, tsl(qt)], ONUM[z][0:64, :], PS[7][0:64, :], ALU.mult,
                   reads=[bONUM[z], bPS[7]], writes=[bATT[h][qt]])

            for h in range(NH):
                qproj(3, h)
            for h in range(NH):
                qproj(0, h)
            LA = 3
            NDEF = 8
            cnt = [0, 0]
            NEXTQ = {3: 2, 0: 1}
            for (qa, qb) in ((3, 0), (2, 1)):
                jobs = []
                for h in range(NH):
                    jobs.append((qa, h))
                    jobs.append((qb, h))
                items = [(ji, kc) for ji, (qt, h) in enumerate(jobs) for kc in range(4 * qt + 4)]
                slots = {}
                pending = []

                def SC(i):
                    ji, kc = items[i]
                    qt, h = jobs[ji]
                    Q, bQ = QT[QBUF[qt]], bQT[QBUF[qt]]
                    j = kc - 4 * qt
                    c0 = 128 * j if j > 0 else 0
                    sb = cnt[0] % 4
                    cnt[0] += 1
                    slots[i] = (sb, c0)
                    mm(PS[sb][:, c0:TT], KT[0:96, h, kc * 128:(kc + 1) * 128], Q[0:96, h, c0:TT],
                       True, j < 0, reads=[bKT[h][kc // 4], bQ[h]], writes=[bPS[sb]])
                    if j >= 0:
                        mm(PS[sb][:, c0:c0 + 128], IDENTB, TRIB, False, True, reads=[bCONST], writes=[bPS[sb]])

                def E(i):
                    ji, kc = items[i]
                    qt, h = jobs[ji]
                    nk = 4 * qt + 4
                    sb, c0 = slots.pop(i)
                    z = ji % 2
                    ob = 4 + z
                    p = cnt[1] % 4
                    cnt[1] += 1
                    act(PT[p][:, c0:TT], PS[sb][:, c0:TT], AF.Exp, reads=[bPS[sb]], writes=[bPT[p]], scale=SCALE)
                    mm(PS[ob][0:65, c0:TT], V[:, kc, h, :], PT[p][:, c0:TT], kc == 0, kc == nk - 1,
                       reads=[bV[kc], bPT[p]], writes=[bPS[ob]])
                    if kc == nk - 1:
                        normalize_a(z, ob)
                        if qt in NEXTQ:
                            qproj(NEXTQ[qt], h)
                            if qt == 0 and h == NH - 1:
                                dead = [b for row in bCQN for b in row] + [bWQ, bWQROT] + bROPE
                                sch.dma("pool", WC, win_v[:, :, 0:1536], writes=[bWC] + dead)
                        pending.append((i + NDEF, z, qt, h))

                n = len(items)
                for i in range(n + LA):
                    if i < n:
                        SC(i)
                    if i >= LA:
                        E(i - LA)
                    while pending and pending[0][0] <= i - LA:
                        _, z, qt, h = pending.pop(0)
                        normalize_b(z, qt, h)
                while pending:
                    _, z, qt, h = pending.pop(0)
                    normalize_b(z, qt, h)

            sch.barrier()
            al = Alloc()
            XNT3 = [al.get([KC, TT], BF16) for _ in range(2)]
            SQ = [al.get([TT], BF16) for _ in range(2)]
            RS = al.get([TT], F32)
            CSB = al.get([TT], F32)
            CU = al.get([4, TT + 16], F32)
            T1 = al.get([TT], F32)
            T2 = CSB
            p3_end = max(al.off, c_off - 16384)
            assert p3_end + 16384 <= c_off, p3_end
            CONVIN = view(ARENA_BYTES - 32768, [4, S], BF16)
            bXNT3 = [[Buf() for _ in range(KC)] for _ in range(2)]
            bSQ = [Buf(), Buf()]
            bRS, bCSB, bT1 = Buf(), Buf(), Buf()
            bT2 = bCSB
            bCU = [Buf() for _ in range(4)]
            bCONVIN = [[Buf() for _ in range(NT)] for _ in range(4)]
            memset("dve", CU[:, :, 0:2], 0.0, bCU)
            WGA = view(p3_end, [KC, 1024], BF16)
            al4 = Alloc(c_off + 24576)
            WO = al4.get([KC, D], BF16)
            WCO = al4.get([4, D], BF16)
            WMO = al4.get([4, D], BF16)
            assert al4.off <= ARENA_BYTES - 32768, al4.off
            bWGA, bWGB, bWCO, bWMO, bWO = Buf(), Buf(), Buf(), Buf(), Buf()
            wload(WGA, win_v[:, :, 2208:3232], bWGA)
            wload(WCO, kview(w_conv_out[l]), bWCO)
            wload(WMO, kview(w_mla_out[l]), bWMO)
            wload(WO, kview(w_o[l]), bWO)
            cw = 50 + l * 12
            for t in range(NT):
                XNT, bXNT = XNT3[t % 2], bXNT3[t % 2]
                if t == 0:
                    emit_xn(0, l * 8, XNT3[0], bXNT3[0], SQ, bSQ, RS, bRS, 7)
                for c in range(4):
                    if c == 2 and t + 1 < NT:
                        emit_xn(t + 1, l * 8, XNT3[(t + 1) % 2], bXNT3[(t + 1) % 2], SQ, bSQ, RS, bRS, 7)
                    pb = 3 * (c % 2)
                    bC, bU, bB = pb, pb + 1, pb + 2
                    for (bk, col0) in ((bC, 512), (bU, 1024), (bB, 0)):
                        for k in range(KC):
                            mm(PS[bk][:], WC[:, k, col0 + c * 128: col0 + (c + 1) * 128], XNT[:, k, :],
                               k == 0, k == KC - 1, reads=[bWC, bXNT[k]], writes=[bPS[bk]])
                    cp("act", CSB, PS[bC][:], reads=[bPS[bC]], writes=[bCSB])
                    tt("dve", CU[:, c, 2:TT + 2], CSB, PS[bU][:], ALU.mult, reads=[bCSB, bPS[bU], bCU[c]], writes=[bCU[c]])
                    ts("dve", T1, CU[:, c, 0:TT], vcol(cw + 0 * 4 + c), ALU.mult, reads=[bCU[c], bCONST], writes=[bT1])
                    stt("dve", T2, CU[:, c, 1:TT + 1], vcol(cw + 1 * 4 + c), T1, ALU.mult, ALU.add,
                        reads=[bCU[c], bT1, bCONST], writes=[bT2])
                    stt("dve", T1, CU[:, c, 2:TT + 2], vcol(cw + 2 * 4 + c), T2, ALU.mult, ALU.add,
                        reads=[bCU[c], bT2, bCONST], writes=[bT1])
                    tt("dve", CONVIN[:, c, tsl(t)], T1, PS[bB][:], ALU.mult, reads=[bT1, bPS[bB]],
                       writes=[bCONVIN[c][t]])
                    cp("act", CU[:, c, 0:2], CU[:, c, TT:TT + 2], reads=[bCU[c]], writes=[bCU[c]])

            sch.barrier()
            WGB = view(c_off, [KC, 1024], BF16)
            wload(WGB, win_v[:, :, 3232:4256], bWGB)
            al = Alloc()
            XNTS = [al.get([KC, TT], BF16) for _ in range(2)]
            MERGED = al.get([KC, TT], BF16)
            SG = [al.get([TT], F32) for _ in range(2)]
            SQ = [al.get([TT], BF16) for _ in range(2)]
            RS = al.get([TT], F32)
            assert al.off <= p3_end, al.off
            bXNTS = [[Buf() for _ in range(KC)] for _ in range(2)]
            bMERGED = [Buf() for _ in range(KC)]
            bSG = [Buf(), Buf()]
            bSQ = [Buf(), Buf()]
            bRS = Buf()
            emit_xn(0, l * 8, XNTS[0], bXNTS[0], SQ, bSQ, RS, bRS, 7)
            for t in range(NT):
                XNT, bXNT = XNTS[t % 2], bXNTS[t % 2]
                for j in range(KC):
                    if j == 4 and t + 1 < NT:
                        emit_xn(t + 1, l * 8, XNTS[(t + 1) % 2], bXNTS[(t + 1) % 2], SQ, bSQ, RS, bRS, 7)
                    pb = 0 if j % 2 == 0 else 3
                    b_gc, b_gm, b_yc = pb, pb + 1, pb + 2
                    b_ym = 6
                    js = slice(j * 128, (j + 1) * 128)
                    for k in range(KC):
                        mm(PS[b_gc][:], WGA[:, k, j * 128:(j + 1) * 128], XNT[:, k, :], k == 0, k == KC - 1,
                           reads=[bWGA, bXNT[k]], writes=[bPS[b_gc]])
                    for k in range(KC):
                        mm(PS[b_gm][:], WGB[:, k, j * 128:(j + 1) * 128], XNT[:, k, :], k == 0, k == KC - 1,
                           reads=[bWGB, bXNT[k]], writes=[bPS[b_gm]])
                    for k in range(4):
                        mm(PS[b_yc][:], WCO[:, k, js], CONVIN[:, k, tsl(t)], k == 0, k == 3,
                           reads=[bWCO, bCONVIN[k][t]], writes=[bPS[b_yc]])
                    for k in range(4):
                        mm(PS[b_ym][:], WMO[:, k, js], ATT[:, k, tsl(t)], k == 0, k == 3,
                           reads=[bWMO, bATT[2 * k][t], bATT[2 * k + 1][t]], writes=[bPS[b_ym]])
                    act(SG[0], PS[b_gc][:], AF.Sigmoid, reads=[bPS[b_gc]], writes=[bSG[0]])
                    act(SG[1], PS[b_gm][:], AF.Sigmoid, reads=[bPS[b_gm]], writes=[bSG[1]])
                    tt("dve", SG[0], SG[0], PS[b_yc][:], ALU.mult, reads=[bSG[0], bPS[b_yc]], writes=[bSG[0]])
                    tt("dve", SG[1], SG[1], PS[b_ym][:], ALU.mult, reads=[bSG[1], bPS[b_ym]], writes=[bSG[1]])
                    tt("dve", MERGED[:, j, :], SG[0], SG[1], ALU.add, reads=[bSG[0], bSG[1]], writes=[bMERGED[j]])
                for j in range(KC):
                    bk = 0 if j % 2 == 0 else 3
                    for k in range(KC):
                        mm(PS[bk][:], WO[:, k, j * 128:(j + 1) * 128], MERGED[:, k, :], k == 0, k == KC - 1,
                           reads=[bWO, bMERGED[k]], writes=[bPS[bk]])
                    tt("dve", XRES[:, j, tsl(t)], XRES[:, j, tsl(t)], PS[bk][:], ALU.add,
                       reads=[bX[j][t], bPS[bk]], writes=[bX[j][t]])

        def ffn(l):
            moe = (l % 2 == 1)
            i = l // 2
            sch.barrier()
            if (l + 1) in layers and not moe and do_mixer:
                w1_load(l + 1)
            al = Alloc()
            HN = al.get([KC, S], BF16)
            HB = [al.get([max(GSZ), S], BF16) for _ in range(2)]
            WGU = [(al.get([KC, 128], BF16), al.get([KC, 128], BF16)) for _ in range(3)]
            WDS = [al.get([D], BF16) for _ in range(8)]
            SG = [al.get([TT], F32) for _ in range(2)]
            _tf = al.get([TT], F32)
            TF = [_tf, _tf]
            SQ = [al.get([TT], BF16) for _ in range(2)]
            RS = al.get([TT], F32)
            if moe:
                CBC = al.get([S], F32)
                COMBT = al.get([S], F32)
                ROUT = al.get([KC, NE], F32)
                GR = al.get([KC, NE], F32)
                LG = al.get([8, 4 * NE], F32)
                SM = al.get([8, 8], F32)
            assert al.off <= ARENA_BYTES, al.off
            bHN = [[Buf() for _ in range(NT)] for _ in range(KC)]
            bHB = [[[Buf() for _ in range(NT)] for _ in range(max(GSZ))] for _ in range(2)]
            bWGU = [Buf() for _ in range(3)]
            bWDS = [Buf() for _ in range(8)]
            _btf = Buf()
            bSG, bTF, bSQ = [Buf(), Buf()], [_btf, _btf], [Buf(), Buf()]
            bRS, bCBC, bCOMBT, bGR, bLG = Buf(), [Buf() for _ in range(NT)], [Buf() for _ in range(NT)], Buf(), Buf()

            gcol = 16 + l * 8
            if moe:
                sch.dma("sp", ROUT, router[i].rearrange("(c p) e -> p c e", p=128), writes=[bGR])
                tt("dve", GR, ROUT, VECS[:, gcol:gcol + 8].unsqueeze(2).to_broadcast([128, KC, NE]), ALU.mult,
                   reads=[bGR, bCONST], writes=[bGR])
            def route_chain(t):
                pb = t

                def v3(i):
                    return LG[:, i, :].rearrange("p (b e) -> p b e", e=NE)

                def bc(ap2):
                    return ap2.unsqueeze(2).to_broadcast([128, 4, NE])
                lg, lg2, e1, e2, cmb = v3(0), v3(1), v3(2), v3(3), v3(4)
                rstd, m1, m2, dm, w1, w2 = (SM[:, 0, 0:4], SM[:, 1, 0:4], SM[:, 2, 0:4], SM[:, 3, 0:4],
                                            SM[:, 4, 0:4], SM[:, 5, 0:4])
                cp("dve", rstd, PS[pb][:, 64:68], reads=[bPS[pb]], writes=[bLG])
                tt("dve", lg, PS[pb][:, 0:4 * NE].rearrange("p (b e) -> p b e", e=NE), bc(rstd), ALU.mult,
                   reads=[bPS[pb], bLG], writes=[bLG])
                sch.op("dve", lambda h, o=m1, i_=lg: h.reduce_max(out=o, in_=i_, axis=AX.X), reads=[bLG], writes=[bLG])
                tt("dve", e1, lg, bc(m1), ALU.is_ge, reads=[bLG], writes=[bLG])
                stt("dve", lg2, e1, -1e30, lg, ALU.mult, ALU.add, reads=[bLG], writes=[bLG])
                sch.op("dve", lambda h, o=m2, i_=lg2: h.reduce_max(out=o, in_=i_, axis=AX.X), reads=[bLG], writes=[bLG])
                tt("dve", e2, lg2, bc(m2), ALU.is_ge, reads=[bLG], writes=[bLG])
                tt("dve", dm, m2, m1, ALU.subtract, reads=[bLG], writes=[bLG])
                act(w2, dm, AF.Sigmoid, reads=[bLG], writes=[bLG])
                act(w1, dm, AF.Sigmoid, reads=[bLG], writes=[bLG], scale=-1.0)
                tt("dve", cmb, e1, bc(w1), ALU.mult, reads=[bLG], writes=[bLG])
                tt("dve", e2, e2, bc(w2), ALU.mult, reads=[bLG], writes=[bLG])
                tt("dve", cmb, cmb, e2, ALU.add, reads=[bLG], writes=[bLG])
                for bi in range(4):
                    mm(PS[6][0:NE, bi * 128:(bi + 1) * 128], LG[:, 4, bi * NE:(bi + 1) * NE], IDENTF, True, True,
                       reads=[bLG, bCONST], writes=[bPS[6]])
                cp("act", COMBT[0:NE, tsl(t)], PS[6][0:NE, :], reads=[bPS[6]], writes=[bCOMBT[t]])

            for t in range(NT):
                rms_scale([XRES[:, c, tsl(t)] for c in range(KC)], [bX[c][t] for c in range(KC)], D,
                          SQ, bSQ, RS, bRS, 7)
                for c in range(KC):
                    stt("dve", HN[:, c, tsl(t)], XRES[:, c, tsl(t)], vcol(gcol + c), RS, ALU.mult, ALU.mult,
                        reads=[bX[c][t], bRS, bCONST], writes=[bHN[c][t]])
                if moe:
                    for bi in range(4):
                        tok = slice(t * TT + bi * 128, t * TT + (bi + 1) * 128)
                        for k in range(KC):
                            mm(PS[t][:, bi * NE:(bi + 1) * NE], XRES[:, k, tok], GR[:, k, :], k == 0, k == KC - 1,
                               reads=[bX[k][t], bGR], writes=[bPS[t]])
                        mm(PS[t][:, 64 + bi:65 + bi], RS[0:1, bi * 128:(bi + 1) * 128], ONESF[0:1, 0:1], True, True,
                           reads=[bRS, bCONST], writes=[bPS[t]])
                    if t > 0:
                        route_chain(t - 1)
            if moe:
                route_chain(NT - 1)

            if moe:
                experts = [(w_gate_e[i, e], w_up_e[i, e], w_down_e[i, e], e) for e in range(NE)]
            else:
                experts = [(w_gate[i][:, h * DFE:(h + 1) * DFE], w_up[i][:, h * DFE:(h + 1) * DFE],
                            w_down[i][h * DFE:(h + 1) * DFE, :], None) for h in range(2)]
            gi = 0
            wi = 0
            di = 0
            ev = 0
            for (wg_d, wu_d, wd_d, e) in experts:
                wg_v = kview(wg_d)
                wu_v = kview(wu_d)
                if moe:
                    for t in range(NT):
                        mm(PS[6][:], SELF[0:NE, e, :], COMBT[0:NE, tsl(t)], True, True,
                           reads=[bCOMBT[t], bCONST], writes=[bPS[6]])
                        cp("act", CBC[:, tsl(t)], PS[6][:], reads=[bPS[6]], writes=[bCBC[t]])
                c0 = 0
                for G in GSZ:
                    hb = gi % 2
                    gi += 1
                    dslots = []
                    for cc in range(G):
                        f = c0 + cc
                        ws = wi % 3
                        wi += 1
                        ds_ = di % 8
                        di += 1
                        dslots.append(ds_)
                        wload(WGU[ws][0], wg_v[:, :, f * 128:(f + 1) * 128], bWGU[ws])
                        wload(WGU[ws][1], wu_v[:, :, f * 128:(f + 1) * 128], bWGU[ws])
                        wload(WDS[ds_], wd_d[f * 128:(f + 1) * 128, :], bWDS[ds_])
                        for t in range(NT):
                            pg = (ev % 2) * 2
                            pu = pg + 1
                            sg = ev % 2
                            ev += 1
                            for k in range(KC):
                                mm(PS[pg][:], WGU[ws][0][:, k, :], HN[:, k, tsl(t)], k == 0, k == KC - 1,
                                   reads=[bWGU[ws], bHN[k][t]], writes=[bPS[pg]])
                            for k in range(KC):
                                mm(PS[pu][:], WGU[ws][1][:, k, :], HN[:, k, tsl(t)], k == 0, k == KC - 1,
                                   reads=[bWGU[ws], bHN[k][t]], writes=[bPS[pu]])
                            act(SG[sg], PS[pg][:], AF.Silu, reads=[bPS[pg]], writes=[bSG[sg]])
                            if moe:
                                tt("dve", TF[sg], SG[sg], PS[pu][:], ALU.mult, reads=[bSG[sg], bPS[pu]], writes=[bTF[sg]])
                                tt("dve", HB[hb][:, cc, tsl(t)], TF[sg], CBC[:, tsl(t)], ALU.mult,
                                   reads=[bTF[sg], bCBC[t]], writes=[bHB[hb][cc][t]])
                            else:
                                tt("dve", HB[hb][:, cc, tsl(t)], SG[sg], PS[pu][:], ALU.mult,
                                   reads=[bSG[sg], bPS[pu]], writes=[bHB[hb][cc][t]])
                    dj = 0
                    for j in range(KC):
                        for t in range(NT):
                            pd = 4 + (dj % 2)
                            dj += 1
                            for cc in range(G):
                                mm(PS[pd][:], WDS[dslots[cc]][:, j * 128:(j + 1) * 128], HB[hb][:, cc, tsl(t)],
                                   cc == 0, cc == G - 1, reads=[bWDS[dslots[cc]], bHB[hb][cc][t]], writes=[bPS[pd]])
                            tt("dve", XRES[:, j, tsl(t)], XRES[:, j, tsl(t)], PS[pd][:], ALU.add,
                               reads=[bX[j][t], bPS[pd]], writes=[bX[j][t]])
                    c0 += G

        if not do_mixer:
            load_x_tiles(1, NT)
            x_loaded[0] = True
        for l in layers:
            if do_mixer:
                mixer(l)
            if do_ffn:
                ffn(l)

        sch.barrier()
        al = Alloc()
        OUTS = [al.get([KC, TT], F32) for _ in range(2)]
        SQ = [al.get([TT], BF16) for _ in range(2)]
        RS = al.get([TT], F32)
        bOUT = [[Buf() for _ in range(KC)] for _ in range(2)]
        bSQ = [Buf(), Buf()]
        bRS = Buf()
        for t in range(NT):
            o = t % 2
            if last:
                rms_scale([XRES[:, c, tsl(t)] for c in range(KC)], [bX[c][t] for c in range(KC)], D,
                          SQ, bSQ, RS, bRS, 7)
                for c in range(KC):
                    stt("dve", OUTS[o][:, c, :], XRES[:, c, tsl(t)], vcol(32 + c), RS, ALU.mult, ALU.mult,
                        reads=[bX[c][t], bRS, bCONST], writes=[bOUT[o][c]])
                    sch.dma("sp", outT[c * 128:(c + 1) * 128, tsl(t)], OUTS[o][:, c, :], reads=[bOUT[o][c]])
            else:
                for c in range(KC):
                    sch.dma("sp", outT[c * 128:(c + 1) * 128, tsl(t)], XRES[:, c, tsl(t)], reads=[bX[c][t]])
        sch.final_wait("sp")
        sch.finalize()

        @block.tensor
        def _(h):
            sch.emit("pe", h)

        @block.scalar
        def _(h):
            sch.emit("act", h)

        @block.vector
        def _(h):
            sch.emit("dve", h)

        @block.gpsimd
        def _(h):
            sch.emit("pool", h)

        @block.sync
        def _(h):
            sch.emit("sp", h)

    return nc


def _host_consts():
    cf = np.zeros((128, 128 + 1024), np.float32)
    cf[:, 0:128] = np.eye(128, dtype=np.float32)
    for e in range(8):
        cf[e, 128 + e * 128: 128 + (e + 1) * 128] = 1.0
    cb = np.zeros((128, 256), np.float32)
    cb[:, 0:128] = np.eye(128, dtype=np.float32)
    k = np.arange(128)[:, None]
    q = np.arange(128)[None, :]
    cb[:, 128:256] = np.where(q >= k, 0.0, -30000.0).astype(np.float32)
    return cf, cb


def _shared_inputs(inp):
    f = lambda a: np.ascontiguousarray(np.asarray(a, dtype=np.float32))
    vecs = np.zeros((128, NV), np.float32)

    def put(col, v):
        v = np.asarray(v, np.float32).reshape(-1, 128)
        vecs[:, col:col + v.shape[0]] = v.T

    for l in range(2):
        put(0 + l * 8, inp["attn_norm"][l])
        put(16 + l * 8, inp["ffn_norm"][l])
        put(40 + l * 3, inp["q_norm"][l])
        put(46 + l * 2, inp["kv_norm"][l])
        for k in range(3):
            put(50 + l * 12 + k * 4, inp["conv_w"][l, k])
    put(32, inp["final_norm"])
    inv_freq = (10000.0 ** (-np.arange(0, 32, 2, dtype=np.float32) / np.float32(32))).astype(np.float32)
    vecs[64:96, 74] = np.concatenate([inv_freq, inv_freq])
    cf, cb = _host_consts()
    w_uq = f(inp["w_uq"])
    w_in = f(inp["w_in"])
    wq4 = w_uq.reshape(2, 384, 8, 96)
    wq_rot = np.concatenate([wq4[..., 80:96], wq4[..., 64:80]], axis=-1).reshape(2, 384, 256)
    wkr = w_in[:, :, 2176:2208]
    wkr_rot = np.concatenate([w_in[:, :, 2192:2208], w_in[:, :, 2176:2192]], axis=-1)
    sh = {
        "vecs": vecs, "cf": cf, "cb": cb, "w_in": w_in, "w_conv_out": f(inp["w_conv_out"]),
        "w_uq": w_uq, "wq_rot": f(wq_rot), "wkr": f(wkr), "wkr_rot": f(wkr_rot),
        "w_ukv": f(inp["w_ukv"]), "w_mla_out": f(inp["w_mla_out"]), "w_o": f(inp["w_o"]),
        "w_gate": f(inp["w_gate"]), "w_up": f(inp["w_up"]), "w_down": f(inp["w_down"]),
        "router": f(inp["router"]), "w_gate_e": f(inp["w_gate_e"]), "w_up_e": f(inp["w_up_e"]),
        "w_down_e": f(inp["w_down_e"]),
    }
    return sh


_NC_CACHE = {}


def run_layers(inp, xT_list, layers, last):
    key = (tuple(layers), last)
    if key not in _NC_CACHE:
        _NC_CACHE[key] = build_nc(layers, last=last)
    nc = _NC_CACHE[key]
    sh = _shared_inputs(inp)
    pos = np.asarray(inp["positions"]).astype(np.int32)
    in_maps = []
    for b in range(8):
        m = dict(sh)
        m["xT"] = np.ascontiguousarray(xT_list[b])
        m["posrep"] = np.ascontiguousarray(np.broadcast_to(pos[b][None, :], (32, S)))
        in_maps.append(m)
    res = run_bass_kernel_spmd(nc, in_maps, core_ids=list(range(8)))
    return [r["outT"] for r in res.results]


def kernel(**inputs):
    x = np.asarray(inputs["x"], dtype=np.float32)
    xT = [np.ascontiguousarray(x[b].T) for b in range(8)]
    outs = run_layers(inputs, xT, [0, 1], True)
    return np.stack([np.ascontiguousarray(o.T) for o in outs], axis=0).astype(np.float32)
```

```python
import math
import numpy as np
import concourse.bass as bass
import concourse.mybir as mybir
from concourse.bass_utils import run_bass_kernel_spmd

F32 = mybir.dt.float32
BF16 = mybir.dt.bfloat16
I32 = mybir.dt.int32
ALU = mybir.AluOpType
AF = mybir.ActivationFunctionType
AX = mybir.AxisListType

D = 1024
S = 2048
KC = 8
TT = 512
NT = 4
NH = 8
DIN = 4256
DFF = 2816
DFE = 1408
NE = 8
EPS = 1e-6
NV = 80
SCALE = 1.0 / math.sqrt(96.0)
ARENA_BYTES = 140800
GSZ = (6, 5)
SAME_ENGINE_SYNC = True


class Tok:
    __slots__ = ("eng", "idx", "sem", "val")

    def __init__(self, eng=None, idx=None, sem=None, val=None):
        self.eng, self.idx, self.sem, self.val = eng, idx, sem, val


class Buf:
    __slots__ = ("w", "r", "name")

    def __init__(self, name=""):
        self.w = None
        self.r = []
        self.name = name


class Eng:
    def __init__(self, name, sem, dma_sems):
        self.name = name
        self.sem = sem
        self.ops = []
        self.waited = {}
        self.last_real = -1
        self.dma_sems = dma_sems
        self.dma_cnt = [0] * len(dma_sems)
        self.dma_rr = 0


class Sched:
    def __init__(self, sems):
        it = iter(sems)
        self.e = {}
        for name, nd in (("pe", 0), ("act", 0), ("dve", 0), ("pool", 24), ("sp", 12)):
            s = next(it)
            self.e[name] = Eng(name, s, [next(it) for _ in range(nd)])

    def _waits(self, e, toks):
        best = {}
        for t in toks:
            if t is None:
                continue
            if t.eng is None:
                k = ("d", id(t.sem))
                if e.waited.get(k, 0) >= t.val:
                    continue
                if k not in best or best[k].val < t.val:
                    best[k] = t
            else:
                if t.eng is e and (e.name == "pe" or not SAME_ENGINE_SYNC):
                    continue
                k = ("e", t.eng.name)
                if e.waited.get(k, -1) >= t.idx:
                    continue
                if k not in best or best[k].idx < t.idx:
                    best[k] = t
        out = []
        for k, t in best.items():
            if t.eng is None:
                e.waited[k] = t.val
            else:
                e.waited[k] = t.idx
                t.eng.ops[t.idx][2] = True
            out.append(t)
        return out

    @staticmethod
    def _deps(reads, writes):
        toks = []
        for b in reads:
            toks.append(b.w)
        for b in writes:
            toks.append(b.w)
            toks.extend(b.r)
        return toks

    @staticmethod
    def _mark(tok, reads, writes):
        for b in reads:
            b.r.append(tok)
        for b in writes:
            b.w = tok
            b.r = []

    def op(self, eng, fn, reads=(), writes=()):
        e = self.e[eng]
        waits = self._waits(e, self._deps(reads, writes))
        idx = len(e.ops)
        e.ops.append([waits, fn, False])
        e.last_real = idx
        tok = Tok(eng=e, idx=idx)
        self._mark(tok, reads, writes)
        return tok

    def dma(self, eng, out, in_, reads=(), writes=()):
        e = self.e[eng]
        slot = e.dma_rr % len(e.dma_sems)
        e.dma_rr += 1
        sem = e.dma_sems[slot]
        toks = self._deps(reads, writes)
        if e.dma_cnt[slot] > 0:
            toks.append(Tok(sem=sem, val=e.dma_cnt[slot]))
        waits = self._waits(e, toks)
        e.dma_cnt[slot] += 16
        tok = Tok(sem=sem, val=e.dma_cnt[slot])
        e.ops.append([waits, (lambda h, o=out, i=in_: h.dma_start(out=o, in_=i)), (sem, 16)])
        self._mark(tok, reads, writes)
        return tok

    def _all_toks(self, skip=None):
        toks = []
        for f in self.e.values():
            if f is not skip and f.last_real >= 0:
                toks.append(Tok(eng=f, idx=f.last_real))
            for s, c in zip(f.dma_sems, f.dma_cnt):
                if c > 0:
                    toks.append(Tok(sem=s, val=c))
        return toks

    def barrier(self):
        for e in self.e.values():
            own = (e.name == "pe" or not SAME_ENGINE_SYNC)
            waits = self._waits(e, self._all_toks(skip=e if own else None))
            if waits:
                e.ops.append([waits, None, False])

    def final_wait(self, eng):
        e = self.e[eng]
        waits = self._waits(e, self._all_toks(skip=e))
        if waits:
            e.ops.append([waits, None, False])

    def finalize(self):
        self.cnt = {}
        for name, e in self.e.items():
            c = 0
            arr = []
            for (_, fn, sig) in e.ops:
                if sig is True:
                    c += 1
                arr.append(c)
            self.cnt[name] = arr

    def emit(self, name, h):
        for waits, fn, sig in self.e[name].ops:
            for t in waits:
                if t.eng is None:
                    h.wait_ge(t.sem, t.val)
                else:
                    h.wait_ge(t.eng.sem, self.cnt[t.eng.name][t.idx])
            if fn is not None:
                ins = fn(h)
                if sig is True:
                    ins.then_inc(self.e[name].sem, 1)
                elif sig:
                    ins.then_inc(sig[0], sig[1])


def build_nc(layers, last=True, do_mixer=True, do_ffn=True):
    nc = bass.Bass("TRN2", target_bir_lowering=False)

    def din(name, shape, dt=F32):
        return nc.dram_tensor(name, list(shape), dt, kind="ExternalInput").ap()

    xT = din("xT", [D, S])
    posrep = din("posrep", [32, S], I32)
    vecs_d = din("vecs", [128, NV])
    cf_d = din("cf", [128, 128 + 1024])
    cb_d = din("cb", [128, 256])
    w_in = din("w_in", [2, D, DIN])
    w_conv_out = din("w_conv_out", [2, 512, D])
    w_uq = din("w_uq", [2, 384, 768])
    wq_rot = din("wq_rot", [2, 384, 256])
    wkr = din("wkr", [2, D, 32])
    wkr_rot = din("wkr_rot", [2, D, 32])
    w_ukv = din("w_ukv", [2, 256, 1024])
    w_mla_out = din("w_mla_out", [2, 512, D])
    w_o = din("w_o", [2, D, D])
    w_gate = din("w_gate", [1, D, DFF])
    w_up = din("w_up", [1, D, DFF])
    w_down = din("w_down", [1, DFF, D])
    router = din("router", [1, D, NE])
    w_gate_e = din("w_gate_e", [1, NE, D, DFE])
    w_up_e = din("w_up_e", [1, NE, D, DFE])
    w_down_e = din("w_down_e", [1, NE, DFE, D])
    outT = nc.dram_tensor("outT", [D, S], F32, kind="ExternalOutput").ap()

    import contextlib
    es = contextlib.ExitStack()
    with es:
        XRES = es.enter_context(nc.sbuf_tensor("XRES", [128, KC, S], F32))
        VECS = es.enter_context(nc.sbuf_tensor("VECS", [128, NV], F32))
        CF = es.enter_context(nc.sbuf_tensor("CF", [128, 128 + 1024], F32))
        CB = es.enter_context(nc.sbuf_tensor("CB", [128, 256], BF16))
        ONESB = es.enter_context(nc.sbuf_tensor("ONESB", [128, 128], BF16))
        ONESF = es.enter_context(nc.sbuf_tensor("ONESF", [128, 128], F32))
        ARENA = es.enter_context(nc.sbuf_tensor("ARENA", [128, ARENA_BYTES // 2], BF16))
        PS = [es.enter_context(nc.psum_tensor(f"PS{i}", [128, 512], F32)) for i in range(8)]
        sems = [es.enter_context(nc.semaphore(f"sem{i}")) for i in range(5 + 24 + 12)]
        block = es.enter_context(nc.Block())

        sch = Sched(sems)
        IDENTF = CF[:, 0:128]
        SELF = CF[:, 128:128 + 1024].rearrange("p (e m) -> p e m", e=8)
        IDENTB = CB[:, 0:128]
        TRIB = CB[:, 128:256]

        def view(off, shape, dt):
            n = int(np.prod(shape))
            esz = 4 if dt in (F32, I32) else 2
            assert off % 4 == 0 and off + n * esz <= ARENA_BYTES, (off, shape, ARENA_BYTES)
            a = ARENA[:, off // 2: off // 2 + n * esz // 2]
            if dt != BF16:
                a = a.bitcast(dt)
            if len(shape) == 2:
                a = a.rearrange("p (a b) -> p a b", a=shape[0])
            elif len(shape) == 3:
                a = a.rearrange("p (a b c) -> p a b c", a=shape[0], b=shape[1])
            return a

        class Alloc:
            def __init__(self, start=0):
                self.off = start

            def get(self, shape, dt):
                n = int(np.prod(shape))
                esz = 4 if dt in (F32, I32) else 2
                v = view(self.off, shape, dt)
                self.off += (n * esz + 63) // 64 * 64
                return v

        def mm(out, lhsT, rhs, start, stop, reads, writes):
            sch.op("pe", lambda h: h.matmul(out, lhsT=lhsT, rhs=rhs, start=start, stop=stop,
                                            skip_group_check=True),
                   reads=reads, writes=writes)

        def act(out, in_, func, reads, writes, scale=1.0, bias=0.0):
            sch.op("act", lambda h: h.activation(out=out, in_=in_, func=func, bias=bias, scale=scale),
                   reads=reads, writes=writes)

        def tt(eng, out, in0, in1, op, reads, writes):
            sch.op(eng, lambda h: h.tensor_tensor(out=out, in0=in0, in1=in1, op=op),
                   reads=reads, writes=writes)

        def stt(eng, out, in0, scalar, in1, op0, op1, reads, writes):
            sch.op(eng, lambda h: h.scalar_tensor_tensor(out=out, in0=in0, scalar=scalar, in1=in1,
                                                          op0=op0, op1=op1),
                   reads=reads, writes=writes)

        def ts(eng, out, in0, s1, op0, reads, writes, s2=None, op1=None):
            if op1 is None:
                sch.op(eng, lambda h: h.tensor_scalar(out=out, in0=in0, scalar1=s1, scalar2=None, op0=op0),
                       reads=reads, writes=writes)
            else:
                sch.op(eng, lambda h: h.tensor_scalar(out=out, in0=in0, scalar1=s1, scalar2=s2,
                                                      op0=op0, op1=op1),
                       reads=reads, writes=writes)

        def cp(eng, out, in_, reads, writes):
            if eng == "act":
                sch.op("act", lambda h: h.copy(out=out, in_=in_), reads=reads, writes=writes)
            else:
                sch.op(eng, lambda h: h.tensor_copy(out=out, in_=in_), reads=reads, writes=writes)

        def memset(eng, ap, val, writes):
            sch.op(eng, lambda h: h.memset(ap, val), reads=(), writes=writes)

        bX = [[Buf(f"x{c}_{t}") for t in range(NT)] for c in range(KC)]
        bPS = [Buf(f"ps{i}") for i in range(8)]
        bCONST = Buf("const")

        def tsl(t):
            return slice(t * TT, (t + 1) * TT)

        sch.dma("sp", VECS[:], vecs_d[:], writes=[bCONST])
        sch.dma("sp", CF[:], cf_d[:], writes=[bCONST])
        sch.dma("pool", CB[:], cb_d[:], writes=[bCONST])
        memset("dve", ONESB[:], 1.0, [bCONST])
        memset("dve", ONESF[:], 1.0, [bCONST])
        xT_v = xT.rearrange("(c p) s -> p c s", p=128)
        x_loaded = [False]

        def load_x_tiles(t0, t1):
            for t in range(t0, t1):
                sch.dma("sp", XRES[:, :, tsl(t)], xT_v[:, :, tsl(t)], writes=[bX[c][t] for c in range(KC)],
                        reads=([bX[0][t - 1]] if t > 0 else []))

        load_x_tiles(0, 1)

        def vcol(i):
            return VECS[:, i:i + 1]

        def rms_scale(srcs, src_bufs, nfeat, SQ, bSQ, RS, bRS, bank):
            n = len(srcs)
            for i, (s, sb) in enumerate(zip(srcs, src_bufs)):
                q = i % 2
                act(SQ[q], s, AF.Square, reads=[sb], writes=[bSQ[q]])
                mm(PS[bank][:], ONESB[:], SQ[q], i == 0, i == n - 1,
                   reads=[bSQ[q], bCONST], writes=[bPS[bank]])
            act(RS, PS[bank][:], AF.Ln, reads=[bPS[bank]], writes=[bRS], scale=1.0 / nfeat, bias=EPS)
            act(RS, RS, AF.Exp, reads=[bRS], writes=[bRS], scale=-0.5)

        def emit_xn(t, gcol0, XNT, bXNT, SQ, bSQ, RS, bRS, bank):
            rms_scale([XRES[:, c, tsl(t)] for c in range(KC)], [bX[c][t] for c in range(KC)], D,
                      SQ, bSQ, RS, bRS, bank)
            for c in range(KC):
                stt("dve", XNT[:, c, :], XRES[:, c, tsl(t)], vcol(gcol0 + c), RS, ALU.mult, ALU.mult,
                    reads=[bX[c][t], bRS, bCONST], writes=[bXNT[c]])

        def wload(dst, src, buf):
            sch.dma("pool", dst, src, writes=[buf])

        def kview(w2d):
            return w2d.rearrange("(c p) n -> p c n", p=128)

        W1_OFF = ARENA_BYTES - 17920
        w1state = {}

        def w1_load(l):
            al = Alloc(W1_OFF)
            WINB = al.get([KC, 672], BF16)
            WKR = al.get([KC, 96], BF16)
            WKRROT = al.get([KC, 96], BF16)
            WKVK = al.get([2, 512], BF16)
            WKVV = al.get([2, 512], BF16)
            assert al.off <= ARENA_BYTES
            bWINB, bWKR, bWKRROT, bWKVK, bWKVV = Buf(), Buf(), Buf(), Buf(), Buf()
            bWKZ = Buf()
            win_v = kview(w_in[l])
            memset("dve", WKR[:, :, 0:64], 0.0, [bWKZ])
            memset("dve", WKRROT[:, :, 0:64], 0.0, [bWKZ])
            wload(WKR[:, :, 64:96], kview(wkr[l]), bWKR)
            wload(WKRROT[:, :, 64:96], kview(wkr_rot[l]), bWKRROT)
            wload(WINB, win_v[:, :, 1536:2208], bWINB)
            ukv = w_ukv[l].rearrange("(c p) (h e) -> p c h e", p=128, e=128)
            for c in range(2):
                wload(WKVK[:, c, :].rearrange("p (h e) -> p h e", e=64), ukv[:, c, :, 0:64], bWKVK)
                wload(WKVV[:, c, :].rearrange("p (h e) -> p h e", e=64), ukv[:, c, :, 64:128], bWKVV)
            w1state[l] = (WINB, WKR, WKRROT, WKVK, WKVV, bWINB, bWKR, bWKRROT, bWKVK, bWKVV, bWKZ)

        def mixer(l):
            sch.barrier()
            al = Alloc()
            KT = al.get([NH, S], BF16)
            V = al.get([16, NH, 65], BF16)
            c_off = al.off
            CQN = al.get([3, S], BF16)
            WQ = al.get([3, 768], BF16)
            WQROT = al.get([3, NH, 96], BF16)
            COS = al.get([S], F32)
            SIN = al.get([S], F32)
            p12 = al.off
            XNTS = [al.get([KC, TT], BF16) for _ in range(2)]
            SQ = [al.get([TT], BF16) for _ in range(2)]
            RS = al.get([TT], F32)
            RS2 = al.get([TT], F32)
            SQ2 = SQ
            CF32 = al.get([5, TT], F32)
            CKVN = al.get([2, TT], BF16)
            T1 = CF32[:, 3, :]
            T2 = CF32[:, 0, :]
            RT0 = CF32[:, 1, :]
            RT1 = CF32[:, 2, :]
            assert al.off <= W1_OFF, al.off

            bKT = [[Buf() for _ in range(NT)] for _ in range(NH)]
            bV = [Buf() for _ in range(16)]
            bCQN = [[Buf() for _ in range(NT)] for _ in range(3)]
            bWQ, bWQROT = Buf(), Buf()
            bROPE = [Buf() for _ in range(NT)]
            bXNTS = [[Buf() for _ in range(KC)] for _ in range(2)]
            bSQ = [Buf(), Buf()]
            bSQ2 = bSQ
            bRS, bRS2 = Buf(), Buf()
            bCF32 = [Buf() for _ in range(5)]
            bT1, bT2, bRT0, bRT1 = bCF32[3], bCF32[0], bCF32[1], bCF32[2]
            bCKVN = [Buf(), Buf()]

            sch.dma("pool", RT0[slice(64, 96), :], posrep[:, tsl(0)], writes=[bRT0])
            if l not in w1state:
                w1_load(l)
            (WINB, WKR, WKRROT, WKVK, WKVV, bWINB, bWKR, bWKRROT, bWKVK, bWKVV, bWKZ) = w1state[l]
            win_v = kview(w_in[l])
            bWQZ = Buf()
            memset("dve", WQROT[:, :, :, 0:64], 0.0, [bWQZ])
            wload(WQ, kview(w_uq[l]), bWQ)
            for c in range(3):
                wload(WQROT[:, c, :, 64:96], kview(wq_rot[l])[:, c, :].rearrange("p (h e) -> p h e", e=32), bWQROT)

            R = slice(64, 96)

            def rope_pass(t):
                if t > 0:
                    sch.dma("pool", RT0[R, :], posrep[:, tsl(t)], writes=[bRT0])
                ts("dve", RT0[R, :], RT0[R, :], VECS[R, 74:75], ALU.mult, reads=[bRT0, bCONST], writes=[bRT0])
                for tab, shift in ((SIN, 0.0), (COS, math.pi / 2)):
                    tv = tab[R, tsl(t)]
                    ts("dve", tv, RT0[R, :], shift, ALU.add, reads=[bRT0], writes=[bROPE[t]],
                       s2=1.0 / (2 * math.pi), op1=ALU.mult)
                    ts("dve", RT1[R, :], tv, 12582912.0, ALU.add, reads=[bROPE[t]], writes=[bRT1])
                    ts("dve", tv, RT1[R, :], -12582912.0, ALU.add, reads=[bRT1], writes=[bROPE[t]])
                    stt("dve", tv, tv, -2 * math.pi, RT0[R, :], ALU.mult, ALU.add,
                        reads=[bROPE[t], bRT0], writes=[bROPE[t]])
                    ts("dve", tv, tv, shift, ALU.add, reads=[bROPE[t]], writes=[bROPE[t]],
                       s2=3.1415925, op1=ALU.min)
                    ts("dve", tv, tv, -3.1415925, ALU.max, reads=[bROPE[t]], writes=[bROPE[t]])
                    act(tv, tv, AF.Sin, reads=[bROPE[t]], writes=[bROPE[t]])

            memset("dve", V.rearrange("p a h e -> p (a h) e")[:, :, 64:65], 1.0, bV)

            qg = 40 + l * 3
            kg = 46 + l * 2
            rope_pass(0)
            if not x_loaded[0]:
                load_x_tiles(1, NT)
                x_loaded[0] = True
            emit_xn(0, l * 8, XNTS[0], bXNTS[0], SQ2, bSQ2, RS2, bRS2, 7)
            for t in range(NT):
                XNT, bXNT = XNTS[t % 2], bXNTS[t % 2]
                for oc in range(5):
                    bk = oc % 4
                    for k in range(KC):
                        mm(PS[bk][:], WINB[:, k, oc * 128:(oc + 1) * 128], XNT[:, k, :], k == 0, k == KC - 1,
                           reads=[bWINB, bXNT[k]], writes=[bPS[bk]])
                    cp("dve", CF32[:, oc, :], PS[bk][:], reads=[bPS[bk]], writes=[bCF32[oc]])
                if t == 0:
                    ts("dve", WKRROT[:, :, 64:80], WKRROT[:, :, 64:80], -1.0, ALU.mult, reads=[], writes=[bWKRROT])
                for k in range(KC):
                    mm(PS[4][0:96, :], WKR[:, k, :], XNT[:, k, :], k == 0, k == KC - 1,
                       reads=[bWKR, bWKZ, bXNT[k]], writes=[bPS[4]])
                for k in range(KC):
                    mm(PS[5][0:96, :], WKRROT[:, k, :], XNT[:, k, :], k == 0, k == KC - 1,
                       reads=[bWKRROT, bWKZ, bXNT[k]], writes=[bPS[5]])
                if t + 1 < NT:
                    emit_xn(t + 1, l * 8, XNTS[(t + 1) % 2], bXNTS[(t + 1) % 2], SQ2, bSQ2, RS2, bRS2, 7)
                rms_scale([CF32[:, 3 + c, :] for c in range(2)], bCF32[3:5], 256, SQ, bSQ, RS, bRS, 6)
                for c in range(2):
                    stt("dve", CKVN[:, c, :], CF32[:, 3 + c, :], vcol(kg + c), RS, ALU.mult, ALU.mult,
                        reads=[bCF32[3 + c], bRS, bCONST], writes=[bCKVN[c]])
                rms_scale([CF32[:, c, :] for c in range(3)], bCF32[0:3], 384, SQ, bSQ, RS, bRS, 6)
                for c in range(3):
                    stt("dve", CQN[:, c, tsl(t)], CF32[:, c, :], vcol(qg + c), RS, ALU.mult, ALU.mult,
                        reads=[bCF32[c], bRS, bCONST], writes=[bCQN[c][t]])
                tt("dve", T1[R, :], PS[4][R, :], COS[R, tsl(t)], ALU.mult, reads=[bPS[4], bROPE[t]], writes=[bT1])
                tt("dve", T2[R, :], PS[5][R, :], SIN[R, tsl(t)], ALU.mult, reads=[bPS[5], bROPE[t]], writes=[bT2])
                tt("dve", KT[R, 0, tsl(t)], T1[R, :], T2[R, :], ALU.add, reads=[bT1, bT2], writes=[bKT[0][t]])
                for h in range(1, NH):
                    cp("act", KT[R, h, tsl(t)], KT[R, 0, tsl(t)], reads=[bKT[0][t]], writes=[bKT[h][t]])
                for hp in range(4):
                    bk = hp % 4
                    for k in range(2):
                        mm(PS[bk][:], WKVK[:, k, hp * 128:(hp + 1) * 128], CKVN[:, k, :], k == 0, k == 1,
                           reads=[bWKVK, bCKVN[k]], writes=[bPS[bk]])
                    cp("act", KT[0:64, 2 * hp, tsl(t)], PS[bk][0:64, :], reads=[bPS[bk]], writes=[bKT[2 * hp][t]])
                    cp("dve", KT[0:64, 2 * hp + 1, tsl(t)], PS[bk][64:128, :], reads=[bPS[bk]],
                       writes=[bKT[2 * hp + 1][t]])
                for bi in range(4):
                    blk = 4 * t + bi
                    bk = 4 + (bi % 2)
                    for k in range(2):
                        mm(PS[bk][:], CKVN[:, k, bi * 128:(bi + 1) * 128], WKVV[:, k, :], k == 0, k == 1,
                           reads=[bWKVV, bCKVN[k]], writes=[bPS[bk]])
                    cp("act", V[:, blk, :, 0:64], PS[bk][:].rearrange("p (h e) -> p h e", e=64),
                       reads=[bPS[bk]], writes=[bV[blk]])
                if t + 1 < NT:
                    rope_pass(t + 1)
            ts("dve", WQROT[:, :, :, 64:80], WQROT[:, :, :, 64:80], -1.0, ALU.mult, reads=[], writes=[bWQROT])

            sch.barrier()
            al = Alloc(p12)
            QT = [al.get([NH, TT], BF16) for _ in range(2)]
            PT = [al.get([TT], BF16) for _ in range(4)]
            ONUM = [al.get([TT], F32) for _ in range(2)]
            RDEN = [al.get([TT], F32) for _ in range(2)]
            RD = [al.get([TT], BF16) for _ in range(2)]
            T1s = [al.get([TT], F32) for _ in range(2)]
            _t2 = al.get([TT], F32)
            T2s = [_t2, _t2]
            assert al.off <= ARENA_BYTES - 16384, al.off
            ATT = view(ARENA_BYTES - 16384, [4, S], BF16)
            WC = view(c_off, [KC, 1536], BF16)
            bWC = Buf()
            bQT = [[Buf() for _ in range(NH)] for _ in range(2)]
            bPT = [Buf() for _ in range(4)]
            bONUM, bRDEN = [Buf(), Buf()], [Buf(), Buf()]
            bRD = [Buf(), Buf()]
            for q in range(2):
                memset("dve", RD[q], 0.0, [bRD[q]])
            _b2 = Buf()
            bT1s, bT2s = [Buf(), Buf()], [_b2, _b2]
            bATT = [[Buf() for _ in range(NT)] for _ in range(NH)]

            QBUF = {3: 0, 2: 0, 0: 1, 1: 1}

            def qproj(qt, h):
                Q, bQ = QT[QBUF[qt]], bQT[QBUF[qt]]
                bq, br = 6, 7
                z = h % 2
                for k in range(3):
                    mm(PS[bq][0:96, :], WQ[:, k, h * 96:(h + 1) * 96], CQN[:, k, tsl(qt)], k == 0, k == 2,
                       reads=[bWQ, bCQN[k][qt]], writes=[bPS[bq]])
                for k in range(3):
                    mm(PS[br][0:96, :], WQROT[:, k, h, :], CQN[:, k, tsl(qt)], k == 0, k == 2,
                       reads=[bWQROT, bWQZ, bCQN[k][qt]], writes=[bPS[br]])
                cp("dve", Q[0:64, h, :], PS[bq][0:64, :], reads=[bPS[bq]], writes=[bQ[h]])
                tt("dve", T1s[z][R, :], PS[bq][R, :], COS[R, tsl(qt)], ALU.mult, reads=[bPS[bq], bROPE[qt]], writes=[bT1s[z]])
                tt("dve", T2s[z][R, :], PS[br][R, :], SIN[R, tsl(qt)], ALU.mult, reads=[bPS[br], bROPE[qt]], writes=[bT2s[z]])
                tt("dve", Q[R, h, :], T1s[z][R, :], T2s[z][R, :], ALU.add, reads=[bT1s[z], bT2s[z]], writes=[bQ[h]])

            def normalize_a(z, ob):
                cp("dve", ONUM[z][0:64, :], PS[ob][0:64, :], reads=[bPS[ob]], writes=[bONUM[z]])
                act(RDEN[z][64:65, :], PS[ob][64:65, :], AF.Ln, reads=[bPS[ob]], writes=[bRDEN[z]])
                act(RDEN[z][64:65, :], RDEN[z][64:65, :], AF.Exp, reads=[bRDEN[z]], writes=[bRDEN[z]], scale=-1.0)
                cp("dve", RD[z][64:65, :], RDEN[z][64:65, :], reads=[bRDEN[z]], writes=[bRD[z]])
                tt("dve", RD[z][0:1, :], RDEN[z][64:65, :], RD[z][64:65, :], ALU.subtract,
                   reads=[bRDEN[z], bRD[z]], writes=[bRD[z]])

            def normalize_b(z, qt, h):
                mm(PS[7][0:64, :], ONESB[0:65, 0:64], RD[z][0:65, :], True, True,
                   reads=[bRD[z], bCONST], writes=[bPS[7]])
                r0 = (h % 2) * 64
                tt("dve", ATT[r0:r0 + 64, h // 2, tsl(qt)], ONUM[z][0:64, :], PS[7][0:64, :], ALU.mult,
                   reads=[bONUM[z], bPS[7]], writes=[bATT[h][qt]])

            for h in range(NH):
                qproj(3, h)
            for h in range(NH):
                qproj(0, h)
            LA = 3
            NDEF = 14
            cnt = [0, 0]
            NEXTQ = {3: 2, 0: 1}
            for (qa, qb) in ((3, 0), (2, 1)):
                jobs = []
                for h in range(NH):
                    jobs.append((qa, h))
                    jobs.append((qb, h))
                items = [(ji, kc) for ji, (qt, h) in enumerate(jobs) for kc in range(4 * qt + 4)]
                slots = {}
                pending = []

                def SC(i):
                    ji, kc = items[i]
                    qt, h = jobs[ji]
                    Q, bQ = QT[QBUF[qt]], bQT[QBUF[qt]]
                    j = kc - 4 * qt
                    c0 = 128 * j if j > 0 else 0
                    sb = cnt[0] % 4
                    cnt[0] += 1
                    slots[i] = (sb, c0)
                    mm(PS[sb][:, c0:TT], KT[0:96, h, kc * 128:(kc + 1) * 128], Q[0:96, h, c0:TT],
                       True, j < 0, reads=[bKT[h][kc // 4], bQ[h]], writes=[bPS[sb]])
                    if j >= 0:
                        mm(PS[sb][:, c0:c0 + 128], IDENTB, TRIB, False, True, reads=[bCONST], writes=[bPS[sb]])

                def E(i):
                    ji, kc = items[i]
                    qt, h = jobs[ji]
                    nk = 4 * qt + 4
                    sb, c0 = slots.pop(i)
                    z = ji % 2
                    ob = 4 + z
                    p = cnt[1] % 4
                    cnt[1] += 1
                    act(PT[p][:, c0:TT], PS[sb][:, c0:TT], AF.Exp, reads=[bPS[sb]], writes=[bPT[p]], scale=SCALE)
                    mm(PS[ob][0:65, c0:TT], V[:, kc, h, :], PT[p][:, c0:TT], kc == 0, kc == nk - 1,
                       reads=[bV[kc], bPT[p]], writes=[bPS[ob]])
                    if kc == nk - 1:
                        normalize_a(z, ob)
                        if qt in NEXTQ:
                            qproj(NEXTQ[qt], h)
                            if qt == 0 and h == NH - 1:
                                dead = [b for row in bCQN for b in row] + [bWQ, bWQROT] + bROPE
                                sch.dma("pool", WC, win_v[:, :, 0:1536], writes=[bWC] + dead)
                        pending.append((i + NDEF, z, qt, h))

                n = len(items)
                for i in range(n + LA):
                    if i < n:
                        SC(i)
                    if i >= LA:
                        E(i - LA)
                    while pending and pending[0][0] <= i - LA:
                        _, z, qt, h = pending.pop(0)
                        normalize_b(z, qt, h)
                while pending:
                    _, z, qt, h = pending.pop(0)
                    normalize_b(z, qt, h)

            sch.barrier()
            al = Alloc()
            XNT3 = [al.get([KC, TT], BF16) for _ in range(2)]
            SQ = [al.get([TT], BF16) for _ in range(2)]
            RS = al.get([TT], F32)
            CSB = al.get([TT], F32)
            CU = al.get([4, TT + 16], F32)
            T1 = al.get([TT], F32)
            T2 = CSB
            p3_end = max(al.off, c_off - 16384)
            assert p3_end + 16384 <= c_off, p3_end
            CONVIN = view(ARENA_BYTES - 32768, [4, S], BF16)
            bXNT3 = [[Buf() for _ in range(KC)] for _ in range(2)]
            bSQ = [Buf(), Buf()]
            bRS, bCSB, bT1 = Buf(), Buf(), Buf()
            bT2 = bCSB
            bCU = [Buf() for _ in range(4)]
            bCONVIN = [[Buf() for _ in range(NT)] for _ in range(4)]
            memset("dve", CU[:, :, 0:2], 0.0, bCU)
            WGA = view(p3_end, [KC, 1024], BF16)
            al4 = Alloc(c_off + 24576)
            WO = al4.get([KC, D], BF16)
            WCO = al4.get([4, D], BF16)
            WMO = al4.get([4, D], BF16)
            assert al4.off <= ARENA_BYTES - 32768, al4.off
            bWGA, bWGB, bWCO, bWMO, bWO = Buf(), Buf(), Buf(), Buf(), Buf()
            wload(WGA, win_v[:, :, 2208:3232], bWGA)
            wload(WCO, kview(w_conv_out[l]), bWCO)
            wload(WMO, kview(w_mla_out[l]), bWMO)
            wload(WO, kview(w_o[l]), bWO)
            cw = 50 + l * 12
            for t in range(NT):
                XNT, bXNT = XNT3[t % 2], bXNT3[t % 2]
                if t == 0:
                    emit_xn(0, l * 8, XNT3[0], bXNT3[0], SQ, bSQ, RS, bRS, 7)
                for c in range(4):
                    if c == 2 and t + 1 < NT:
                        emit_xn(t + 1, l * 8, XNT3[(t + 1) % 2], bXNT3[(t + 1) % 2], SQ, bSQ, RS, bRS, 7)
                    pb = 3 * (c % 2)
                    bC, bU, bB = pb, pb + 1, pb + 2
                    for (bk, col0) in ((bC, 512), (bU, 1024), (bB, 0)):
                        for k in range(KC):
                            mm(PS[bk][:], WC[:, k, col0 + c * 128: col0 + (c + 1) * 128], XNT[:, k, :],
                               k == 0, k == KC - 1, reads=[bWC, bXNT[k]], writes=[bPS[bk]])
                    cp("act", CSB, PS[bC][:], reads=[bPS[bC]], writes=[bCSB])
                    tt("dve", CU[:, c, 2:TT + 2], CSB, PS[bU][:], ALU.mult, reads=[bCSB, bPS[bU], bCU[c]], writes=[bCU[c]])
                    ts("dve", T1, CU[:, c, 0:TT], vcol(cw + 0 * 4 + c), ALU.mult, reads=[bCU[c], bCONST], writes=[bT1])
                    stt("dve", T2, CU[:, c, 1:TT + 1], vcol(cw + 1 * 4 + c), T1, ALU.mult, ALU.add,
                        reads=[bCU[c], bT1, bCONST], writes=[bT2])
                    stt("dve", T1, CU[:, c, 2:TT + 2], vcol(cw + 2 * 4 + c), T2, ALU.mult, ALU.add,
                        reads=[bCU[c], bT2, bCONST], writes=[bT1])
                    tt("dve", CONVIN[:, c, tsl(t)], T1, PS[bB][:], ALU.mult, reads=[bT1, bPS[bB]],
                       writes=[bCONVIN[c][t]])
                    cp("act", CU[:, c, 0:2], CU[:, c, TT:TT + 2], reads=[bCU[c]], writes=[bCU[c]])

            sch.barrier()
            WGB = view(c_off, [KC, 1024], BF16)
            wload(WGB, win_v[:, :, 3232:4256], bWGB)
            al = Alloc()
            XNTS = [al.get([KC, TT], BF16) for _ in range(2)]
            MERGED = al.get([KC, TT], BF16)
            SG = [al.get([TT], F32) for _ in range(2)]
            SQ = [al.get([TT], BF16) for _ in range(2)]
            RS = al.get([TT], F32)
            assert al.off <= p3_end, al.off
            bXNTS = [[Buf() for _ in range(KC)] for _ in range(2)]
            bMERGED = [Buf() for _ in range(KC)]
            bSG = [Buf(), Buf()]
            bSQ = [Buf(), Buf()]
            bRS = Buf()
            emit_xn(0, l * 8, XNTS[0], bXNTS[0], SQ, bSQ, RS, bRS, 7)
            for t in range(NT):
                XNT, bXNT = XNTS[t % 2], bXNTS[t % 2]
                for j in range(KC):
                    if j == 4 and t + 1 < NT:
                        emit_xn(t + 1, l * 8, XNTS[(t + 1) % 2], bXNTS[(t + 1) % 2], SQ, bSQ, RS, bRS, 7)
                    pb = 0 if j % 2 == 0 else 3
                    b_gc, b_gm, b_yc = pb, pb + 1, pb + 2
                    b_ym = 6
                    js = slice(j * 128, (j + 1) * 128)
                    for k in range(KC):
                        mm(PS[b_gc][:], WGA[:, k, j * 128:(j + 1) * 128], XNT[:, k, :], k == 0, k == KC - 1,
                           reads=[bWGA, bXNT[k]], writes=[bPS[b_gc]])
                    for k in range(KC):
                        mm(PS[b_gm][:], WGB[:, k, j * 128:(j + 1) * 128], XNT[:, k, :], k == 0, k == KC - 1,
                           reads=[bWGB, bXNT[k]], writes=[bPS[b_gm]])
                    for k in range(4):
                        mm(PS[b_yc][:], WCO[:, k, js], CONVIN[:, k, tsl(t)], k == 0, k == 3,
                           reads=[bWCO, bCONVIN[k][t]], writes=[bPS[b_yc]])
                    for k in range(4):
                        mm(PS[b_ym][:], WMO[:, k, js], ATT[:, k, tsl(t)], k == 0, k == 3,
                           reads=[bWMO, bATT[2 * k][t], bATT[2 * k + 1][t]], writes=[bPS[b_ym]])
                    act(SG[0], PS[b_gc][:], AF.Sigmoid, reads=[bPS[b_gc]], writes=[bSG[0]])
                    act(SG[1], PS[b_gm][:], AF.Sigmoid, reads=[bPS[b_gm]], writes=[bSG[1]])
                    tt("dve", SG[0], SG[0], PS[b_yc][:], ALU.mult, reads=[bSG[0], bPS[b_yc]], writes=[bSG[0]])
                    tt("dve", SG[1], SG[1], PS[b_ym][:], ALU.mult, reads=[bSG[1], bPS[b_ym]], writes=[bSG[1]])
                    tt("dve", MERGED[:, j, :], SG[0], SG[1], ALU.add, reads=[bSG[0], bSG[1]], writes=[bMERGED[j]])
                for j in range(KC):
                    bk = 0 if j % 2 == 0 else 3
                    for k in range(KC):
                        mm(PS[bk][:], WO[:, k, j * 128:(j + 1) * 128], MERGED[:, k, :], k == 0, k == KC - 1,
                           reads=[bWO, bMERGED[k]], writes=[bPS[bk]])
                    tt("dve", XRES[:, j, tsl(t)], XRES[:, j, tsl(t)], PS[bk][:], ALU.add,
                       reads=[bX[j][t], bPS[bk]], writes=[bX[j][t]])

        def ffn(l):
            moe = (l % 2 == 1)
            i = l // 2
            sch.barrier()
            if (l + 1) in layers and not moe and do_mixer:
                w1_load(l + 1)
            al = Alloc()
            HN = al.get([KC, S], BF16)
            HB = [al.get([max(GSZ), S], BF16) for _ in range(2)]
            WGU = [(al.get([KC, 128], BF16), al.get([KC, 128], BF16)) for _ in range(3)]
            WDS = [al.get([D], BF16) for _ in range(8)]
            SG = [al.get([TT], F32) for _ in range(2)]
            _tf = al.get([TT], F32)
            TF = [_tf, _tf]
            SQ = [al.get([TT], BF16) for _ in range(2)]
            RS = al.get([TT], F32)
            if moe:
                CBC = al.get([S], F32)
                COMBT = al.get([S], F32)
                ROUT = al.get([KC, NE], F32)
                GR = al.get([KC, NE], F32)
                LG = al.get([8, 4 * NE], F32)
                SM = al.get([8, 8], F32)
            assert al.off <= ARENA_BYTES, al.off
            bHN = [[Buf() for _ in range(NT)] for _ in range(KC)]
            bHB = [[[Buf() for _ in range(NT)] for _ in range(max(GSZ))] for _ in range(2)]
            bWGU = [Buf() for _ in range(3)]
            bWDS = [Buf() for _ in range(8)]
            _btf = Buf()
            bSG, bTF, bSQ = [Buf(), Buf()], [_btf, _btf], [Buf(), Buf()]
            bRS, bCBC, bCOMBT, bGR, bLG = Buf(), [Buf() for _ in range(NT)], [Buf() for _ in range(NT)], Buf(), Buf()

            gcol = 16 + l * 8
            if moe:
                sch.dma("sp", ROUT, router[i].rearrange("(c p) e -> p c e", p=128), writes=[bGR])
                tt("dve", GR, ROUT, VECS[:, gcol:gcol + 8].unsqueeze(2).to_broadcast([128, KC, NE]), ALU.mult,
                   reads=[bGR, bCONST], writes=[bGR])
            def route_chain(t):
                pb = t

                def v3(i):
                    return LG[:, i, :].rearrange("p (b e) -> p b e", e=NE)

                def bc(ap2):
                    return ap2.unsqueeze(2).to_broadcast([128, 4, NE])
                lg, lg2, e1, e2, cmb = v3(0), v3(1), v3(2), v3(3), v3(4)
                rstd, m1, m2, dm, w1, w2 = (SM[:, 0, 0:4], SM[:, 1, 0:4], SM[:, 2, 0:4], SM[:, 3, 0:4],
                                            SM[:, 4, 0:4], SM[:, 5, 0:4])
                cp("dve", rstd, PS[pb][:, 64:68], reads=[bPS[pb]], writes=[bLG])
                tt("dve", lg, PS[pb][:, 0:4 * NE].rearrange("p (b e) -> p b e", e=NE), bc(rstd), ALU.mult,
                   reads=[bPS[pb], bLG], writes=[bLG])
                sch.op("dve", lambda h, o=m1, i_=lg: h.reduce_max(out=o, in_=i_, axis=AX.X), reads=[bLG], writes=[bLG])
                tt("dve", e1, lg, bc(m1), ALU.is_ge, reads=[bLG], writes=[bLG])
                stt("dve", lg2, e1, -1e30, lg, ALU.mult, ALU.add, reads=[bLG], writes=[bLG])
                sch.op("dve", lambda h, o=m2, i_=lg2: h.reduce_max(out=o, in_=i_, axis=AX.X), reads=[bLG], writes=[bLG])
                tt("dve", e2, lg2, bc(m2), ALU.is_ge, reads=[bLG], writes=[bLG])
                tt("dve", dm, m2, m1, ALU.subtract, reads=[bLG], writes=[bLG])
                act(w2, dm, AF.Sigmoid, reads=[bLG], writes=[bLG])
                act(w1, dm, AF.Sigmoid, reads=[bLG], writes=[bLG], scale=-1.0)
                tt("dve", cmb, e1, bc(w1), ALU.mult, reads=[bLG], writes=[bLG])
                tt("dve", e2, e2, bc(w2), ALU.mult, reads=[bLG], writes=[bLG])
                tt("dve", cmb, cmb, e2, ALU.add, reads=[bLG], writes=[bLG])
                for bi in range(4):
                    mm(PS[6][0:NE, bi * 128:(bi + 1) * 128], LG[:, 4, bi * NE:(bi + 1) * NE], IDENTF, True, True,
                       reads=[bLG, bCONST], writes=[bPS[6]])
                cp("act", COMBT[0:NE, tsl(t)], PS[6][0:NE, :], reads=[bPS[6]], writes=[bCOMBT[t]])

            for t in range(NT):
                rms_scale([XRES[:, c, tsl(t)] for c in range(KC)], [bX[c][t] for c in range(KC)], D,
                          SQ, bSQ, RS, bRS, 7)
                for c in range(KC):
                    stt("dve", HN[:, c, tsl(t)], XRES[:, c, tsl(t)], vcol(gcol + c), RS, ALU.mult, ALU.mult,
                        reads=[bX[c][t], bRS, bCONST], writes=[bHN[c][t]])
                if moe:
                    for bi in range(4):
                        tok = slice(t * TT + bi * 128, t * TT + (bi + 1) * 128)
                        for k in range(KC):
                            mm(PS[t][:, bi * NE:(bi + 1) * NE], XRES[:, k, tok], GR[:, k, :], k == 0, k == KC - 1,
                               reads=[bX[k][t], bGR], writes=[bPS[t]])
                        mm(PS[t][:, 64 + bi:65 + bi], RS[0:1, bi * 128:(bi + 1) * 128], ONESF[0:1, 0:1], True, True,
                           reads=[bRS, bCONST], writes=[bPS[t]])
                    if t > 0:
                        route_chain(t - 1)
            if moe:
                route_chain(NT - 1)

            if moe:
                experts = [(w_gate_e[i, e], w_up_e[i, e], w_down_e[i, e], e) for e in range(NE)]
            else:
                experts = [(w_gate[i][:, h * DFE:(h + 1) * DFE], w_up[i][:, h * DFE:(h + 1) * DFE],
                            w_down[i][h * DFE:(h + 1) * DFE, :], None) for h in range(2)]
            gi = 0
            wi = 0
            di = 0
            ev = 0
            for (wg_d, wu_d, wd_d, e) in experts:
                wg_v = kview(wg_d)
                wu_v = kview(wu_d)
                if moe:
                    for t in range(NT):
                        mm(PS[6][:], SELF[0:NE, e, :], COMBT[0:NE, tsl(t)], True, True,
                           reads=[bCOMBT[t], bCONST], writes=[bPS[6]])
                        cp("act", CBC[:, tsl(t)], PS[6][:], reads=[bPS[6]], writes=[bCBC[t]])
                c0 = 0
                for G in GSZ:
                    hb = gi % 2
                    gi += 1
                    dslots = []
                    for cc in range(G):
                        f = c0 + cc
                        ws = wi % 3
                        wi += 1
                        ds_ = di % 8
                        di += 1
                        dslots.append(ds_)
                        wload(WGU[ws][0], wg_v[:, :, f * 128:(f + 1) * 128], bWGU[ws])
                        wload(WGU[ws][1], wu_v[:, :, f * 128:(f + 1) * 128], bWGU[ws])
                        wload(WDS[ds_], wd_d[f * 128:(f + 1) * 128, :], bWDS[ds_])
                        for t in range(NT):
                            pg = (ev % 2) * 2
                            pu = pg + 1
                            sg = ev % 2
                            ev += 1
                            for k in range(KC):
                                mm(PS[pg][:], WGU[ws][0][:, k, :], HN[:, k, tsl(t)], k == 0, k == KC - 1,
                                   reads=[bWGU[ws], bHN[k][t]], writes=[bPS[pg]])
                            for k in range(KC):
                                mm(PS[pu][:], WGU[ws][1][:, k, :], HN[:, k, tsl(t)], k == 0, k == KC - 1,
                                   reads=[bWGU[ws], bHN[k][t]], writes=[bPS[pu]])
                            act(SG[sg], PS[pg][:], AF.Silu, reads=[bPS[pg]], writes=[bSG[sg]])
                            if moe:
                                tt("dve", TF[sg], SG[sg], PS[pu][:], ALU.mult, reads=[bSG[sg], bPS[pu]], writes=[bTF[sg]])
                                tt("dve", HB[hb][:, cc, tsl(t)], TF[sg], CBC[:, tsl(t)], ALU.mult,
                                   reads=[bTF[sg], bCBC[t]], writes=[bHB[hb][cc][t]])
                            else:
                                tt("dve", HB[hb][:, cc, tsl(t)], SG[sg], PS[pu][:], ALU.mult,
                                   reads=[bSG[sg], bPS[pu]], writes=[bHB[hb][cc][t]])
                    dj = 0
                    for j in range(KC):
                        for t in range(NT):
                            pd = 4 + (dj % 2)
                            dj += 1
                            for cc in range(G):
                                mm(PS[pd][:], WDS[dslots[cc]][:, j * 128:(j + 1) * 128], HB[hb][:, cc, tsl(t)],
                                   cc == 0, cc == G - 1, reads=[bWDS[dslots[cc]], bHB[hb][cc][t]], writes=[bPS[pd]])
                            tt("dve", XRES[:, j, tsl(t)], XRES[:, j, tsl(t)], PS[pd][:], ALU.add,
                               reads=[bX[j][t], bPS[pd]], writes=[bX[j][t]])
                    c0 += G

        if not do_mixer:
            load_x_tiles(1, NT)
            x_loaded[0] = True
        for l in layers:
            if do_mixer:
                mixer(l)
            if do_ffn:
                ffn(l)

        sch.barrier()
        al = Alloc()
        OUTS = [al.get([KC, TT], F32) for _ in range(2)]
        SQ = [al.get([TT], BF16) for _ in range(2)]
        RS = al.get([TT], F32)
        bOUT = [[Buf() for _ in range(KC)] for _ in range(2)]
        bSQ = [Buf(), Buf()]
        bRS = Buf()
        for t in range(NT):
            o = t % 2
            if last:
                rms_scale([XRES[:, c, tsl(t)] for c in range(KC)], [bX[c][t] for c in range(KC)], D,
                          SQ, bSQ, RS, bRS, 7)
                for c in range(KC):
                    stt("dve", OUTS[o][:, c, :], XRES[:, c, tsl(t)], vcol(32 + c), RS, ALU.mult, ALU.mult,
                        reads=[bX[c][t], bRS, bCONST], writes=[bOUT[o][c]])
                    sch.dma("sp", outT[c * 128:(c + 1) * 128, tsl(t)], OUTS[o][:, c, :], reads=[bOUT[o][c]])
            else:
                for c in range(KC):
                    sch.dma("sp", outT[c * 128:(c + 1) * 128, tsl(t)], XRES[:, c, tsl(t)], reads=[bX[c][t]])
        sch.final_wait("sp")
        sch.finalize()

        @block.tensor
        def _(h):
            sch.emit("pe", h)

        @block.scalar
        def _(h):
            sch.emit("act", h)

        @block.vector
        def _(h):
            sch.emit("dve", h)

        @block.gpsimd
        def _(h):
            sch.emit("pool", h)

        @block.sync
        def _(h):
            sch.emit("sp", h)

    return nc


def _host_consts():
    cf = np.zeros((128, 128 + 1024), np.float32)
    cf[:, 0:128] = np.eye(128, dtype=np.float32)
    for e in range(8):
        cf[e, 128 + e * 128: 128 + (e + 1) * 128] = 1.0
    cb = np.zeros((128, 256), np.float32)
    cb[:, 0:128] = np.eye(128, dtype=np.float32)
    k = np.arange(128)[:, None]
    q = np.arange(128)[None, :]
    cb[:, 128:256] = np.where(q >= k, 0.0, -30000.0).astype(np.float32)
    return cf, cb


def _shared_inputs(inp):
    f = lambda a: np.ascontiguousarray(np.asarray(a, dtype=np.float32))
    vecs = np.zeros((128, NV), np.float32)

    def put(col, v):
        v = np.asarray(v, np.float32).reshape(-1, 128)
        vecs[:, col:col + v.shape[0]] = v.T

    for l in range(2):
        put(0 + l * 8, inp["attn_norm"][l])
        put(16 + l * 8, inp["ffn_norm"][l])
        put(40 + l * 3, inp["q_norm"][l])
        put(46 + l * 2, inp["kv_norm"][l])
        for k in range(3):
            put(50 + l * 12 + k * 4, inp["conv_w"][l, k])
    put(32, inp["final_norm"])
    inv_freq = (10000.0 ** (-np.arange(0, 32, 2, dtype=np.float32) / np.float32(32))).astype(np.float32)
    vecs[64:96, 74] = np.concatenate([inv_freq, inv_freq])
    cf, cb = _host_consts()
    w_uq = f(inp["w_uq"])
    w_in = f(inp["w_in"])
    wq4 = w_uq.reshape(2, 384, 8, 96)
    wq_rot = np.concatenate([wq4[..., 80:96], wq4[..., 64:80]], axis=-1).reshape(2, 384, 256)
    wkr = w_in[:, :, 2176:2208]
    wkr_rot = np.concatenate([w_in[:, :, 2192:2208], w_in[:, :, 2176:2192]], axis=-1)
    sh = {
        "vecs": vecs, "cf": cf, "cb": cb, "w_in": w_in, "w_conv_out": f(inp["w_conv_out"]),
        "w_uq": w_uq, "wq_rot": f(wq_rot), "wkr": f(wkr), "wkr_rot": f(wkr_rot),
        "w_ukv": f(inp["w_ukv"]), "w_mla_out": f(inp["w_mla_out"]), "w_o": f(inp["w_o"]),
        "w_gate": f(inp["w_gate"]), "w_up": f(inp["w_up"]), "w_down": f(inp["w_down"]),
        "router": f(inp["router"]), "w_gate_e": f(inp["w_gate_e"]), "w_up_e": f(inp["w_up_e"]),
        "w_down_e": f(inp["w_down_e"]),
    }
    return sh


_NC_CACHE = {}


def run_layers(inp, xT_list, layers, last):
    key = (tuple(layers), last)
    if key not in _NC_CACHE:
        _NC_CACHE[key] = build_nc(layers, last=last)
    nc = _NC_CACHE[key]
    sh = _shared_inputs(inp)
    pos = np.asarray(inp["positions"]).astype(np.int32)
    in_maps = []
    for b in range(8):
        m = dict(sh)
        m["xT"] = np.ascontiguousarray(xT_list[b])
        m["posrep"] = np.ascontiguousarray(np.broadcast_to(pos[b][None, :], (32, S)))
        in_maps.append(m)
    res = run_bass_kernel_spmd(nc, in_maps, core_ids=list(range(8)))
    return [r["outT"] for r in res.results]


def kernel(**inputs):
    x = np.asarray(inputs["x"], dtype=np.float32)
    xT = [np.ascontiguousarray(x[b].T) for b in range(8)]
    outs = run_layers(inputs, xT, [0, 1], True)
    return np.stack([np.ascontiguousarray(o.T) for o in outs], axis=0).astype(np.float32)
```

```python
import math
import numpy as np
import concourse.bass as bass
import concourse.mybir as mybir
from concourse.bass_utils import run_bass_kernel_spmd

F32 = mybir.dt.float32
BF16 = mybir.dt.bfloat16
I32 = mybir.dt.int32
ALU = mybir.AluOpType
AF = mybir.ActivationFunctionType
AX = mybir.AxisListType

D = 1024
S = 2048
KC = 8
TT = 512
NT = 4
NH = 8
DIN = 4256
DFF = 2816
DFE = 1408
NE = 8
EPS = 1e-6
NV = 80
SCALE = 1.0 / math.sqrt(96.0)
ARENA_BYTES = 140800
GSZ = (6, 5)
SAME_ENGINE_SYNC = True


class Tok:
    __slots__ = ("eng", "idx", "sem", "val")

    def __init__(self, eng=None, idx=None, sem=None, val=None):
        self.eng, self.idx, self.sem, self.val = eng, idx, sem, val


class Buf:
    __slots__ = ("w", "r", "name")

    def __init__(self, name=""):
        self.w = None
        self.r = []
        self.name = name


class Eng:
    def __init__(self, name, sem, dma_sems):
        self.name = name
        self.sem = sem
        self.ops = []
        self.waited = {}
        self.last_real = -1
        self.dma_sems = dma_sems
        self.dma_cnt = [0] * len(dma_sems)
        self.dma_rr = 0


class Sched:
    def __init__(self, sems):
        it = iter(sems)
        self.e = {}
        for name, nd in (("pe", 0), ("act", 0), ("dve", 0), ("pool", 24), ("sp", 12)):
            s = next(it)
            self.e[name] = Eng(name, s, [next(it) for _ in range(nd)])

    def _waits(self, e, toks):
        best = {}
        for t in toks:
            if t is None:
                continue
            if t.eng is None:
                k = ("d", id(t.sem))
                if e.waited.get(k, 0) >= t.val:
                    continue
                if k not in best or best[k].val < t.val:
                    best[k] = t
            else:
                if t.eng is e and (e.name == "pe" or not SAME_ENGINE_SYNC):
                    continue
                k = ("e", t.eng.name)
                if e.waited.get(k, -1) >= t.idx:
                    continue
                if k not in best or best[k].idx < t.idx:
                    best[k] = t
        out = []
        for k, t in best.items():
            if t.eng is None:
                e.waited[k] = t.val
            else:
                e.waited[k] = t.idx
                t.eng.ops[t.idx][2] = True
            out.append(t)
        return out

    @staticmethod
    def _deps(reads, writes):
        toks = []
        for b in reads:
            toks.append(b.w)
        for b in writes:
            toks.append(b.w)
            toks.extend(b.r)
        return toks

    @staticmethod
    def _mark(tok, reads, writes):
        for b in reads:
            b.r.append(tok)
        for b in writes:
            b.w = tok
            b.r = []

    def op(self, eng, fn, reads=(), writes=()):
        e = self.e[eng]
        waits = self._waits(e, self._deps(reads, writes))
        idx = len(e.ops)
        e.ops.append([waits, fn, False])
        e.last_real = idx
        tok = Tok(eng=e, idx=idx)
        self._mark(tok, reads, writes)
        return tok

    def dma(self, eng, out, in_, reads=(), writes=()):
        e = self.e[eng]
        slot = e.dma_rr % len(e.dma_sems)
        e.dma_rr += 1
        sem = e.dma_sems[slot]
        toks = self._deps(reads, writes)
        if e.dma_cnt[slot] > 0:
            toks.append(Tok(sem=sem, val=e.dma_cnt[slot]))
        waits = self._waits(e, toks)
        e.dma_cnt[slot] += 16
        tok = Tok(sem=sem, val=e.dma_cnt[slot])
        e.ops.append([waits, (lambda h, o=out, i=in_: h.dma_start(out=o, in_=i)), (sem, 16)])
        self._mark(tok, reads, writes)
        return tok

    def _all_toks(self, skip=None):
        toks = []
        for f in self.e.values():
            if f is not skip and f.last_real >= 0:
                toks.append(Tok(eng=f, idx=f.last_real))
            for s, c in zip(f.dma_sems, f.dma_cnt):
                if c > 0:
                    toks.append(Tok(sem=s, val=c))
        return toks

    def barrier(self):
        for e in self.e.values():
            own = (e.name == "pe" or not SAME_ENGINE_SYNC)
            waits = self._waits(e, self._all_toks(skip=e if own else None))
            if waits:
                e.ops.append([waits, None, False])

    def final_wait(self, eng):
        e = self.e[eng]
        waits = self._waits(e, self._all_toks(skip=e))
        if waits:
            e.ops.append([waits, None, False])

    def finalize(self):
        self.cnt = {}
        for name, e in self.e.items():
            c = 0
            arr = []
            for (_, fn, sig) in e.ops:
                if sig is True:
                    c += 1
                arr.append(c)
            self.cnt[name] = arr

    def emit(self, name, h):
        for waits, fn, sig in self.e[name].ops:
            for t in waits:
                if t.eng is None:
                    h.wait_ge(t.sem, t.val)
                else:
                    h.wait_ge(t.eng.sem, self.cnt[t.eng.name][t.idx])
            if fn is not None:
                ins = fn(h)
                if sig is True:
                    ins.then_inc(self.e[name].sem, 1)
                elif sig:
                    ins.then_inc(sig[0], sig[1])


def build_nc(layers, last=True, do_mixer=True, do_ffn=True):
    nc = bass.Bass("TRN2", target_bir_lowering=False)

    def din(name, shape, dt=F32):
        return nc.dram_tensor(name, list(shape), dt, kind="ExternalInput").ap()

    xT = din("xT", [D, S])
    posrep = din("posrep", [32, S], I32)
    vecs_d = din("vecs", [128, NV])
    cf_d = din("cf", [128, 128 + 1024])
    cb_d = din("cb", [128, 256])
    w_in = din("w_in", [2, D, DIN])
    w_conv_out = din("w_conv_out", [2, 512, D])
    w_uq = din("w_uq", [2, 384, 768])
    wq_rot = din("wq_rot", [2, 384, 256])
    wkr = din("wkr", [2, D, 32])
    wkr_rot = din("wkr_rot", [2, D, 32])
    w_ukv = din("w_ukv", [2, 256, 1024])
    w_mla_out = din("w_mla_out", [2, 512, D])
    w_o = din("w_o", [2, D, D])
    w_gate = din("w_gate", [1, D, DFF])
    w_up = din("w_up", [1, D, DFF])
    w_down = din("w_down", [1, DFF, D])
    router = din("router", [1, D, NE])
    w_gate_e = din("w_gate_e", [1, NE, D, DFE])
    w_up_e = din("w_up_e", [1, NE, D, DFE])
    w_down_e = din("w_down_e", [1, NE, DFE, D])
    outT = nc.dram_tensor("outT", [D, S], F32, kind="ExternalOutput").ap()

    import contextlib
    es = contextlib.ExitStack()
    with es:
        XRES = es.enter_context(nc.sbuf_tensor("XRES", [128, KC, S], F32))
        VECS = es.enter_context(nc.sbuf_tensor("VECS", [128, NV], F32))
        CF = es.enter_context(nc.sbuf_tensor("CF", [128, 128 + 1024], F32))
        CB = es.enter_context(nc.sbuf_tensor("CB", [128, 256], BF16))
        ONESB = es.enter_context(nc.sbuf_tensor("ONESB", [128, 128], BF16))
        ONESF = es.enter_context(nc.sbuf_tensor("ONESF", [128, 128], F32))
        ARENA = es.enter_context(nc.sbuf_tensor("ARENA", [128, ARENA_BYTES // 2], BF16))
        PS = [es.enter_context(nc.psum_tensor(f"PS{i}", [128, 512], F32)) for i in range(8)]
        sems = [es.enter_context(nc.semaphore(f"sem{i}")) for i in range(5 + 24 + 12)]
        block = es.enter_context(nc.Block())

        sch = Sched(sems)
        IDENTF = CF[:, 0:128]
        SELF = CF[:, 128:128 + 1024].rearrange("p (e m) -> p e m", e=8)
        IDENTB = CB[:, 0:128]
        TRIB = CB[:, 128:256]

        def view(off, shape, dt):
            n = int(np.prod(shape))
            esz = 4 if dt in (F32, I32) else 2
            assert off % 4 == 0 and off + n * esz <= ARENA_BYTES, (off, shape, ARENA_BYTES)
            a = ARENA[:, off // 2: off // 2 + n * esz // 2]
            if dt != BF16:
                a = a.bitcast(dt)
            if len(shape) == 2:
                a = a.rearrange("p (a b) -> p a b", a=shape[0])
            elif len(shape) == 3:
                a = a.rearrange("p (a b c) -> p a b c", a=shape[0], b=shape[1])
            return a

        class Alloc:
            def __init__(self, start=0):
                self.off = start

            def get(self, shape, dt):
                n = int(np.prod(shape))
                esz = 4 if dt in (F32, I32) else 2
                v = view(self.off, shape, dt)
                self.off += (n * esz + 63) // 64 * 64
                return v

        def mm(out, lhsT, rhs, start, stop, reads, writes):
            sch.op("pe", lambda h: h.matmul(out, lhsT=lhsT, rhs=rhs, start=start, stop=stop,
                                            skip_group_check=True),
                   reads=reads, writes=writes)

        def act(out, in_, func, reads, writes, scale=1.0, bias=0.0):
            sch.op("act", lambda h: h.activation(out=out, in_=in_, func=func, bias=bias, scale=scale),
                   reads=reads, writes=writes)

        def tt(eng, out, in0, in1, op, reads, writes):
            sch.op(eng, lambda h: h.tensor_tensor(out=out, in0=in0, in1=in1, op=op),
                   reads=reads, writes=writes)

        def stt(eng, out, in0, scalar, in1, op0, op1, reads, writes):
            sch.op(eng, lambda h: h.scalar_tensor_tensor(out=out, in0=in0, scalar=scalar, in1=in1,
                                                          op0=op0, op1=op1),
                   reads=reads, writes=writes)

        def ts(eng, out, in0, s1, op0, reads, writes, s2=None, op1=None):
            if op1 is None:
                sch.op(eng, lambda h: h.tensor_scalar(out=out, in0=in0, scalar1=s1, scalar2=None, op0=op0),
                       reads=reads, writes=writes)
            else:
                sch.op(eng, lambda h: h.tensor_scalar(out=out, in0=in0, scalar1=s1, scalar2=s2,
                                                      op0=op0, op1=op1),
                       reads=reads, writes=writes)

        def cp(eng, out, in_, reads, writes):
            if eng == "act":
                sch.op("act", lambda h: h.copy(out=out, in_=in_), reads=reads, writes=writes)
            else:
                sch.op(eng, lambda h: h.tensor_copy(out=out, in_=in_), reads=reads, writes=writes)

        def memset(eng, ap, val, writes):
            sch.op(eng, lambda h: h.memset(ap, val), reads=(), writes=writes)

        bX = [[Buf(f"x{c}_{t}") for t in range(NT)] for c in range(KC)]
        bPS = [Buf(f"ps{i}") for i in range(8)]
        bCONST = Buf("const")

        def tsl(t):
            return slice(t * TT, (t + 1) * TT)

        sch.dma("sp", VECS[:], vecs_d[:], writes=[bCONST])
        sch.dma("sp", CF[:], cf_d[:], writes=[bCONST])
        sch.dma("pool", CB[:], cb_d[:], writes=[bCONST])
        memset("dve", ONESB[:], 1.0, [bCONST])
        memset("dve", ONESF[:], 1.0, [bCONST])
        xT_v = xT.rearrange("(c p) s -> p c s", p=128)
        x_loaded = [False]

        def load_x_tiles(t0, t1):
            for t in range(t0, t1):
                sch.dma("sp", XRES[:, :, tsl(t)], xT_v[:, :, tsl(t)], writes=[bX[c][t] for c in range(KC)],
                        reads=([bX[0][t - 1]] if t > 0 else []))

        load_x_tiles(0, 1)

        def vcol(i):
            return VECS[:, i:i + 1]

        def rms_scale(srcs, src_bufs, nfeat, SQ, bSQ, RS, bRS, bank):
            n = len(srcs)
            for i, (s, sb) in enumerate(zip(srcs, src_bufs)):
                q = i % 2
                act(SQ[q], s, AF.Square, reads=[sb], writes=[bSQ[q]])
                mm(PS[bank][:], ONESB[:], SQ[q], i == 0, i == n - 1,
                   reads=[bSQ[q], bCONST], writes=[bPS[bank]])
            act(RS, PS[bank][:], AF.Ln, reads=[bPS[bank]], writes=[bRS], scale=1.0 / nfeat, bias=EPS)
            act(RS, RS, AF.Exp, reads=[bRS], writes=[bRS], scale=-0.5)

        def emit_xn(t, gcol0, XNT, bXNT, SQ, bSQ, RS, bRS, bank):
            rms_scale([XRES[:, c, tsl(t)] for c in range(KC)], [bX[c][t] for c in range(KC)], D,
                      SQ, bSQ, RS, bRS, bank)
            for c in range(KC):
                stt("dve", XNT[:, c, :], XRES[:, c, tsl(t)], vcol(gcol0 + c), RS, ALU.mult, ALU.mult,
                    reads=[bX[c][t], bRS, bCONST], writes=[bXNT[c]])

        def wload(dst, src, buf):
            sch.dma("pool", dst, src, writes=[buf])

        def kview(w2d):
            return w2d.rearrange("(c p) n -> p c n", p=128)

        W1_OFF = ARENA_BYTES - 17920
        w1state = {}

        def w1_load(l):
            al = Alloc(W1_OFF)
            WINB = al.get([KC, 672], BF16)
            WKR = al.get([KC, 96], BF16)
            WKRROT = al.get([KC, 96], BF16)
            WKVK = al.get([2, 512], BF16)
            WKVV = al.get([2, 512], BF16)
            assert al.off <= ARENA_BYTES
            bWINB, bWKR, bWKRROT, bWKVK, bWKVV = Buf(), Buf(), Buf(), Buf(), Buf()
            bWKZ = Buf()
            win_v = kview(w_in[l])
            memset("dve", WKR[:, :, 0:64], 0.0, [bWKZ])
            memset("dve", WKRROT[:, :, 0:64], 0.0, [bWKZ])
            wload(WKR[:, :, 64:96], kview(wkr[l]), bWKR)
            wload(WKRROT[:, :, 64:96], kview(wkr_rot[l]), bWKRROT)
            wload(WINB, win_v[:, :, 1536:2208], bWINB)
            ukv = w_ukv[l].rearrange("(c p) (h e) -> p c h e", p=128, e=128)
            for c in range(2):
                wload(WKVK[:, c, :].rearrange("p (h e) -> p h e", e=64), ukv[:, c, :, 0:64], bWKVK)
                wload(WKVV[:, c, :].rearrange("p (h e) -> p h e", e=64), ukv[:, c, :, 64:128], bWKVV)
            w1state[l] = (WINB, WKR, WKRROT, WKVK, WKVV, bWINB, bWKR, bWKRROT, bWKVK, bWKVV, bWKZ)

        def mixer(l):
            sch.barrier()
            al = Alloc()
            KT = al.get([NH, S], BF16)
            V = al.get([16, NH, 65], BF16)
            c_off = al.off
            CQN = al.get([3, S], BF16)
            WQ = al.get([3, 768], BF16)
            WQROT = al.get([3, NH, 96], BF16)
            COS = al.get([S], F32)
            SIN = al.get([S], F32)
            p12 = al.off
            XNTS = [al.get([KC, TT], BF16) for _ in range(2)]
            SQ = [al.get([TT], BF16) for _ in range(2)]
            RS = al.get([TT], F32)
            RS2 = al.get([TT], F32)
            SQ2 = SQ
            CF32 = al.get([5, TT], F32)
            CKVN = al.get([2, TT], BF16)
            T1 = CF32[:, 3, :]
            T2 = CF32[:, 0, :]
            RT0 = CF32[:, 1, :]
            RT1 = CF32[:, 2, :]
            assert al.off <= W1_OFF, al.off

            bKT = [[Buf() for _ in range(NT)] for _ in range(NH)]
            bV = [Buf() for _ in range(16)]
            bCQN = [[Buf() for _ in range(NT)] for _ in range(3)]
            bWQ, bWQROT = Buf(), Buf()
            bROPE = [Buf() for _ in range(NT)]
            bXNTS = [[Buf() for _ in range(KC)] for _ in range(2)]
            bSQ = [Buf(), Buf()]
            bSQ2 = bSQ
            bRS, bRS2 = Buf(), Buf()
            bCF32 = [Buf() for _ in range(5)]
            bT1, bT2, bRT0, bRT1 = bCF32[3], bCF32[0], bCF32[1], bCF32[2]
            bCKVN = [Buf(), Buf()]

            sch.dma("pool", RT0[slice(64, 96), :], posrep[:, tsl(0)], writes=[bRT0])
            if l not in w1state:
                w1_load(l)
            (WINB, WKR, WKRROT, WKVK, WKVV, bWINB, bWKR, bWKRROT, bWKVK, bWKVV, bWKZ) = w1state[l]
            win_v = kview(w_in[l])
            bWQZ = Buf()
            memset("dve", WQROT[:, :, :, 0:64], 0.0, [bWQZ])
            wload(WQ, kview(w_uq[l]), bWQ)
            for c in range(3):
                wload(WQROT[:, c, :, 64:96], kview(wq_rot[l])[:, c, :].rearrange("p (h e) -> p h e", e=32), bWQROT)

            R = slice(64, 96)

            def rope_pass(t):
                if t > 0:
                    sch.dma("pool", RT0[R, :], posrep[:, tsl(t)], writes=[bRT0])
                ts("dve", RT0[R, :], RT0[R, :], VECS[R, 74:75], ALU.mult, reads=[bRT0, bCONST], writes=[bRT0])
                for tab, shift in ((SIN, 0.0), (COS, math.pi / 2)):
                    tv = tab[R, tsl(t)]
                    ts("dve", tv, RT0[R, :], shift, ALU.add, reads=[bRT0], writes=[bROPE[t]],
                       s2=1.0 / (2 * math.pi), op1=ALU.mult)
                    ts("dve", RT1[R, :], tv, 12582912.0, ALU.add, reads=[bROPE[t]], writes=[bRT1])
                    ts("dve", tv, RT1[R, :], -12582912.0, ALU.add, reads=[bRT1], writes=[bROPE[t]])
                    stt("dve", tv, tv, -2 * math.pi, RT0[R, :], ALU.mult, ALU.add,
                        reads=[bROPE[t], bRT0], writes=[bROPE[t]])
                    ts("dve", tv, tv, shift, ALU.add, reads=[bROPE[t]], writes=[bROPE[t]],
                       s2=3.1415925, op1=ALU.min)
                    ts("dve", tv, tv, -3.1415925, ALU.max, reads=[bROPE[t]], writes=[bROPE[t]])
                    act(tv, tv, AF.Sin, reads=[bROPE[t]], writes=[bROPE[t]])

            memset("dve", V.rearrange("p a h e -> p (a h) e")[:, :, 64:65], 1.0, bV)

            qg = 40 + l * 3
            kg = 46 + l * 2
            rope_pass(0)
            if not x_loaded[0]:
                load_x_tiles(1, NT)
                x_loaded[0] = True
            emit_xn(0, l * 8, XNTS[0], bXNTS[0], SQ2, bSQ2, RS2, bRS2, 7)
            for t in range(NT):
                XNT, bXNT = XNTS[t % 2], bXNTS[t % 2]
                for oc in range(5):
                    bk = oc % 4
                    for k in range(KC):
                        mm(PS[bk][:], WINB[:, k, oc * 128:(oc + 1) * 128], XNT[:, k, :], k == 0, k == KC - 1,
                           reads=[bWINB, bXNT[k]], writes=[bPS[bk]])
                    cp("dve", CF32[:, oc, :], PS[bk][:], reads=[bPS[bk]], writes=[bCF32[oc]])
                if t == 0:
                    ts("dve", WKRROT[:, :, 64:80], WKRROT[:, :, 64:80], -1.0, ALU.mult, reads=[], writes=[bWKRROT])
                for k in range(KC):
                    mm(PS[4][0:96, :], WKR[:, k, :], XNT[:, k, :], k == 0, k == KC - 1,
                       reads=[bWKR, bWKZ, bXNT[k]], writes=[bPS[4]])
                for k in range(KC):
                    mm(PS[5][0:96, :], WKRROT[:, k, :], XNT[:, k, :], k == 0, k == KC - 1,
                       reads=[bWKRROT, bWKZ, bXNT[k]], writes=[bPS[5]])
                if t + 1 < NT:
                    emit_xn(t + 1, l * 8, XNTS[(t + 1) % 2], bXNTS[(t + 1) % 2], SQ2, bSQ2, RS2, bRS2, 7)
                rms_scale([CF32[:, 3 + c, :] for c in range(2)], bCF32[3:5], 256, SQ, bSQ, RS, bRS, 6)
                for c in range(2):
                    stt("dve", CKVN[:, c, :], CF32[:, 3 + c, :], vcol(kg + c), RS, ALU.mult, ALU.mult,
                        reads=[bCF32[3 + c], bRS, bCONST], writes=[bCKVN[c]])
                rms_scale([CF32[:, c, :] for c in range(3)], bCF32[0:3], 384, SQ, bSQ, RS, bRS, 6)
                for c in range(3):
                    stt("dve", CQN[:, c, tsl(t)], CF32[:, c, :], vcol(qg + c), RS, ALU.mult, ALU.mult,
                        reads=[bCF32[c], bRS, bCONST], writes=[bCQN[c][t]])
                tt("dve", T1[R, :], PS[4][R, :], COS[R, tsl(t)], ALU.mult, reads=[bPS[4], bROPE[t]], writes=[bT1])
                tt("dve", T2[R, :], PS[5][R, :], SIN[R, tsl(t)], ALU.mult, reads=[bPS[5], bROPE[t]], writes=[bT2])
                tt("dve", KT[R, 0, tsl(t)], T1[R, :], T2[R, :], ALU.add, reads=[bT1, bT2], writes=[bKT[0][t]])
                for h in range(1, NH):
                    cp("act", KT[R, h, tsl(t)], KT[R, 0, tsl(t)], reads=[bKT[0][t]], writes=[bKT[h][t]])
                for hp in range(4):
                    bk = hp % 4
                    for k in range(2):
                        mm(PS[bk][:], WKVK[:, k, hp * 128:(hp + 1) * 128], CKVN[:, k, :], k == 0, k == 1,
                           reads=[bWKVK, bCKVN[k]], writes=[bPS[bk]])
                    cp("act", KT[0:64, 2 * hp, tsl(t)], PS[bk][0:64, :], reads=[bPS[bk]], writes=[bKT[2 * hp][t]])
                    cp("dve", KT[0:64, 2 * hp + 1, tsl(t)], PS[bk][64:128, :], reads=[bPS[bk]],
                       writes=[bKT[2 * hp + 1][t]])
                for bi in range(4):
                    blk = 4 * t + bi
                    bk = 4 + (bi % 2)
                    for k in range(2):
                        mm(PS[bk][:], CKVN[:, k, bi * 128:(bi + 1) * 128], WKVV[:, k, :], k == 0, k == 1,
                           reads=[bWKVV, bCKVN[k]], writes=[bPS[bk]])
                    cp("act", V[:, blk, :, 0:64], PS[bk][:].rearrange("p (h e) -> p h e", e=64),
                       reads=[bPS[bk]], writes=[bV[blk]])
                if t + 1 < NT:
                    rope_pass(t + 1)
            ts("dve", WQROT[:, :, :, 64:80], WQROT[:, :, :, 64:80], -1.0, ALU.mult, reads=[], writes=[bWQROT])

            sch.barrier()
            al = Alloc(p12)
            QT = [al.get([NH, TT], BF16) for _ in range(2)]
            PT = [al.get([TT], BF16) for _ in range(4)]
            ONUM = [al.get([TT], F32) for _ in range(2)]
            RDEN = [al.get([TT], F32) for _ in range(2)]
            RD = [al.get([TT], BF16) for _ in range(2)]
            T1s = [al.get([TT], F32) for _ in range(2)]
            _t2 = al.get([TT], F32)
            T2s = [_t2, _t2]
            assert al.off <= ARENA_BYTES - 16384, al.off
            ATT = view(ARENA_BYTES - 16384, [4, S], BF16)
            WC = view(c_off, [KC, 1536], BF16)
            bWC = Buf()
            bQT = [[Buf() for _ in range(NH)] for _ in range(2)]
            bPT = [Buf() for _ in range(4)]
            bONUM, bRDEN = [Buf(), Buf()], [Buf(), Buf()]
            bRD = [Buf(), Buf()]
            for q in range(2):
                memset("dve", RD[q], 0.0, [bRD[q]])
            _b2 = Buf()
            bT1s, bT2s = [Buf(), Buf()], [_b2, _b2]
            bATT = [[Buf() for _ in range(NT)] for _ in range(NH)]

            QBUF = {3: 0, 2: 0, 0: 1, 1: 1}

            def qproj(qt, h):
                Q, bQ = QT[QBUF[qt]], bQT[QBUF[qt]]
                bq, br = 6, 7
                z = h % 2
                for k in range(3):
                    mm(PS[bq][0:96, :], WQ[:, k, h * 96:(h + 1) * 96], CQN[:, k, tsl(qt)], k == 0, k == 2,
                       reads=[bWQ, bCQN[k][qt]], writes=[bPS[bq]])
                for k in range(3):
                    mm(PS[br][0:96, :], WQROT[:, k, h, :], CQN[:, k, tsl(qt)], k == 0, k == 2,
                       reads=[bWQROT, bWQZ, bCQN[k][qt]], writes=[bPS[br]])
                cp("dve", Q[0:64, h, :], PS[bq][0:64, :], reads=[bPS[bq]], writes=[bQ[h]])
                tt("dve", T1s[z][R, :], PS[bq][R, :], COS[R, tsl(qt)], ALU.mult, reads=[bPS[bq], bROPE[qt]], writes=[bT1s[z]])
                tt("dve", T2s[z][R, :], PS[br][R, :], SIN[R, tsl(qt)], ALU.mult, reads=[bPS[br], bROPE[qt]], writes=[bT2s[z]])
                tt("dve", Q[R, h, :], T1s[z][R, :], T2s[z][R, :], ALU.add, reads=[bT1s[z], bT2s[z]], writes=[bQ[h]])

            def normalize_a(z, ob):
                cp("dve", ONUM[z][0:64, :], PS[ob][0:64, :], reads=[bPS[ob]], writes=[bONUM[z]])
                act(RDEN[z][64:65, :], PS[ob][64:65, :], AF.Ln, reads=[bPS[ob]], writes=[bRDEN[z]])
                act(RDEN[z][64:65, :], RDEN[z][64:65, :], AF.Exp, reads=[bRDEN[z]], writes=[bRDEN[z]], scale=-1.0)
                cp("dve", RD[z][64:65, :], RDEN[z][64:65, :], reads=[bRDEN[z]], writes=[bRD[z]])
                tt("dve", RD[z][0:1, :], RDEN[z][64:65, :], RD[z][64:65, :], ALU.subtract,
                   reads=[bRDEN[z], bRD[z]], writes=[bRD[z]])

            def normalize_b(z, qt, h):
                mm(PS[7][0:64, :], ONESB[0:65, 0:64], RD[z][0:65, :], True, True,
                   reads=[bRD[z], bCONST], writes=[bPS[7]])
                r0 = (h % 2) * 64
                tt("dve", ATT[r0:r0 + 64, h // 2, tsl(qt)], ONUM[z][0:64, :], PS[7][0:64, :], ALU.mult,
                   reads=[bONUM[z], bPS[7]], writes=[bATT[h][qt]])

            for h in range(NH):
                qproj(3, h)
            for h in range(NH):
                qproj(0, h)
            LA = 3
            NDEF = 14
            cnt = [0, 0]
            NEXTQ = {3: 2, 0: 1}
            for (qa, qb) in ((3, 0), (2, 1)):
                jobs = []
                for h in range(NH):
                    jobs.append((qa, h))
                    jobs.append((qb, h))
                items = [(ji, kc) for ji, (qt, h) in enumerate(jobs) for kc in range(4 * qt + 4)]
                slots = {}
                pending = []

                def SC(i):
                    ji, kc = items[i]
                    qt, h = jobs[ji]
                    Q, bQ = QT[QBUF[qt]], bQT[QBUF[qt]]
                    j = kc - 4 * qt
                    c0 = 128 * j if j > 0 else 0
                    sb = cnt[0] % 4
                    cnt[0] += 1
                    slots[i] = (sb, c0)
                    mm(PS[sb][:, c0:TT], KT[0:96, h, kc * 128:(kc + 1) * 128], Q[0:96, h, c0:TT],
                       True, j < 0, reads=[bKT[h][kc // 4], bQ[h]], writes=[bPS[sb]])
                    if j >= 0:
                        mm(PS[sb][:, c0:c0 + 128], IDENTB, TRIB, False, True, reads=[bCONST], writes=[bPS[sb]])

                def E(i):
                    ji, kc = items[i]
                    qt, h = jobs[ji]
                    nk = 4 * qt + 4
                    sb, c0 = slots.pop(i)
                    z = ji % 2
                    ob = 4 + z
                    p = cnt[1] % 4
                    cnt[1] += 1
                    act(PT[p][:, c0:TT], PS[sb][:, c0:TT], AF.Exp, reads=[bPS[sb]], writes=[bPT[p]], scale=SCALE)
                    mm(PS[ob][0:65, c0:TT], V[:, kc, h, :], PT[p][:, c0:TT], kc == 0, kc == nk - 1,
                       reads=[bV[kc], bPT[p]], writes=[bPS[ob]])
                    if kc == nk - 1:
                        normalize_a(z, ob)
                        if qt in NEXTQ:
                            qproj(NEXTQ[qt], h)
                            if qt == 0 and h == NH - 1:
                                dead = [b for row in bCQN for b in row] + [bWQ, bWQROT] + bROPE
                                sch.dma("pool", WC, win_v[:, :, 0:1536], writes=[bWC] + dead)
                        pending.append((i + NDEF, z, qt, h))

                n = len(items)
                for i in range(n + LA):
                    if i < n:
                        SC(i)
                    if i >= LA:
                        E(i - LA)
                    while pending and pending[0][0] <= i - LA:
                        _, z, qt, h = pending.pop(0)
                        normalize_b(z, qt, h)
                while pending:
                    _, z, qt, h = pending.pop(0)
                    normalize_b(z, qt, h)

            sch.barrier()
            al = Alloc()
            XNT3 = [al.get([KC, TT], BF16) for _ in range(2)]
            SQ = [al.get([TT], BF16) for _ in range(2)]
            RS = al.get([TT], F32)
            CSB = al.get([TT], F32)
            CU = al.get([4, TT + 16], F32)
            T1 = al.get([TT], F32)
            T2 = CSB
            p3_end = max(al.off, c_off - 16384)
            assert p3_end + 16384 <= c_off, p3_end
            CONVIN = view(ARENA_BYTES - 32768, [4, S], BF16)
            bXNT3 = [[Buf() for _ in range(KC)] for _ in range(2)]
            bSQ = [Buf(), Buf()]
            bRS, bCSB, bT1 = Buf(), Buf(), Buf()
            bT2 = bCSB
            bCU = [Buf() for _ in range(4)]
            bCONVIN = [[Buf() for _ in range(NT)] for _ in range(4)]
            memset("dve", CU[:, :, 0:2], 0.0, bCU)
            WGA = view(p3_end, [KC, 1024], BF16)
            al4 = Alloc(c_off + 24576)
            WO = al4.get([KC, D], BF16)
            WCO = al4.get([4, D], BF16)
            WMO = al4.get([4, D], BF16)
            assert al4.off <= ARENA_BYTES - 32768, al4.off
            bWGA, bWGB, bWCO, bWMO, bWO = Buf(), Buf(), Buf(), Buf(), Buf()
            wload(WGA, win_v[:, :, 2208:3232], bWGA)
            wload(WCO, kview(w_conv_out[l]), bWCO)
            wload(WMO, kview(w_mla_out[l]), bWMO)
            wload(WO, kview(w_o[l]), bWO)
            cw = 50 + l * 12
            for t in range(NT):
                XNT, bXNT = XNT3[t % 2], bXNT3[t % 2]
                if t == 0:
                    emit_xn(0, l * 8, XNT3[0], bXNT3[0], SQ, bSQ, RS, bRS, 7)
                for c in range(4):
                    if c == 2 and t + 1 < NT:
                        emit_xn(t + 1, l * 8, XNT3[(t + 1) % 2], bXNT3[(t + 1) % 2], SQ, bSQ, RS, bRS, 7)
                    pb = 3 * (c % 2)
                    bC, bU, bB = pb, pb + 1, pb + 2
                    for (bk, col0) in ((bC, 512), (bU, 1024), (bB, 0)):
                        for k in range(KC):
                            mm(PS[bk][:], WC[:, k, col0 + c * 128: col0 + (c + 1) * 128], XNT[:, k, :],
                               k == 0, k == KC - 1, reads=[bWC, bXNT[k]], writes=[bPS[bk]])
                    cp("act", CSB, PS[bC][:], reads=[bPS[bC]], writes=[bCSB])
                    tt("dve", CU[:, c, 2:TT + 2], CSB, PS[bU][:], ALU.mult, reads=[bCSB, bPS[bU], bCU[c]], writes=[bCU[c]])
                    ts("dve", T1, CU[:, c, 0:TT], vcol(cw + 0 * 4 + c), ALU.mult, reads=[bCU[c], bCONST], writes=[bT1])
                    stt("dve", T2, CU[:, c, 1:TT + 1], vcol(cw + 1 * 4 + c), T1, ALU.mult, ALU.add,
                        reads=[bCU[c], bT1, bCONST], writes=[bT2])
                    stt("dve", T1, CU[:, c, 2:TT + 2], vcol(cw + 2 * 4 + c), T2, ALU.mult, ALU.add,
                        reads=[bCU[c], bT2, bCONST], writes=[bT1])
                    tt("dve", CONVIN[:, c, tsl(t)], T1, PS[bB][:], ALU.mult, reads=[bT1, bPS[bB]],
                       writes=[bCONVIN[c][t]])
                    cp("act", CU[:, c, 0:2], CU[:, c, TT:TT + 2], reads=[bCU[c]], writes=[bCU[c]])

            sch.barrier()
            WGB = view(c_off, [KC, 1024], BF16)
            wload(WGB, win_v[:, :, 3232:4256], bWGB)
            al = Alloc()
            XNTS = [al.get([KC, TT], BF16) for _ in range(2)]
            MERGED = al.get([KC, TT], BF16)
            SG = [al.get([TT], F32) for _ in range(2)]
            SQ = [al.get([TT], BF16) for _ in range(2)]
            RS = al.get([TT], F32)
            assert al.off <= p3_end, al.off
            bXNTS = [[Buf() for _ in range(KC)] for _ in range(2)]
            bMERGED = [Buf() for _ in range(KC)]
            bSG = [Buf(), Buf()]
            bSQ = [Buf(), Buf()]
            bRS = Buf()
            emit_xn(0, l * 8, XNTS[0], bXNTS[0], SQ, bSQ, RS, bRS, 7)
            for t in range(NT):
                XNT, bXNT = XNTS[t % 2], bXNTS[t % 2]
                for j in range(KC):
                    if j == 4 and t + 1 < NT:
                        emit_xn(t + 1, l * 8, XNTS[(t + 1) % 2], bXNTS[(t + 1) % 2], SQ, bSQ, RS, bRS, 7)
                    pb = 0 if j % 2 == 0 else 3
                    b_gc, b_gm, b_yc = pb, pb + 1, pb + 2
                    b_ym = 6
                    js = slice(j * 128, (j + 1) * 128)
                    for k in range(KC):
                        mm(PS[b_gc][:], WGA[:, k, j * 128:(j + 1) * 128], XNT[:, k, :], k == 0, k == KC - 1,
                           reads=[bWGA, bXNT[k]], writes=[bPS[b_gc]])
                    for k in range(KC):
                        mm(PS[b_gm][:], WGB[:, k, j * 128:(j + 1) * 128], XNT[:, k, :], k == 0, k == KC - 1,
                           reads=[bWGB, bXNT[k]], writes=[bPS[b_gm]])
                    for k in range(4):
                        mm(PS[b_yc][:], WCO[:, k, js], CONVIN[:, k, tsl(t)], k == 0, k == 3,
                           reads=[bWCO, bCONVIN[k][t]], writes=[bPS[b_yc]])
                    for k in range(4):
                        mm(PS[b_ym][:], WMO[:, k, js], ATT[:, k, tsl(t)], k == 0, k == 3,
                           reads=[bWMO, bATT[2 * k][t], bATT[2 * k + 1][t]], writes=[bPS[b_ym]])
                    act(SG[0], PS[b_gc][:], AF.Sigmoid, reads=[bPS[b_gc]], writes=[bSG[0]])
                    act(SG[1], PS[b_gm][:], AF.Sigmoid, reads=[bPS[b_gm]], writes=[bSG[1]])
                    tt("dve", SG[0], SG[0], PS[b_yc][:], ALU.mult, reads=[bSG[0], bPS[b_yc]], writes=[bSG[0]])
                    tt("dve", SG[1], SG[1], PS[b_ym][:], ALU.mult, reads=[bSG[1], bPS[b_ym]], writes=[bSG[1]])
                    tt("dve", MERGED[:, j, :], SG[0], SG[1], ALU.add, reads=[bSG[0], bSG[1]], writes=[bMERGED[j]])
                for j in range(KC):
                    bk = 0 if j % 2 == 0 else 3
                    for k in range(KC):
                        mm(PS[bk][:], WO[:, k, j * 128:(j + 1) * 128], MERGED[:, k, :], k == 0, k == KC - 1,
                           reads=[bWO, bMERGED[k]], writes=[bPS[bk]])
                    tt("dve", XRES[:, j, tsl(t)], XRES[:, j, tsl(t)], PS[bk][:], ALU.add,
                       reads=[bX[j][t], bPS[bk]], writes=[bX[j][t]])

        def ffn(l):
            moe = (l % 2 == 1)
            i = l // 2
            sch.barrier()
            if (l + 1) in layers and not moe and do_mixer:
                w1_load(l + 1)
            al = Alloc()
            HN = al.get([KC, S], BF16)
            HB = [al.get([max(GSZ), S], BF16) for _ in range(2)]
            WGU = [(al.get([KC, 128], BF16), al.get([KC, 128], BF16)) for _ in range(3)]
            WDS = [al.get([D], BF16) for _ in range(8)]
            SG = [al.get([TT], F32) for _ in range(2)]
            _tf = al.get([TT], F32)
            TF = [_tf, _tf]
            SQ = [al.get([TT], BF16) for _ in range(2)]
            RS = al.get([TT], F32)
            if moe:
                CBC = al.get([S], F32)
                COMBT = al.get([S], F32)
                ROUT = al.get([KC, NE], F32)
                GR = al.get([KC, NE], F32)
                LG = al.get([8, 4 * NE], F32)
                SM = al.get([8, 8], F32)
            assert al.off <= ARENA_BYTES, al.off
            bHN = [[Buf() for _ in range(NT)] for _ in range(KC)]
            bHB = [[[Buf() for _ in range(NT)] for _ in range(max(GSZ))] for _ in range(2)]
            bWGU = [Buf() for _ in range(3)]
            bWDS = [Buf() for _ in range(8)]
            _btf = Buf()
            bSG, bTF, bSQ = [Buf(), Buf()], [_btf, _btf], [Buf(), Buf()]
            bRS, bCBC, bCOMBT, bGR, bLG = Buf(), [Buf() for _ in range(NT)], [Buf() for _ in range(NT)], Buf(), Buf()

            gcol = 16 + l * 8
            if moe:
                sch.dma("sp", ROUT, router[i].rearrange("(c p) e -> p c e", p=128), writes=[bGR])
                tt("dve", GR, ROUT, VECS[:, gcol:gcol + 8].unsqueeze(2).to_broadcast([128, KC, NE]), ALU.mult,
                   reads=[bGR, bCONST], writes=[bGR])
            def route_chain(t):
                pb = t

                def v3(i):
                    return LG[:, i, :].rearrange("p (b e) -> p b e", e=NE)

                def bc(ap2):
                    return ap2.unsqueeze(2).to_broadcast([128, 4, NE])
                lg, lg2, e1, e2, cmb = v3(0), v3(1), v3(2), v3(3), v3(4)
                rstd, m1, m2, dm, w1, w2 = (SM[:, 0, 0:4], SM[:, 1, 0:4], SM[:, 2, 0:4], SM[:, 3, 0:4],
                                            SM[:, 4, 0:4], SM[:, 5, 0:4])
                cp("dve", rstd, PS[pb][:, 64:68], reads=[bPS[pb]], writes=[bLG])
                tt("dve", lg, PS[pb][:, 0:4 * NE].rearrange("p (b e) -> p b e", e=NE), bc(rstd), ALU.mult,
                   reads=[bPS[pb], bLG], writes=[bLG])
                sch.op("dve", lambda h, o=m1, i_=lg: h.reduce_max(out=o, in_=i_, axis=AX.X), reads=[bLG], writes=[bLG])
                tt("dve", e1, lg, bc(m1), ALU.is_ge, reads=[bLG], writes=[bLG])
                stt("dve", lg2, e1, -1e30, lg, ALU.mult, ALU.add, reads=[bLG], writes=[bLG])
                sch.op("dve", lambda h, o=m2, i_=lg2: h.reduce_max(out=o, in_=i_, axis=AX.X), reads=[bLG], writes=[bLG])
                tt("dve", e2, lg2, bc(m2), ALU.is_ge, reads=[bLG], writes=[bLG])
                tt("dve", dm, m2, m1, ALU.subtract, reads=[bLG], writes=[bLG])
                act(w2, dm, AF.Sigmoid, reads=[bLG], writes=[bLG])
                act(w1, dm, AF.Sigmoid, reads=[bLG], writes=[bLG], scale=-1.0)
                tt("dve", cmb, e1, bc(w1), ALU.mult, reads=[bLG], writes=[bLG])
                tt("dve", e2, e2, bc(w2), ALU.mult, reads=[bLG], writes=[bLG])
                tt("dve", cmb, cmb, e2, ALU.add, reads=[bLG], writes=[bLG])
                for bi in range(4):
                    mm(PS[6][0:NE, bi * 128:(bi + 1) * 128], LG[:, 4, bi * NE:(bi + 1) * NE], IDENTF, True, True,
                       reads=[bLG, bCONST], writes=[bPS[6]])
                cp("act", COMBT[0:NE, tsl(t)], PS[6][0:NE, :], reads=[bPS[6]], writes=[bCOMBT[t]])

            for t in range(NT):
                rms_scale([XRES[:, c, tsl(t)] for c in range(KC)], [bX[c][t] for c in range(KC)], D,
                          SQ, bSQ, RS, bRS, 7)
                for c in range(KC):
                    stt("dve", HN[:, c, tsl(t)], XRES[:, c, tsl(t)], vcol(gcol + c), RS, ALU.mult, ALU.mult,
                        reads=[bX[c][t], bRS, bCONST], writes=[bHN[c][t]])
                if moe:
                    for bi in range(4):
                        tok = slice(t * TT + bi * 128, t * TT + (bi + 1) * 128)
                        for k in range(KC):
                            mm(PS[t][:, bi * NE:(bi + 1) * NE], XRES[:, k, tok], GR[:, k, :], k == 0, k == KC - 1,
                               reads=[bX[k][t], bGR], writes=[bPS[t]])
                        mm(PS[t][:, 64 + bi:65 + bi], RS[0:1, bi * 128:(bi + 1) * 128], ONESF[0:1, 0:1], True, True,
                           reads=[bRS, bCONST], writes=[bPS[t]])
                    if t > 0:
                        route_chain(t - 1)
            if moe:
                route_chain(NT - 1)

            if moe:
                experts = [(w_gate_e[i, e], w_up_e[i, e], w_down_e[i, e], e) for e in range(NE)]
            else:
                experts = [(w_gate[i][:, h * DFE:(h + 1) * DFE], w_up[i][:, h * DFE:(h + 1) * DFE],
                            w_down[i][h * DFE:(h + 1) * DFE, :], None) for h in range(2)]
            gi = 0
            wi = 0
            di = 0
            ev = 0
            for (wg_d, wu_d, wd_d, e) in experts:
                wg_v = kview(wg_d)
                wu_v = kview(wu_d)
                if moe:
                    for t in range(NT):
                        cb = 6 + (t % 2)
                        mm(PS[cb][:], SELF[0:NE, e, :], COMBT[0:NE, tsl(t)], True, True,
                           reads=[bCOMBT[t], bCONST], writes=[bPS[cb]])
                        cp("act", CBC[:, tsl(t)], PS[cb][:], reads=[bPS[cb]], writes=[bCBC[t]])
                c0 = 0
                for G in GSZ:
                    hb = gi % 2
                    gi += 1
                    dslots = []
                    for cc in range(G):
                        f = c0 + cc
                        ws = wi % 3
                        wi += 1
                        ds_ = di % 8
                        di += 1
                        dslots.append(ds_)
                        wload(WGU[ws][0], wg_v[:, :, f * 128:(f + 1) * 128], bWGU[ws])
                        wload(WGU[ws][1], wu_v[:, :, f * 128:(f + 1) * 128], bWGU[ws])
                        wload(WDS[ds_], wd_d[f * 128:(f + 1) * 128, :], bWDS[ds_])
                        for t in range(NT):
                            pg = (ev % 2) * 2
                            pu = pg + 1
                            sg = ev % 2
                            ev += 1
                            for k in range(KC):
                                mm(PS[pg][:], WGU[ws][0][:, k, :], HN[:, k, tsl(t)], k == 0, k == KC - 1,
                                   reads=[bWGU[ws], bHN[k][t]], writes=[bPS[pg]])
                            for k in range(KC):
                                mm(PS[pu][:], WGU[ws][1][:, k, :], HN[:, k, tsl(t)], k == 0, k == KC - 1,
                                   reads=[bWGU[ws], bHN[k][t]], writes=[bPS[pu]])
                            act(SG[sg], PS[pg][:], AF.Silu, reads=[bPS[pg]], writes=[bSG[sg]])
                            if moe:
                                tt("dve", TF[sg], SG[sg], PS[pu][:], ALU.mult, reads=[bSG[sg], bPS[pu]], writes=[bTF[sg]])
                                tt("dve", HB[hb][:, cc, tsl(t)], TF[sg], CBC[:, tsl(t)], ALU.mult,
                                   reads=[bTF[sg], bCBC[t]], writes=[bHB[hb][cc][t]])
                            else:
                                tt("dve", HB[hb][:, cc, tsl(t)], SG[sg], PS[pu][:], ALU.mult,
                                   reads=[bSG[sg], bPS[pu]], writes=[bHB[hb][cc][t]])
                    dj = 0
                    for j in range(KC):
                        for t in range(NT):
                            pd = 4 + (dj % 2)
                            dj += 1
                            for cc in range(G):
                                mm(PS[pd][:], WDS[dslots[cc]][:, j * 128:(j + 1) * 128], HB[hb][:, cc, tsl(t)],
                                   cc == 0, cc == G - 1, reads=[bWDS[dslots[cc]], bHB[hb][cc][t]], writes=[bPS[pd]])
                            tt("dve", XRES[:, j, tsl(t)], XRES[:, j, tsl(t)], PS[pd][:], ALU.add,
                               reads=[bX[j][t], bPS[pd]], writes=[bX[j][t]])
                    c0 += G

        if not do_mixer:
            load_x_tiles(1, NT)
            x_loaded[0] = True
        for l in layers:
            if do_mixer:
                mixer(l)
            if do_ffn:
                ffn(l)

        sch.barrier()
        al = Alloc()
        OUTS = [al.get([KC, TT], F32) for _ in range(2)]
        SQ = [al.get([TT], BF16) for _ in range(2)]
        RS = al.get([TT], F32)
        bOUT = [[Buf() for _ in range(KC)] for _ in range(2)]
        bSQ = [Buf(), Buf()]
        bRS = Buf()
        for t in range(NT):
            o = t % 2
            if last:
                rms_scale([XRES[:, c, tsl(t)] for c in range(KC)], [bX[c][t] for c in range(KC)], D,
                          SQ, bSQ, RS, bRS, 7)
                for c in range(KC):
                    stt("dve", OUTS[o][:, c, :], XRES[:, c, tsl(t)], vcol(32 + c), RS, ALU.mult, ALU.mult,
                        reads=[bX[c][t], bRS, bCONST], writes=[bOUT[o][c]])
                    sch.dma("sp", outT[c * 128:(c + 1) * 128, tsl(t)], OUTS[o][:, c, :], reads=[bOUT[o][c]])
            else:
                for c in range(KC):
                    sch.dma("sp", outT[c * 128:(c + 1) * 128, tsl(t)], XRES[:, c, tsl(t)], reads=[bX[c][t]])
        sch.final_wait("sp")
        sch.finalize()

        @block.tensor
        def _(h):
            sch.emit("pe", h)

        @block.scalar
        def _(h):
            sch.emit("act", h)

        @block.vector
        def _(h):
            sch.emit("dve", h)

        @block.gpsimd
        def _(h):
            sch.emit("pool", h)

        @block.sync
        def _(h):
            sch.emit("sp", h)

    return nc


def _host_consts():
    cf = np.zeros((128, 128 + 1024), np.float32)
    cf[:, 0:128] = np.eye(128, dtype=np.float32)
    for e in range(8):
        cf[e, 128 + e * 128: 128 + (e + 1) * 128] = 1.0
    cb = np.zeros((128, 256), np.float32)
    cb[:, 0:128] = np.eye(128, dtype=np.float32)
    k = np.arange(128)[:, None]
    q = np.arange(128)[None, :]
    cb[:, 128:256] = np.where(q >= k, 0.0, -30000.0).astype(np.float32)
    return cf, cb


def _shared_inputs(inp):
    f = lambda a: np.ascontiguousarray(np.asarray(a, dtype=np.float32))
    vecs = np.zeros((128, NV), np.float32)

    def put(col, v):
        v = np.asarray(v, np.float32).reshape(-1, 128)
        vecs[:, col:col + v.shape[0]] = v.T

    for l in range(2):
        put(0 + l * 8, inp["attn_norm"][l])
        put(16 + l * 8, inp["ffn_norm"][l])
        put(40 + l * 3, inp["q_norm"][l])
        put(46 + l * 2, inp["kv_norm"][l])
        for k in range(3):
            put(50 + l * 12 + k * 4, inp["conv_w"][l, k])
    put(32, inp["final_norm"])
    inv_freq = (10000.0 ** (-np.arange(0, 32, 2, dtype=np.float32) / np.float32(32))).astype(np.float32)
    vecs[64:96, 74] = np.concatenate([inv_freq, inv_freq])
    cf, cb = _host_consts()
    w_uq = f(inp["w_uq"])
    w_in = f(inp["w_in"])
    wq4 = w_uq.reshape(2, 384, 8, 96)
    wq_rot = np.concatenate([wq4[..., 80:96], wq4[..., 64:80]], axis=-1).reshape(2, 384, 256)
    wkr = w_in[:, :, 2176:2208]
    wkr_rot = np.concatenate([w_in[:, :, 2192:2208], w_in[:, :, 2176:2192]], axis=-1)
    sh = {
        "vecs": vecs, "cf": cf, "cb": cb, "w_in": w_in, "w_conv_out": f(inp["w_conv_out"]),
        "w_uq": w_uq, "wq_rot": f(wq_rot), "wkr": f(wkr), "wkr_rot": f(wkr_rot),
        "w_ukv": f(inp["w_ukv"]), "w_mla_out": f(inp["w_mla_out"]), "w_o": f(inp["w_o"]),
        "w_gate": f(inp["w_gate"]), "w_up": f(inp["w_up"]), "w_down": f(inp["w_down"]),
        "router": f(inp["router"]), "w_gate_e": f(inp["w_gate_e"]), "w_up_e": f(inp["w_up_e"]),
        "w_down_e": f(inp["w_down_e"]),
    }
    return sh


_NC_CACHE = {}


def run_layers(inp, xT_list, layers, last):
    key = (tuple(layers), last)
    if key not in _NC_CACHE:
        _NC_CACHE[key] = build_nc(layers, last=last)
    nc = _NC_CACHE[key]
    sh = _shared_inputs(inp)
    pos = np.asarray(inp["positions"]).astype(np.int32)
    in_maps = []
    for b in range(8):
        m = dict(sh)
        m["xT"] = np.ascontiguousarray(xT_list[b])
        m["posrep"] = np.ascontiguousarray(np.broadcast_to(pos[b][None, :], (32, S)))
        in_maps.append(m)
    res = run_bass_kernel_spmd(nc, in_maps, core_ids=list(range(8)))
    return [r["outT"] for r in res.results]


def kernel(**inputs):
    x = np.asarray(inputs["x"], dtype=np.float32)
    xT = [np.ascontiguousarray(x[b].T) for b in range(8)]
    outs = run_layers(inputs, xT, [0, 1], True)
    return np.stack([np.ascontiguousarray(o.T) for o in outs], axis=0).astype(np.float32)
```
